# Optimizing a Trainium2 kernel written in Bass

```python
import math
import jax
import jax.numpy as jnp
from jax import lax
import numpy as np

D_MODEL = 1024
BATCH = 4
SEQ = 4096
DEPTH = 1

N_HEADS = 8
HEAD_DIM = 64
N_KV = 2
GQA_RATIO = N_HEADS // N_KV
CMP_BLOCK = 32
CMP_STRIDE = 16
CMP_HIDDEN = 256
SEL_BLOCK = 64
N_SELECT = 16
WINDOW = 512
Q_BLOCK = 128
ATTN_SCALE = HEAD_DIM ** -0.5
NSA_WIDTH = N_HEADS * HEAD_DIM
KV_WIDTH = N_KV * HEAD_DIM
SSM_WIDTH = 512
GROUP = 16
N_GROUPS = SSM_WIDTH // GROUP
STATE = 64
DT_MIN = 1e-3
DT_MAX = 1e-1
N_EXPERTS = 256
TOP_K = 8
D_EXPERT = 256
N_EXPERT_GROUPS = 8
TOPK_GROUPS = 4
ROUTE_SCALE = 2.5
DISPATCH_BLOCK = 128
EPS = 1e-6
ADA_SCALE = 0.3
IN_SPLITS = (NSA_WIDTH,) + (KV_WIDTH,) * 6 + (3 * N_HEADS, SSM_WIDTH, D_MODEL, D_MODEL)
IN_COLS = sum(IN_SPLITS)

kernel_name = 'hybrid_nsa_s5_moe_block'


def rms_norm(x, g):
    xf = x.astype(jnp.float32)
    y = xf * lax.rsqrt(jnp.mean(xf * xf, axis=-1, keepdims=True) + EPS)
    return (y * g.astype(jnp.float32)).astype(x.dtype)


def masked_softmax(s, mask):
    s = jnp.where(mask, s.astype(jnp.float32), -1e30)
    return jax.nn.softmax(s, axis=-1) * mask


def swiglu(x, wg, wu, wd):
    return (jax.nn.silu(x @ wg) * (x @ wu)) @ wd


def compress_blocks(k, pe, w1, w2):
    B, S, G, Dh = k.shape
    nc = (S - CMP_BLOCK) // CMP_STRIDE + 1
    idx = CMP_STRIDE * jnp.arange(nc)[:, None] + jnp.arange(CMP_BLOCK)[None, :]
    kb = k[:, idx] + pe[None, None, :, None, :]
    kb = jnp.transpose(kb, (0, 1, 3, 2, 4)).reshape(B, nc, G, CMP_BLOCK * Dh)
    return jax.nn.gelu(kb @ w1) @ w2


def compressed_attention(q, kc, vc):
    S = q.shape[1]
    nc = kc.shape[1]
    t = jnp.arange(S)
    block_end = CMP_STRIDE * jnp.arange(nc) + CMP_BLOCK - 1
    mask = block_end[None, :] <= t[:, None]
    s = jnp.einsum('bsgrd,bngd->bgrsn', q, kc) * ATTN_SCALE
    p = masked_softmax(s, mask)
    o = jnp.einsum('bgrsn,bngd->bsgrd', p.astype(vc.dtype), vc)
    return o, p


def select_blocks(p_cmp, S):
    nc = p_cmp.shape[-1]
    nsb = S // SEL_BLOCK
    cstart = CMP_STRIDE * jnp.arange(nc)
    sstart = SEL_BLOCK * jnp.arange(nsb)
    overlap = ((cstart[:, None] < sstart[None, :] + SEL_BLOCK)
               & (cstart[:, None] + CMP_BLOCK > sstart[None, :])).astype(jnp.float32)
    imp = jnp.einsum('bgrsn,nj->bgsj', p_cmp, overlap)
    cur = (jnp.arange(S) // SEL_BLOCK)[:, None]
    j = jnp.arange(nsb)[None, :]
    valid = j <= cur
    forced = (j == 0) | (j == cur) | (j == cur - 1)
    imp = jnp.where(forced, jnp.inf, jnp.where(valid, imp, -jnp.inf))
    n_sel = min(N_SELECT, nsb)
    _, idx = lax.top_k(imp, n_sel)
    return idx


def selected_attention(q, ks, vs, idx):
    B, S, G, R, Dh = q.shape
    nsb = S // SEL_BLOCK
    kb = ks.reshape(B, nsb, SEL_BLOCK, G, Dh).transpose(0, 3, 1, 2, 4)
    vb = vs.reshape(B, nsb, SEL_BLOCK, G, Dh).transpose(0, 3, 1, 2, 4)
    qc = q.reshape(B, nsb, SEL_BLOCK, G, R, Dh).transpose(1, 0, 2, 3, 4, 5)
    ic = idx.reshape(B, G, nsb, SEL_BLOCK, -1).transpose(2, 0, 1, 3, 4)
    bi = jnp.arange(B)[:, None, None, None]
    gi = jnp.arange(G)[None, :, None, None]
    offs = jnp.arange(SEL_BLOCK)

    def chunk(args):
        qb, ib, ci = args
        kg = kb[bi, gi, ib]
        vg = vb[bi, gi, ib]
        t = ci * SEL_BLOCK + offs
        kpos = ib[..., None] * SEL_BLOCK + offs
        mask = kpos <= t[None, None, :, None, None]
        s = jnp.einsum('bqgrd,bgqnkd->bgrqnk', qb, kg) * ATTN_SCALE
        shp = s.shape
        m = jnp.broadcast_to(mask[:, :, None], shp).reshape(shp[:4] + (-1,))
        p = masked_softmax(s.reshape(shp[:4] + (-1,)), m).reshape(shp)
        return jnp.einsum('bgrqnk,bgqnkd->bqgrd', p.astype(vg.dtype), vg)

    o = lax.map(chunk, (qc, ic, jnp.arange(nsb)))
    return o.transpose(1, 0, 2, 3, 4, 5).reshape(B, S, G, R, Dh)


def window_attention(q, kw, vw):
    B, S, G, R, Dh = q.shape
    nq = S // Q_BLOCK
    nback = WINDOW // Q_BLOCK
    pad = ((0, 0), (WINDOW, 0), (0, 0), (0, 0))
    kp = jnp.pad(kw, pad).reshape(B, nq + nback, Q_BLOCK, G, Dh)
    vp = jnp.pad(vw, pad).reshape(B, nq + nback, Q_BLOCK, G, Dh)
    kwin = jnp.concatenate([kp[:, j:j + nq] for j in range(nback + 1)], axis=2)
    vwin = jnp.concatenate([vp[:, j:j + nq] for j in range(nback + 1)], axis=2)
    qb = q.reshape(B, nq, Q_BLOCK, G, R, Dh)
    blk = jnp.arange(nq)[:, None] * Q_BLOCK
    t = blk + jnp.arange(Q_BLOCK)[None, :]
    kpos = blk - WINDOW + jnp.arange((nback + 1) * Q_BLOCK)[None, :]
    diff = t[:, :, None] - kpos[:, None, :]
    mask = (diff >= 0) & (diff < WINDOW) & (kpos[:, None, :] >= 0)
    s = jnp.einsum('bnqgrd,bnkgd->bgrnqk', qb, kwin) * ATTN_SCALE
    p = masked_softmax(s, mask)
    o = jnp.einsum('bgrnqk,bnkgd->bnqgrd', p.astype(vwin.dtype), vwin)
    return o.reshape(B, S, G, R, Dh)


def s5_mixer(u, a_re, a_im, log_dt, b_re, b_im, c_re, c_im, d_skip, w_glu, b_glu):
    B, S, _ = u.shape
    f32 = jnp.float32
    lam = lax.complex(a_re.astype(f32), a_im.astype(f32))
    dt = jnp.exp(log_dt.astype(f32))[:, None]
    lam_bar = jnp.exp(lam * dt)
    b_mat = lax.complex(b_re.astype(f32), b_im.astype(f32))
    b_bar = ((lam_bar - 1.0) / lam)[..., None] * b_mat
    c_mat = lax.complex(c_re.astype(f32), c_im.astype(f32))
    uf = u.astype(f32)
    ug = uf.reshape(B, S, N_GROUPS, GROUP).astype(jnp.complex64)
    bu = jnp.einsum('bsgp,gnp->bsgn', ug, b_bar)
    a = jnp.broadcast_to(lam_bar, bu.shape)

    def combine(left, right):
        a1, b1 = left
        a2, b2 = right
        return a1 * a2, a2 * b1 + b2

    _, states = lax.associative_scan(combine, (a, bu), axis=1)
    y = jnp.einsum('bsgn,gpn->bsgp', states, c_mat).real.reshape(B, S, SSM_WIDTH)
    y = (y + d_skip.astype(f32) * uf).astype(u.dtype)
    z = jax.nn.gelu(y)
    return z * jax.nn.sigmoid(z @ w_glu + b_glu)


def route(h, w_router, router_bias):
    f32 = jnp.float32
    n = h.shape[0]
    scores = jax.nn.sigmoid(h.astype(f32) @ w_router.astype(f32))
    biased = scores + router_bias.astype(f32)
    grp = biased.reshape(n, N_EXPERT_GROUPS, N_EXPERTS // N_EXPERT_GROUPS)
    grp_score = jnp.sum(lax.top_k(grp, 2)[0], axis=-1)
    _, gidx = lax.top_k(grp_score, TOPK_GROUPS)
    gmask = jnp.any(gidx[:, :, None] == jnp.arange(N_EXPERT_GROUPS)[None, None, :], axis=1)
    emask = jnp.repeat(gmask, N_EXPERTS // N_EXPERT_GROUPS, axis=1)
    _, eidx = lax.top_k(jnp.where(emask, biased, -jnp.inf), TOP_K)
    w = jnp.take_along_axis(scores, eidx, axis=1)
    w = w / jnp.sum(w, axis=-1, keepdims=True) * ROUTE_SCALE
    return eidx, w


def routed_experts(h, eidx, w, w_gate, w_up, w_down):
    n, d = h.shape
    nk = n * TOP_K
    e_flat = eidx.reshape(-1)
    tok_flat = jnp.repeat(jnp.arange(n, dtype=jnp.int32), TOP_K)
    w_flat = w.reshape(-1)
    order = jnp.argsort(e_flat)
    se = e_flat[order]
    stok = tok_flat[order]
    sw = w_flat[order]
    counts = jax.ops.segment_sum(jnp.ones_like(se), se, num_segments=N_EXPERTS)
    start = jnp.cumsum(counts) - counts
    padded = (counts + DISPATCH_BLOCK - 1) // DISPATCH_BLOCK * DISPATCH_BLOCK
    pad_end = jnp.cumsum(padded)
    pad_start = pad_end - padded
    dest = pad_start[se] + jnp.arange(nk, dtype=jnp.int32) - start[se]
    cap = (nk + N_EXPERTS * DISPATCH_BLOCK + DISPATCH_BLOCK - 1) // DISPATCH_BLOCK * DISPATCH_BLOCK
    nb = cap // DISPATCH_BLOCK
    buf_tok = jnp.full((cap,), n, jnp.int32).at[dest].set(stok)
    buf_w = jnp.zeros((cap,), h.dtype).at[dest].set(sw.astype(h.dtype))
    blk_e = jnp.minimum(jnp.searchsorted(pad_end, jnp.arange(nb, dtype=jnp.int32) * DISPATCH_BLOCK,
                                         side='right'), N_EXPERTS - 1)
    hp = jnp.concatenate([h, jnp.zeros((1, d), h.dtype)], axis=0)

    def run(args):
        toks, e = args
        return swiglu(hp[toks], w_gate[e], w_up[e], w_down[e])

    y = lax.map(run, (buf_tok.reshape(nb, DISPATCH_BLOCK), blk_e)).reshape(cap, d)
    y = y * buf_w[:, None]
    return jnp.zeros((n + 1, d), h.dtype).at[buf_tok].add(y)[:n]


def hybrid_block(x, c, w_ada, b_ada, g_norm1, g_norm2, w_in, q_gain, kc_gain, ks_gain, kw_gain,
                 pe_k, pe_v, w_cmp_k1, w_cmp_k2, w_cmp_v1, w_cmp_v2,
                 a_re, a_im, log_dt, b_re, b_im, c_re, c_im, d_skip, w_glu, b_glu,
                 w_up_attn, w_up_ssm, w_out, w_router, router_bias,
                 w_gate, w_up, w_down, ws_gate, ws_up, ws_down):
    B, S, D = x.shape
    mod = jax.nn.silu(c) @ w_ada + b_ada
    sh1, sc1, gt1, sh2, sc2, gt2 = jnp.split(mod[:, None, :], 6, axis=-1)

    h = rms_norm(x, g_norm1) * (1.0 + sc1) + sh1
    parts = jnp.split(h @ w_in, np.cumsum(IN_SPLITS)[:-1].tolist(), axis=-1)
    q, kc, vc, ks, vs, kw, vw, g_nsa, u, g_attn, g_ssm = parts
    q = rms_norm(q.reshape(B, S, N_HEADS, HEAD_DIM), q_gain).reshape(B, S, N_KV, GQA_RATIO, HEAD_DIM)
    kc = rms_norm(compress_blocks(kc.reshape(B, S, N_KV, HEAD_DIM), pe_k, w_cmp_k1, w_cmp_k2), kc_gain)
    vc = compress_blocks(vc.reshape(B, S, N_KV, HEAD_DIM), pe_v, w_cmp_v1, w_cmp_v2)
    ks = rms_norm(ks.reshape(B, S, N_KV, HEAD_DIM), ks_gain)
    vs = vs.reshape(B, S, N_KV, HEAD_DIM)
    kw = rms_norm(kw.reshape(B, S, N_KV, HEAD_DIM), kw_gain)
    vw = vw.reshape(B, S, N_KV, HEAD_DIM)
    o_cmp, p_cmp = compressed_attention(q, kc, vc)
    sel_idx = select_blocks(p_cmp, S)
    o_sel = selected_attention(q, ks, vs, sel_idx)
    o_win = window_attention(q, kw, vw)
    g = jax.nn.sigmoid(g_nsa).reshape(B, S, N_KV, GQA_RATIO, 3)
    o_nsa = (g[..., 0:1] * o_cmp + g[..., 1:2] * o_sel + g[..., 2:3] * o_win).reshape(B, S, NSA_WIDTH)
    y_ssm = s5_mixer(u, a_re, a_im, log_dt, b_re, b_im, c_re, c_im, d_skip, w_glu, b_glu)
    merged = (jax.nn.sigmoid(g_attn) * (o_nsa @ w_up_attn)
              + jax.nn.sigmoid(g_ssm) * (y_ssm @ w_up_ssm))
    x = x + gt1 * (merged @ w_out)

    h2 = rms_norm(x, g_norm2) * (1.0 + sc2) + sh2
    hf = h2.reshape(B * S, D)
    eidx, ew = route(hf, w_router, router_bias)
    y = swiglu(hf, ws_gate, ws_up, ws_down) + routed_experts(hf, eidx, ew, w_gate, w_up, w_down)
    return x + gt2 * y.reshape(B, S, D)


def setup_inputs(seed: int = 0) -> dict:
    key = jax.random.key(seed)
    keys = iter(jax.random.split(key, 48))
    f32 = jnp.float32
    L = DEPTH

    def nrm(shape, scale):
        return scale * jax.random.normal(next(keys), shape, f32)

    def gain(shape):
        return 1.0 + nrm(shape, 0.01)

    D = D_MODEL
    inp = {}
    inp['x'] = nrm((BATCH, SEQ, D), 1.0)
    inp['c'] = nrm((BATCH, D), 1.0)
    inp['w_ada'] = nrm((L, D, 6 * D), ADA_SCALE * D ** -0.5)
    inp['b_ada'] = nrm((L, 6 * D), 0.01)
    inp['g_norm1'] = gain((L, D))
    inp['g_norm2'] = gain((L, D))
    inp['w_in'] = nrm((L, D, IN_COLS), D ** -0.5)
    inp['q_gain'] = gain((L, HEAD_DIM))
    inp['kc_gain'] = gain((L, HEAD_DIM))
    inp['ks_gain'] = gain((L, HEAD_DIM))
    inp['kw_gain'] = gain((L, HEAD_DIM))
    inp['pe_k'] = nrm((L, CMP_BLOCK, HEAD_DIM), 0.02)
    inp['pe_v'] = nrm((L, CMP_BLOCK, HEAD_DIM), 0.02)
    inp['w_cmp_k1'] = nrm((L, CMP_BLOCK * HEAD_DIM, CMP_HIDDEN), (CMP_BLOCK * HEAD_DIM) ** -0.5)
    inp['w_cmp_k2'] = nrm((L, CMP_HIDDEN, HEAD_DIM), CMP_HIDDEN ** -0.5)
    inp['w_cmp_v1'] = nrm((L, CMP_BLOCK * HEAD_DIM, CMP_HIDDEN), (CMP_BLOCK * HEAD_DIM) ** -0.5)
    inp['w_cmp_v2'] = nrm((L, CMP_HIDDEN, HEAD_DIM), CMP_HIDDEN ** -0.5)
    inp['a_re'] = -0.5 + nrm((L, N_GROUPS, STATE), 0.01)
    inp['a_im'] = math.pi * jnp.arange(STATE, dtype=f32)[None, None, :] + nrm((L, N_GROUPS, STATE), 0.01)
    inp['log_dt'] = jax.random.uniform(next(keys), (L, N_GROUPS), f32,
                                       minval=math.log(DT_MIN), maxval=math.log(DT_MAX))
    inp['b_re'] = nrm((L, N_GROUPS, STATE, GROUP), (2 * GROUP) ** -0.5)
    inp['b_im'] = nrm((L, N_GROUPS, STATE, GROUP), (2 * GROUP) ** -0.5)
    inp['c_re'] = nrm((L, N_GROUPS, GROUP, STATE), STATE ** -0.5)
    inp['c_im'] = nrm((L, N_GROUPS, GROUP, STATE), STATE ** -0.5)
    inp['d_skip'] = nrm((L, SSM_WIDTH), 1.0)
    inp['w_glu'] = nrm((L, SSM_WIDTH, SSM_WIDTH), SSM_WIDTH ** -0.5)
    inp['b_glu'] = nrm((L, SSM_WIDTH), 0.01)
    inp['w_up_attn'] = nrm((L, NSA_WIDTH, D), NSA_WIDTH ** -0.5)
    inp['w_up_ssm'] = nrm((L, SSM_WIDTH, D), SSM_WIDTH ** -0.5)
    inp['w_out'] = nrm((L, D, D), D ** -0.5)
    inp['w_router'] = nrm((L, D, N_EXPERTS), D ** -0.5)
    inp['router_bias'] = nrm((L, N_EXPERTS), 0.01)
    inp['w_gate'] = nrm((L, N_EXPERTS, D, D_EXPERT), D ** -0.5)
    inp['w_up'] = nrm((L, N_EXPERTS, D, D_EXPERT), D ** -0.5)
    inp['w_down'] = nrm((L, N_EXPERTS, D_EXPERT, D), D_EXPERT ** -0.5)
    inp['ws_gate'] = nrm((L, D, D_EXPERT), D ** -0.5)
    inp['ws_up'] = nrm((L, D, D_EXPERT), D ** -0.5)
    inp['ws_down'] = nrm((L, D_EXPERT, D), D_EXPERT ** -0.5)
    return inp


def reference(x, c, w_ada, b_ada, g_norm1, g_norm2, w_in, q_gain, kc_gain, ks_gain, kw_gain,
              pe_k, pe_v, w_cmp_k1, w_cmp_k2, w_cmp_v1, w_cmp_v2,
              a_re, a_im, log_dt, b_re, b_im, c_re, c_im, d_skip, w_glu, b_glu,
              w_up_attn, w_up_ssm, w_out, w_router, router_bias,
              w_gate, w_up, w_down, ws_gate, ws_up, ws_down):
    layer_params = (w_ada, b_ada, g_norm1, g_norm2, w_in, q_gain, kc_gain, ks_gain, kw_gain,
                    pe_k, pe_v, w_cmp_k1, w_cmp_k2, w_cmp_v1, w_cmp_v2,
                    a_re, a_im, log_dt, b_re, b_im, c_re, c_im, d_skip, w_glu, b_glu,
                    w_up_attn, w_up_ssm, w_out, w_router, router_bias,
                    w_gate, w_up, w_down, ws_gate, ws_up, ws_down)
    for layer in range(DEPTH):
        x = hybrid_block(x, c, *[p[layer] for p in layer_params])
    return x
```

```python
import numpy as np
from contextlib import ExitStack
import concourse.bass as bass
import concourse.mybir as mybir
from concourse.bass_utils import run_bass_kernel_spmd

F32 = mybir.dt.float32
BF16 = mybir.dt.bfloat16
I32 = mybir.dt.int32
ALU = mybir.AluOpType
AF = mybir.ActivationFunctionType
AX = mybir.AxisListType

S = 4096
D = 1024
NT = 32
NO = 16
SO = 2048
CAP = 192
NEXP = 256
BIG = 30000.0
EPS = 1e-6
CH = 256
NDS = 6
PI = 3.14159265358979
DBG_NJ = 0
NPRE_S5 = 0
NPRE_ATT = 88
NPRE = NPRE_S5 + NPRE_ATT
DBG_NE = 0


class KB:
    def __init__(self, nc, es):
        self.nc = nc
        self.es = es
        self.E = {'pe': nc.tensor, 'act': nc.scalar, 'dve': nc.vector, 'pool': nc.gpsimd, 'sp': nc.sync}
        self.sem = {}
        self.cnt = {}
        for e in ['pe', 'act', 'dve', 'pool']:
            self.sem[e] = es.enter_context(nc.semaphore('sem_' + e))
            self.cnt[e] = 0
        self.dq = {}
        self.nds = {'sp': 16, 'pool': 24, 'act': 8}
        for q in ['sp', 'pool', 'act']:
            names = []
            for i in range(self.nds[q]):
                n = f'd_{q}{i}'
                self.sem[n] = es.enter_context(nc.semaphore(n))
                self.cnt[n] = 0
                names.append(n)
            self.dq[q] = names
        self.dqi = {q: 0 for q in self.dq}
        self.waited = {e: {} for e in self.E}
        self.lastw = {}
        self.readers = {}
        self.ninst = 0
        self.inputs = {}
        self.outputs = {}
        self.psb = [es.enter_context(nc.psum_tensor(f'psb{i}', [128, 512], F32)) for i in range(8)]

    def inp(self, name, shape, dt=F32):
        if name not in self.inputs:
            self.inputs[name] = self.nc.dram_tensor(name, list(shape), dt, kind="ExternalInput").ap()
        return self.inputs[name]

    def outp(self, name, shape, dt=F32):
        self.outputs[name] = self.nc.dram_tensor(name, list(shape), dt, kind="ExternalOutput").ap()
        return self.outputs[name]

    def scratch(self, name, shape, dt=F32):
        return self.nc.dram_tensor(name, list(shape), dt).ap()

    def sb(self, stack, name, shape, dt):
        self.uid = getattr(self, 'uid', 0) + 1
        return stack.enter_context(self.nc.sbuf_tensor(f'sb{self.uid}_' + name, list(shape), dt))

    def _deps(self, eng, r, w):
        deps = {}

        def need(name, val):
            if val > deps.get(name, 0):
                deps[name] = val
        for k in r:
            if k in self.lastw:
                need(*self.lastw[k])
        for k in w:
            if k in self.lastw:
                need(*self.lastw[k])
            for n, v in self.readers.get(k, {}).items():
                need(n, v)
        e = self.E[eng]
        for name, val in deps.items():
            if eng == 'pe' and name == 'pe':
                continue
            if self.waited[eng].get(name, 0) >= val:
                continue
            e.wait_ge(self.sem[name], val)
            self.waited[eng][name] = val
            self.ninst += 1

    def _record(self, sv, r, w):
        for k in w:
            self.lastw[k] = sv
            self.readers[k] = {}
        for k in r:
            d = self.readers.setdefault(k, {})
            if sv[1] > d.get(sv[0], 0):
                d[sv[0]] = sv[1]

    def op(self, eng, fn, r=(), w=()):
        self._deps(eng, r, w)
        inst = fn(self.E[eng])
        self.cnt[eng] += 1
        inst.then_inc(self.sem[eng], 1)
        self.ninst += 1
        self._record((eng, self.cnt[eng]), r, w)

    def dma(self, q, out, in_, r=(), w=(), fn=None):
        self._deps(q, r, w)
        names = self.dq[q]
        n = names[self.dqi[q] % self.nds[q]]
        self.dqi[q] += 1
        if self.cnt[n] > self.waited[q].get(n, 0):
            self.E[q].wait_ge(self.sem[n], self.cnt[n])
            self.waited[q][n] = self.cnt[n]
        if fn is None:
            inst = self.E[q].dma_start(out=out, in_=in_)
        else:
            inst = fn(self.E[q])
        self.cnt[n] += 16
        inst.then_inc(self.sem[n], 16)
        self.ninst += 1
        self._record((n, self.cnt[n]), r, w)

    def barrier(self):
        for eng in ['pe', 'act', 'dve', 'pool', 'sp']:
            e = self.E[eng]
            for name, val in self.cnt.items():
                if val > self.waited[eng].get(name, 0) and not (name == eng):
                    e.wait_ge(self.sem[name], val)
                    self.waited[eng][name] = val

    def finish(self):
        e = self.E['sp']
        for name, val in self.cnt.items():
            if val > self.waited['sp'].get(name, 0):
                e.wait_ge(self.sem[name], val)
                self.waited['sp'][name] = val

    def mm(self, out, lhsT, rhs, start, stop, r=(), w=(), sgc=False):
        self.op('pe', lambda e: e.matmul(out, lhsT=lhsT, rhs=rhs, start=start, stop=stop, skip_group_check=sgc), r=r, w=w)

    def tr(self, out, in_, ident, r=(), w=()):
        self.op('pe', lambda e: e.transpose(out=out, in_=in_, identity=ident), r=r, w=w)

    def act(self, out, in_, func, r=(), w=(), bias=None, scale=None, accum=None):
        kw = {}
        if bias is not None:
            kw['bias'] = bias
        if scale is not None:
            kw['scale'] = scale
        if accum is not None:
            kw['accum_out'] = accum
        self.op('act', lambda e: e.activation(out=out, in_=in_, func=func, **kw), r=r, w=w)

    def tt(self, eng, out, a, b, op, r=(), w=()):
        self.op(eng, lambda e: e.tensor_tensor(out=out, in0=a, in1=b, op=op), r=r, w=w)

    def ts(self, eng, out, a, s1, op0, s2=None, op1=None, r=(), w=(), accum=None):
        kw = {}
        if op1 is not None:
            kw['op1'] = op1
        if accum is not None:
            kw['accum_out'] = accum
        self.op(eng, lambda e: e.tensor_scalar(out=out, in0=a, scalar1=s1, scalar2=s2, op0=op0, **kw), r=r, w=w)

    def stt(self, out, a, s, b, op0, op1, r=(), w=(), accum=None):
        kw = {}
        if accum is not None:
            kw['accum_out'] = accum
        self.op('dve', lambda e: e.scalar_tensor_tensor(out=out, in0=a, scalar=s, in1=b, op0=op0, op1=op1, **kw), r=r, w=w)

    def cp(self, eng, out, in_, r=(), w=()):
        self.op(eng, lambda e: e.tensor_copy(out=out, in_=in_), r=r, w=w)

    def dump(self, name, ap, shape, key, dt=F32):
        o = self.outp('dbg_' + name, shape, dt)
        self.dma('sp', o, ap, r=[key])


def bc(ap, shape):
    return ap.to_broadcast(list(shape))


def build(stage='all', dbg=False):
    nc = bass.Bass("TRN2", target_bir_lowering=False)
    es = ExitStack()
    kb = KB(nc, es)
    P = es
    ps = kb.psb

    def st(x):
        order = ['p0', 'A', 'KV', 'S5', 'ATT', 'MERGE', 'MOE', 'all']
        return order.index(stage) >= order.index(x)

    ident = kb.sb(P, 'ident', [128, 128], F32)
    identb = kb.sb(P, 'identb', [128, 128], BF16)
    kb.op('pool', lambda e: e.memset(ident[:], 0.0), w=['ident'])
    kb.op('pool', lambda e: e.affine_select(out=ident[:], in_=ident[:], pattern=[[-1, 128]], compare_op=ALU.not_equal,
                                            fill=1.0, base=0, channel_multiplier=1), r=['ident'], w=['ident'])
    kb.cp('pool', identb[:], ident[:], r=['ident'], w=['identb'])
    hfm = kb.sb(P, 'hfm', [128, 2], F32)
    kb.dma('sp', hfm[:], kb.inp('hfm', [128, 2]), w=['hfm'])

    modT = kb.sb(P, 'modT', [128, 16], F32)
    a1T = kb.sb(P, 'a1T', [128, 8], F32)
    modrow_d = kb.scratch('modrow_d', [1, 4 * 1024], F32)
    a2row_d = kb.scratch('a2row_d', [1, 1024], F32)
    with ExitStack() as ph:
        modrow = kb.sb(ph, 'modrow', [128, 4, 1024], F32)
        a2row = kb.sb(ph, 'a2row', [128, 1024], F32)
        cT = kb.sb(ph, 'cT', [128, 8], F32)
        scb = kb.sb(ph, 'scb', [128, 8], BF16)
        scbb = kb.sb(ph, 'scbb', [128, 8, 128], BF16)
        badaT = kb.sb(ph, 'badaT', [128, 48], F32)
        badarow = kb.sb(ph, 'badarow', [128, 4096], F32)
        g1T = kb.sb(ph, 'g1T', [128, 8], F32)
        g2row = kb.sb(ph, 'g2row', [128, 1024], F32)
        wbf = [kb.sb(ph, f'wbf{i}', [128, 8, 512], BF16) for i in range(2)]
        kb.dma('sp', cT[:], kb.inp('cT', [128, 8]), w=['cT'])
        kb.dma('sp', badaT[:], kb.inp('b_adaT', [128, 48]), w=['badaT'])
        kb.dma('sp', badarow[:], kb.inp('b_ada_row', [1, 6144])[0:1, 2048:6144].partition_broadcast(128), w=['badarow'])
        kb.dma('sp', g1T[:], kb.inp('g1T', [128, 8]), w=['g1T'])
        kb.dma('sp', g2row[:], kb.inp('g2row', [1, 1024])[0:1, :].partition_broadcast(128), w=['g2row'])
        kb.act(scb[:], cT[:], AF.Silu, r=['cT'], w=['scb'])
        kb.cp('dve', scbb[:], bc(scb[:].unsqueeze(2), [128, 8, 128]), r=['scb'], w=['scbb'])
        w_ada = kb.inp('w_ada', [1024, 6144])
        for n in range(12):
            b = n % 2
            kb.dma('pool', wbf[b][:], w_ada[:, n * 512:(n + 1) * 512].rearrange("(kc p) n -> p kc n", p=128), w=[f'wbf{b}'])
            if n < 4:
                for ct in range(4):
                    idx = n * 4 + ct
                    for kc in range(8):
                        kb.mm(ps[7][:, idx:idx + 1], wbf[b][:, kc, ct * 128:(ct + 1) * 128], scb[:, kc:kc + 1],
                              kc == 0, kc == 7, r=[f'wbf{b}', 'scb'], w=['ps7'])
                if n == 3:
                    kb.tt('dve', modT[:], ps[7][:, 0:16], badaT[:, 0:16], ALU.add, r=['ps7', 'badaT'], w=['modT'])
            else:
                pb = n % 2
                for kc in range(8):
                    kb.mm(ps[pb][:, :], scbb[:, kc, :], wbf[b][:, kc, :], kc == 0, kc == 7, r=[f'wbf{b}', 'scbb'], w=[f'ps{pb}'])
                sec = n // 2 - 2
                half = n % 2
                kb.tt('dve', modrow[:, sec, half * 512:(half + 1) * 512], ps[pb][:, :], badarow[:, (n - 4) * 512:(n - 3) * 512],
                      ALU.add, r=[f'ps{pb}', 'badarow'], w=['modrow'])
        kb.stt(a1T[:], modT[:, 8:16], 1.0, g1T[:], ALU.add, ALU.mult, r=['modT', 'g1T'], w=['a1T'])
        kb.stt(a2row[:], modrow[:, 2, :], 1.0, g2row[:], ALU.add, ALU.mult, r=['modrow', 'g2row'], w=['a2row'])
        kb.dma('sp', modrow_d[0:1, :], modrow[0:1, :, :].rearrange("p a b -> p (a b)"), r=['modrow'], w=['modrow_d'])
        kb.dma('sp', a2row_d[0:1, :], a2row[0:1, :], r=['a2row'], w=['a2row_d'])
        if dbg and stage == 'p0':
            kb.dump('modT', modT[:], [128, 16], 'modT')
            kb.dump('modrow', modrow[:], [128, 4, 1024], 'modrow')
            kb.dump('a1T', a1T[:], [128, 8], 'a1T')
        kb.barrier()
    if not st('A'):
        kb.finish()
        es.close()
        return nc, kb

    def run_interleaved(gens, width):
        active = []
        gens = list(gens)
        gi = 0
        while gi < len(gens) or active:
            while len(active) < width and gi < len(gens):
                active.append(gens[gi])
                gi += 1
            for g in list(active):
                try:
                    next(g)
                except StopIteration:
                    active.remove(g)

    def norm_transpose(xd, ntiles, hT, hkey, ph):
        xt = [kb.sb(ph, f'nt_x{i}', [128, 1024], F32) for i in range(2)]
        junk = [kb.sb(ph, f'nt_junk{i}', [128, 1024], BF16) for i in range(2)]
        ss = [kb.sb(ph, f'nt_ss{i}', [128, 4], F32) for i in range(2)]

        def tile_gen(t):
            b = t % 2
            B = str(b)
            kb.dma('sp', xt[b][:], xd[t * 128:(t + 1) * 128, :], w=['nt_x' + B])
            kb.act(junk[b][:], xt[b][:], AF.Square, r=['nt_x' + B], w=['nt_junk' + B, 'nt_ss0' + B], accum=ss[b][:, 0:1])
            yield
            kb.act(ss[b][:, 1:2], ss[b][:, 0:1], AF.Sqrt, r=['nt_ss0' + B], w=['nt_ss1' + B], scale=1.0 / D, bias=EPS)
            yield
            kb.op('dve', lambda e: e.reciprocal(out=ss[b][:, 2:3], in_=ss[b][:, 1:2]), r=['nt_ss1' + B], w=['nt_ss2' + B])
            yield
            kb.ts('dve', xt[b][:], xt[b][:], ss[b][:, 2:3], ALU.mult, r=['nt_x' + B, 'nt_ss2' + B], w=['nt_x' + B])
            yield
            for half in range(2):
                pb = 4 + 2 * b + half
                for q in range(4):
                    kc = half * 4 + q
                    kb.tr(ps[pb][:, q * 128:(q + 1) * 128], xt[b][:, kc * 128:(kc + 1) * 128], ident[:],
                          r=['nt_x' + B, 'ident'], w=[f'ps{pb}'])
                yield
                for q in range(4):
                    kc = half * 4 + q
                    kb.act(hT[:, kc, t * 128:(t + 1) * 128], ps[pb][:, q * 128:(q + 1) * 128], AF.Identity,
                           r=[f'ps{pb}', 'a1T', 'modT'], w=[f'{hkey}_{t}_{kc}'], scale=a1T[:, kc:kc + 1], bias=modT[:, kc:kc + 1])
                yield
        for t0_ in range(0, ntiles, 2):
            run_interleaved([tile_gen(t0_), tile_gen(t0_ + 1)], 2)

    def load_w(dst, dkey, src_ap):
        kb.dma('pool', dst, src_ap.rearrange("(kc p) n -> p kc n", p=128), w=[dkey])

    w_in = kb.inp('w_in', [1024, 3864])
    OQ, OKC, OVC, OKS, OVS, OKW, OVW, OGN, OU, OGA, OGS = 0, 512, 640, 768, 896, 1024, 1152, 1280, 1304, 1816, 2840

    pre_on = st('MOE') and NPRE > 0
    if pre_on:
        w_gate_d = kb.inp('w_gate', [NEXP, 1024, 256])
        w_up_d = kb.inp('w_up', [NEXP, 1024, 256])
        w_down_d = kb.inp('w_down', [NEXP, 256, 1024])
        wpre = [kb.scratch(f'wpre{t}', [NPRE, 128, 2048], BF16) for t in range(3)]
    else:
        wpre = None

    def make_pre_gen(stack, tag, e0, ne, depth, cast_eng, pcols):
        pst = [kb.sb(stack, f'pst{tag}{t}', [128, pcols], F32) for t in range(depth)]
        pbf = [kb.sb(stack, f'pbf{tag}{t}', [128, pcols], BF16) for t in range(depth)]
        ppw = 2048 // pcols
        ppe = 3 * ppw

        def pre_src(n):
            e = e0 + n // ppe
            t = n % ppe
            w, hh = t // ppw, t % ppw
            c0 = hh * pcols
            if w == 0:
                return w_gate_d[e].rearrange("(p r) n -> p (r n)", r=8)[:, c0:c0 + pcols]
            if w == 1:
                return w_up_d[e].rearrange("(p r) n -> p (r n)", r=8)[:, c0:c0 + pcols]
            kc, cc = c0 // 1024, c0 % 1024
            return w_down_d[e][kc * 128:(kc + 1) * 128, cc:cc + pcols]

        def pre_load(n):
            s_ = n % depth
            kb.dma('sp', pst[s_][:], pre_src(n), w=[f'pst{tag}{s_}'])

        def gen():
            npc = ppe * ne
            for n in range(min(depth, npc)):
                pre_load(n)
            yield
            for n in range(npc):
                s_ = n % depth
                e = e0 + n // ppe
                t = n % ppe
                w, hh = t // ppw, t % ppw
                if cast_eng == 'act':
                    kb.act(pbf[s_][:], pst[s_][:], AF.Copy, r=[f'pst{tag}{s_}'], w=[f'pbf{tag}{s_}'])
                else:
                    kb.cp(cast_eng, pbf[s_][:], pst[s_][:], r=[f'pst{tag}{s_}'], w=[f'pbf{tag}{s_}'])
                kb.dma('sp', wpre[w][e][:, hh * pcols:(hh + 1) * pcols], pbf[s_][:], r=[f'pbf{tag}{s_}'], w=[])
                if n + depth < npc:
                    pre_load(n + depth)
                yield
        return gen()

    def finish_now():
        kb.barrier()
        kb.finish()
        es.close()
        return nc, kb

    y_ssmT = kb.sb(P, 'y_ssmT', [128, 4, SO], BF16)
    onesblk = kb.sb(P, 'onesblk', [128, 128], BF16)
    kb.op('pool', lambda e: e.memset(onesblk[:], 0.0), w=['onesblk'])
    kb.op('pool', lambda e: e.memset(onesblk[0:64, 0:64], 1.0), r=['onesblk'], w=['onesblk'])
    kb.op('pool', lambda e: e.memset(onesblk[64:128, 64:128], 1.0), r=['onesblk'], w=['onesblk'])
    gainT = kb.sb(P, 'gainT', [128, 4], F32)
    kb.dma('sp', gainT[:], kb.inp('gainT', [128, 4]), w=['gainT'])

    attK = ExitStack()
    ksT = kb.sb(attK, 'ksT', [128, S], BF16)
    kwT = kb.sb(attK, 'kwT', [128, S], BF16)
    VS = kb.sb(attK, 'VS', [128, NT, 2, 65], BF16)
    VW = kb.sb(attK, 'VW', [128, NT, 2, 65], BF16)
    kcn = kb.sb(attK, 'kcn', [128, 256], BF16)
    VOX = kb.sb(attK, 'VOX', [128, 2, 2, 129], BF16)
    phU = ExitStack()
    uT = kb.sb(phU, 'uT', [128, 4, S], BF16)

    def fm_norm(psrc, pskey, n, dst, dkey, gcol, tmp, sc_scale, sc_bias, pbn):
        kb.act(tmp['raw'][:, 0:n], psrc, AF.Copy, r=[pskey], w=['fn_raw'])
        kb.act(tmp['sq'][:, 0:n], psrc, AF.Square, r=[pskey], w=['fn_sq'])
        kb.mm(ps[pbn][:, 0:n], onesblk[:, :], tmp['sq'][:, 0:n], True, True, r=['onesblk', 'fn_sq'], w=[f'ps{pbn}'])
        kb.act(tmp['rs'][:, 0:n], ps[pbn][:, 0:n], AF.Sqrt, r=[f'ps{pbn}'], w=['fn_rs'], scale=sc_scale, bias=sc_bias)
        kb.op('dve', lambda e: e.reciprocal(out=tmp['rs'][:, 0:n], in_=tmp['rs'][:, 0:n]), r=['fn_rs'], w=['fn_rs'])
        kb.stt(dst, tmp['raw'][:, 0:n], gainT[:, gcol:gcol + 1], tmp['rs'][:, 0:n], ALU.mult, ALU.mult,
               r=['fn_raw', 'fn_rs', 'gainT'], w=[dkey])

    with ExitStack() as phA:
        hT = kb.sb(phA, 'hT', [128, 8, S], BF16)
        with ExitStack() as ph:
            norm_transpose(kb.inp('xf', [S, D]), NT, hT, 'hT', ph)
            kb.barrier()
        if dbg and stage == 'A':
            kb.dump('hT', hT[:], [128, 8, S], 'hT', BF16)
        if not st('KV'):
            return finish_now()
        with ExitStack() as ph:
            wb_ = kb.sb(ph, 'kv_wb', [128, 8, 512], BF16)
            tmp = {'raw': kb.sb(ph, 'fn_raw', [128, 512], F32), 'sq': kb.sb(ph, 'fn_sq', [128, 512], BF16),
                   'rs': kb.sb(ph, 'fn_rs', [128, 512], F32)}
            load_w(wb_[:], 'kv_wb', w_in[:, OU:OU + 512])
            for mt in range(4):
                for c in range(8):
                    pb = c % 2
                    for kc in range(8):
                        kb.mm(ps[pb][:, :], wb_[:, kc, mt * 128:(mt + 1) * 128], hT[:, kc, c * 512:(c + 1) * 512], kc == 0, kc == 7,
                              r=['kv_wb', 'hT'], w=[f'ps{pb}'])
                    kb.act(uT[:, mt, c * 512:(c + 1) * 512], ps[pb][:, :], AF.Copy, r=[f'ps{pb}'], w=['uT'])
            load_w(wb_[:], 'kv_wb', w_in[:, OKS:OKS + 512])
            for (off, dst, dkey, gcol) in ((0, ksT, 'ksT', 2), (256, kwT, 'kwT', 3)):
                for c in range(8):
                    pb = c % 2
                    for kc in range(8):
                        kb.mm(ps[pb][:, :], wb_[:, kc, off:off + 128], hT[:, kc, c * 512:(c + 1) * 512], kc == 0, kc == 7,
                              r=['kv_wb', 'hT'], w=[f'ps{pb}'])
                    fm_norm(ps[pb][:, :], f'ps{pb}', 512, dst[:, c * 512:(c + 1) * 512], dkey, gcol, tmp, 1.0 / 64, EPS, 2 + pb)
            kb.op('pool', lambda e: e.memset(VS[:, :, :, 64:65], 1.0), w=['VS'])
            kb.op('pool', lambda e: e.memset(VW[:, :, :, 64:65], 1.0), w=['VW'])
            for t in range(NT):
                pb = t % 2
                for (off, dst, dkey, col0) in ((128, VS, 'VS', 0), (384, VW, 'VW', 128)):
                    for kc in range(8):
                        kb.mm(ps[pb][:, col0:col0 + 128], hT[:, kc, t * 128:(t + 1) * 128], wb_[:, kc, off:off + 128], kc == 0, kc == 7,
                              r=['kv_wb', 'hT'], w=[f'ps{pb}'], sgc=(col0 > 0))
                kb.act(VS[:, t, :, 0:64], ps[pb][:, 0:128].rearrange("p (g d) -> p g d", g=2), AF.Copy, r=[f'ps{pb}'], w=['VS'])
                kb.act(VW[:, t, :, 0:64], ps[pb][:, 128:256].rearrange("p (g d) -> p g d", g=2), AF.Copy, r=[f'ps{pb}'], w=['VW'])
            kcS = kb.sb(ph, 'kcS', [128, S], BF16)
            w2b = kb.sb(ph, 'w2b', [128, 2, 64], BF16)
            peS = kb.sb(ph, 'peS', [128, 16], F32)
            peb = kb.sb(ph, 'peb', [128, 16], BF16)
            hb = kb.sb(ph, 'hb', [128, 2], F32)
            hid = kb.sb(ph, 'hid', [128, 2, 256], BF16)
            wkc = kb.sb(ph, 'wkc', [128, 8, 256], BF16)
            load_w(wkc[:], 'wkc', w_in[:, OKC:OKC + 256])
            kb.op('pool', lambda e: e.memset(kcn[:], 0.0), w=['kcn'])
            kb.op('pool', lambda e: e.memset(VOX[:], 0.0), w=['VOX'])
            ovl = kb.sb(ph, 'ovl', [128, 2, 64], F32)
            kb.dma('sp', ovl[:], kb.inp('ovl', [128, 2, 64]), w=['ovl'])
            for g in range(2):
                kb.cp('pool', VOX[:, :, g, 65:129], ovl[:], r=['ovl', 'VOX'], w=['VOX'])
            kb.op('pool', lambda e: e.memset(VOX[:, 0, :, 64:65], 1.0), r=['VOX'], w=['VOX'])
            kb.op('pool', lambda e: e.memset(VOX[0:127, 1, :, 64:65], 1.0), r=['VOX'], w=['VOX'])
            wb16 = wb_[:].rearrange("p a (b c) -> p (a b) c", b=2)
            for kv in range(2):
                nm = ['k', 'v'][kv]
                kb.dma('pool', wb16, kb.inp(f'w_cmp_{nm}1', [2048, 256]).rearrange("(lp p) n -> p lp n", p=128), w=['kv_wb'])
                kb.dma('pool', w2b[:], kb.inp(f'w_cmp_{nm}2', [256, 64]).rearrange("(m p) n -> p m n", p=128), w=['w2b'])
                kb.dma('sp', peS[:], kb.inp(f'peS_{nm}', [128, 16]), w=['peS'])
                kb.cp('pool', peb[:], peS[:], r=['peS'], w=['peb'])
                for m in range(2):
                    for lp in range(16):
                        kb.mm(ps[6][:, m:m + 1], wb16[:, lp, m * 128:(m + 1) * 128], peb[:, lp:lp + 1], lp == 0, lp == 15,
                              r=['kv_wb', 'peb'], w=['ps6'])
                kb.cp('dve', hb[:], ps[6][:, 0:2], r=['ps6'], w=['hb'])
                for g in range(2):
                    wcol = kv * 128 + g * 64
                    kb.op('pool', lambda e: e.memset(kcS[64:128, S - 1:S], 0.0), w=['kcS'])
                    for c in range(8):
                        pb = c % 2
                        n2 = 512 if c < 7 else 511
                        for kc in range(8):
                            kb.mm(ps[pb][0:64, :], wkc[:, kc, wcol:wcol + 64], hT[:, kc, c * 512:(c + 1) * 512], kc == 0, kc == 7,
                                  r=['wkc', 'hT'], w=[f'ps{pb}'])
                        for kc in range(8):
                            kb.mm(ps[pb][64:128, 0:n2], wkc[:, kc, wcol:wcol + 64], hT[:, kc, c * 512 + 1:c * 512 + 1 + n2], kc == 0, kc == 7,
                                  r=['wkc', 'hT'], w=[f'ps{pb}'])
                        kb.act(kcS[0:64, c * 512:(c + 1) * 512], ps[pb][0:64, :], AF.Copy, r=[f'ps{pb}'], w=['kcS'])
                        kb.act(kcS[64:128, c * 512:c * 512 + n2], ps[pb][64:128, 0:n2], AF.Copy, r=[f'ps{pb}'], w=['kcS'])
                    for m in range(2):
                        pb = 2 + m
                        for lp in range(16):
                            kb.mm(ps[pb][:, 0:255], wb16[:, lp, m * 128:(m + 1) * 128], kcS[:, 2 * lp:2 * lp + 16 * 254 + 1:16], lp == 0, lp == 15,
                                  r=['kv_wb', 'kcS'], w=[f'ps{pb}'])
                        kb.act(hid[:, m, 0:255], ps[pb][:, 0:255], AF.Gelu_apprx_tanh, r=[f'ps{pb}', 'hb'], w=['hid'], bias=hb[:, m:m + 1])
                    if kv == 0:
                        for m in range(2):
                            kb.mm(ps[4][g * 64:(g + 1) * 64, 0:255], w2b[:, m, :], hid[:, m, 0:255], m == 0, m == 1, r=['w2b', 'hid'], w=['ps4'])
                    else:
                        for bt in range(2):
                            nb = 128 if bt == 0 else 127
                            for m in range(2):
                                kb.mm(ps[4][0:nb, bt * 64:(bt + 1) * 64], hid[:, m, bt * 128:bt * 128 + nb], w2b[:, m, :], m == 0, m == 1,
                                      r=['w2b', 'hid'], w=['ps4'], sgc=(bt == 1))
                        kb.act(VOX[:, 0, g, 0:64], ps[4][:, 0:64], AF.Copy, r=['ps4'], w=['VOX'])
                        kb.act(VOX[0:127, 1, g, 0:64], ps[4][0:127, 64:128], AF.Copy, r=['ps4'], w=['VOX'])
                if kv == 0:
                    fm_norm(ps[4][:, 0:255], 'ps4', 255, kcn[:, 0:255], 'kcn', 1, tmp, 1.0 / 64, EPS, 5)
            if dbg and stage == 'KV':
                kb.dump('uT', uT[:], [128, 4, S], 'uT', BF16)
                kb.dump('ksT', ksT[:], [128, S], 'ksT', BF16)
                kb.dump('kwT', kwT[:], [128, S], 'kwT', BF16)
                kb.dump('VS', VS[:], [128, NT, 2, 65], 'VS', BF16)
                kb.dump('kcn', kcn[:], [128, 256], 'kcn', BF16)
                kb.dump('VOX', VOX[:], [128, 2, 2, 129], 'VOX', BF16)
            kb.barrier()
    if not st('S5'):
        return finish_now()
    with ExitStack() as ph:
        prm = kb.sb(ph, 's5_prm', [128, 3, 16], F32)
        kb.dma('sp', prm[:], kb.inp('s5_prm', [128, 3, 16]), w=['prm'])
        are, aim, ldt = prm[:, 0, :], prm[:, 1, :], prm[:, 2, :]
        sc = kb.sb(ph, 's5_sc', [128, 24, 16], F32)
        DT, RHO, TH, C0, S0, T1, T2, T3, FRE, FIM, ERE, EIM, NR, NI, DEN, MFIM = range(16)
        K = 's5sc'
        kb.act(sc[:, DT, :], ldt, AF.Exp, r=['prm'], w=[K])
        kb.tt('dve', sc[:, T1, :], are, sc[:, DT, :], ALU.mult, r=['prm', K], w=[K])
        kb.act(sc[:, RHO, :], sc[:, T1, :], AF.Exp, r=[K], w=[K])
        kb.tt('dve', sc[:, TH, :], aim, sc[:, DT, :], ALU.mult, r=['prm', K], w=[K])
        hpi = kb.sb(ph, 's5_hpi', [128, 1], F32)
        kb.op('dve', lambda e: e.memset(hpi[:], PI / 2), w=['hpi'])
        kb.act(sc[:, S0, :], sc[:, TH, :], AF.Sin, r=[K], w=[K], scale=1.0 / 16)
        kb.act(sc[:, C0, :], sc[:, TH, :], AF.Sin, r=[K, 'hpi'], w=[K], scale=1.0 / 16, bias=hpi[:, 0:1])

        def csq(cre, cim):
            kb.tt('dve', sc[:, T1, :], sc[:, cre, :], sc[:, cre, :], ALU.mult, r=[K], w=[K])
            kb.tt('dve', sc[:, T2, :], sc[:, cim, :], sc[:, cim, :], ALU.mult, r=[K], w=[K])
            kb.tt('dve', sc[:, T3, :], sc[:, cre, :], sc[:, cim, :], ALU.mult, r=[K], w=[K])
            kb.tt('dve', sc[:, cre, :], sc[:, T1, :], sc[:, T2, :], ALU.subtract, r=[K], w=[K])
            kb.ts('dve', sc[:, cim, :], sc[:, T3, :], 2.0, ALU.mult, r=[K], w=[K])
        for _ in range(4):
            csq(C0, S0)
        kb.tt('dve', sc[:, NR, :], sc[:, RHO, :], sc[:, C0, :], ALU.mult, r=[K], w=[K])
        kb.ts('dve', sc[:, NR, :], sc[:, NR, :], -1.0, ALU.add, r=[K], w=[K])
        kb.tt('dve', sc[:, NI, :], sc[:, RHO, :], sc[:, S0, :], ALU.mult, r=[K], w=[K])
        kb.tt('dve', sc[:, T1, :], are, are, ALU.mult, r=['prm'], w=[K])
        kb.tt('dve', sc[:, T2, :], aim, aim, ALU.mult, r=['prm'], w=[K])
        kb.tt('dve', sc[:, DEN, :], sc[:, T1, :], sc[:, T2, :], ALU.add, r=[K], w=[K])
        kb.op('dve', lambda e: e.reciprocal(out=sc[:, DEN, :], in_=sc[:, DEN, :]), r=[K], w=[K])
        kb.tt('dve', sc[:, T1, :], sc[:, NR, :], are, ALU.mult, r=[K, 'prm'], w=[K])
        kb.tt('dve', sc[:, T2, :], sc[:, NI, :], aim, ALU.mult, r=[K, 'prm'], w=[K])
        kb.tt('dve', sc[:, T1, :], sc[:, T1, :], sc[:, T2, :], ALU.add, r=[K], w=[K])
        kb.tt('dve', sc[:, FRE, :], sc[:, T1, :], sc[:, DEN, :], ALU.mult, r=[K], w=[K])
        kb.tt('dve', sc[:, T1, :], sc[:, NI, :], are, ALU.mult, r=[K, 'prm'], w=[K])
        kb.tt('dve', sc[:, T2, :], sc[:, NR, :], aim, ALU.mult, r=[K, 'prm'], w=[K])
        kb.tt('dve', sc[:, T1, :], sc[:, T1, :], sc[:, T2, :], ALU.subtract, r=[K], w=[K])
        kb.tt('dve', sc[:, FIM, :], sc[:, T1, :], sc[:, DEN, :], ALU.mult, r=[K], w=[K])
        kb.ts('dve', sc[:, MFIM, :], sc[:, FIM, :], -1.0, ALU.mult, r=[K], w=[K])
        cs3 = kb.sb(ph, 's5_cs3', [128, 16, 3 * CH], BF16)
        cpo = kb.sb(ph, 's5_cpo', [128, 16, 128], BF16)
        spo = kb.sb(ph, 's5_spo', [128, 16, 128], BF16)
        Bbf = kb.sb(ph, 's5_Bbf', [128, 2, 16, 128], BF16)
        CA = kb.sb(ph, 's5_CA', [128, 16, 128], BF16)
        CBm = kb.sb(ph, 's5_CB', [128, 16, 128], BF16)
        with ExitStack() as ph2:
            cpF = kb.sb(ph2, 's5_cpF', [128, 16, CH], F32)
            spF = kb.sb(ph2, 's5_spF', [128, 16, CH], F32)
            tA = kb.sb(ph2, 's5_tA', [128, 16, CH // 2], F32)
            tB = kb.sb(ph2, 's5_tB', [128, 16, CH // 2], F32)
            kb.op('pool', lambda e: e.memset(cpF[:, :, 0:1], 1.0), w=['cpF'])
            kb.op('pool', lambda e: e.memset(spF[:, :, 0:1], 0.0), w=['spF'])
            kb.cp('dve', sc[:, ERE, :], sc[:, C0, :], r=[K], w=[K])
            kb.cp('dve', sc[:, EIM, :], sc[:, S0, :], r=[K], w=[K])
            k = 1
            while k < CH:
                eb_re = bc(sc[:, ERE, :].unsqueeze(2), [128, 16, k])
                eb_im = bc(sc[:, EIM, :].unsqueeze(2), [128, 16, k])
                kb.tt('dve', tA[:, :, 0:k], cpF[:, :, 0:k], eb_re, ALU.mult, r=['cpF', K], w=['tA'])
                kb.tt('dve', tB[:, :, 0:k], spF[:, :, 0:k], eb_im, ALU.mult, r=['spF', K], w=['tB'])
                kb.tt('dve', cpF[:, :, k:2 * k], tA[:, :, 0:k], tB[:, :, 0:k], ALU.subtract, r=['tA', 'tB'], w=['cpF'])
                kb.tt('dve', tA[:, :, 0:k], spF[:, :, 0:k], eb_re, ALU.mult, r=['spF', K], w=['tA'])
                kb.tt('dve', tB[:, :, 0:k], cpF[:, :, 0:k], eb_im, ALU.mult, r=['cpF', K], w=['tB'])
                kb.tt('dve', spF[:, :, k:2 * k], tA[:, :, 0:k], tB[:, :, 0:k], ALU.add, r=['tA', 'tB'], w=['spF'])
                csq(ERE, EIM)
                k *= 2
            kb.cp('dve', cs3[:, :, 0:CH], cpF[:], r=['cpF'], w=['cs3a'])
            kb.act(cs3[:, :, CH:2 * CH], spF[:], AF.Copy, r=['spF'], w=['cs3b'])
            kb.cp('dve', cs3[:, :, 2 * CH:3 * CH], cpF[:], r=['cpF'], w=['cs3c'])
            for (dst, src, kd, ks_) in ((cpo, cpF, 'cpo', 'cpF'), (spo, spF, 'spo', 'spF')):
                kb.ts('dve', tA[:], src[:, :, 0:128], hfm[:, 0:1], ALU.mult, r=[ks_, 'hfm'], w=['tA'])
                kb.stt(dst[:], src[:, :, 128:256], hfm[:, 1:2], tA[:], ALU.mult, ALU.add, r=[ks_, 'hfm', 'tA'], w=[kd])
            kb.barrier()
        with ExitStack() as ph2:
            Bst = kb.sb(ph2, 's5_Bst', [128, 2, 16, 128], F32)
            kb.dma('sp', Bst[:], kb.inp('s5_B', [128, 2, 16, 128]), w=['Bst'])
            kb.cp('pool', Bbf[:], Bst[:], r=['Bst'], w=['Bbf'])
            Cst = kb.sb(ph2, 's5_Cst', [128, 2, 16, 128], F32)
            kb.dma('sp', Cst[:], kb.inp('s5_C', [128, 2, 16, 128]), w=['Cst'])
            tC = kb.sb(ph2, 's5_tC', [128, 16, 128], F32)
            tD = kb.sb(ph2, 's5_tD', [128, 16, 128], F32)
            fre_b = bc(sc[:, FRE, :].unsqueeze(2), [128, 16, 128])
            fim_b = bc(sc[:, FIM, :].unsqueeze(2), [128, 16, 128])
            mfim_b = bc(sc[:, MFIM, :].unsqueeze(2), [128, 16, 128])
            kb.tt('dve', tC[:], Cst[:, 0, :, :], fre_b, ALU.mult, r=['Cst', K], w=['tC'])
            kb.tt('dve', tD[:], Cst[:, 1, :, :], fim_b, ALU.mult, r=['Cst', K], w=['tD'])
            kb.tt('dve', CA[:], tC[:], tD[:], ALU.subtract, r=['tC', 'tD'], w=['CA'])
            kb.tt('dve', tC[:], Cst[:, 0, :, :], mfim_b, ALU.mult, r=['Cst', K], w=['tC'])
            kb.tt('dve', tD[:], Cst[:, 1, :, :], fre_b, ALU.mult, r=['Cst', K], w=['tD'])
            kb.tt('dve', CBm[:], tC[:], tD[:], ALU.subtract, r=['tC', 'tD'], w=['CB'])
            kb.barrier()
        dsk = kb.sb(ph, 's5_dsk', [128, 4], F32)
        kb.dma('sp', dsk[:], kb.inp('dskipT', [128, 4]), w=['dsk'])
        zT = kb.sb(ph, 's5_zT', [128, 4, SO], BF16)
        ph3 = ExitStack()
        init = kb.sb(ph3, 's5_init', [128, 2, 16], F32)
        kb.op('pool', lambda e: e.memset(init[:], 0.0), w=['init'])
        NA = 3
        tA4 = [kb.sb(ph3, f's5_tA4{i}', [128, 2, 2 * CH], BF16) for i in range(NA)]
        bub = [kb.sb(ph3, f's5_bub{i}', [128, 2 * CH], BF16) for i in range(NA)]
        dre = [kb.sb(ph3, f's5_dre{i}', [128, CH], F32) for i in range(3)]
        dim_ = [kb.sb(ph3, f's5_dim{i}', [128, CH], F32) for i in range(3)]
        w2 = [kb.sb(ph3, f's5_w2{i}', [128, 2, CH], F32) for i in range(2)]
        tmpb = [kb.sb(ph3, f's5_tmpb{i}', [128, 2, 128], F32) for i in range(2)]
        wlast = kb.sb(ph3, 's5_wlast', [128, 2, 16], F32)
        woall = kb.sb(ph3, 's5_woall', [128, 2, 16, 128], BF16)
        rt = [kb.sb(ph3, f's5_rt{i}', [128, 4, 128], BF16) for i in range(8)]
        ct = kb.sb(ph3, 's5_ct', [128, 4, 16], F32)
        zre = kb.sb(ph3, 's5_zre', [128, 16, 128], BF16)
        zim = kb.sb(ph3, 's5_zim', [128, 16, 128], BF16)
        uo = kb.sb(ph3, 's5_uo', [128, 4, 128], F32)
        ytmp = kb.sb(ph3, 's5_ytmp', [128, 128], F32)
        if pre_on and NPRE_S5 > 0:
            pgen5 = make_pre_gen(ph3, 's', 0, NPRE_S5, 3, 'act', 512)
        else:
            pgen5 = iter(())
        its = [(c, pt) for c in range(S // CH) for pt in range(16)]
        nit = len(its)

        def stA(i):
            c, pt = its[i]
            t0 = c * CH
            pb = i % 3
            a = i % NA
            kt = pt // 4
            kb.mm(ps[pb][:, 0:CH], Bbf[:, 0, pt, :], uT[:, kt, t0:t0 + CH], True, False, r=['Bbf', 'uT'], w=[f'ps{pb}'])
            kb.mm(ps[pb][:, CH:2 * CH], Bbf[:, 1, pt, :], uT[:, kt, t0:t0 + CH], False, True, r=['Bbf', 'uT'], w=[f'ps{pb}'], sgc=True)
            kb.act(bub[a][:], ps[pb][:, :], AF.Copy, r=[f'ps{pb}'], w=[f'bub{a}'])
            kb.tt('dve', tA4[a][:, 0, :], bub[a][:], cs3[:, pt, 0:2 * CH], ALU.mult, r=[f'bub{a}', 'cs3a', 'cs3b'], w=[f'tA4{a}'])
            kb.tt('dve', tA4[a][:, 1, :], bub[a][:], cs3[:, pt, CH:3 * CH], ALU.mult, r=[f'bub{a}', 'cs3b', 'cs3c'], w=[f'tA4{a}'])

        def stB(i):
            a = i % NA
            d = i % 3
            kb.tt('pool', dre[d][:], tA4[a][:, 0, 0:CH], tA4[a][:, 0, CH:2 * CH], ALU.add, r=[f'tA4{a}'], w=[f'dre{d}'])
            kb.tt('pool', dim_[d][:], tA4[a][:, 1, CH:2 * CH], tA4[a][:, 1, 0:CH], ALU.subtract, r=[f'tA4{a}'], w=[f'dim{d}'])

        def stC1(i):
            c, pt = its[i]
            d = i % 3
            b = i % 2
            rho_b = bc(sc[:, RHO, pt:pt + 1], [128, CH])
            kb.op('dve', lambda e: e.tensor_tensor_scan(out=w2[b][:, 0, :], data0=rho_b, data1=dre[d][:], initial=init[:, 0, pt:pt + 1],
                                                        op0=ALU.mult, op1=ALU.add), r=[f'dre{d}', K, 'init'], w=[f'w2{b}'])
            kb.op('dve', lambda e: e.tensor_tensor_scan(out=w2[b][:, 1, :], data0=rho_b, data1=dim_[d][:], initial=init[:, 1, pt:pt + 1],
                                                        op0=ALU.mult, op1=ALU.add), r=[f'dim{d}', K, 'init'], w=[f'w2{b}'])
            kb.act(tmpb[b][:], w2[b][:, :, 0:128], AF.Copy, r=[f'w2{b}', 'hfm'], w=[f'tmpb{b}'], scale=hfm[:, 0:1])
            kb.act(wlast[:, :, pt], w2[b][:, :, CH - 1], AF.Copy, r=[f'w2{b}'], w=['wlast'])

        def stC2(i):
            c, pt = its[i]
            b = i % 2
            kb.stt(woall[:, :, pt, :], w2[b][:, :, 128:256], hfm[:, 1:2], tmpb[b][:], ALU.mult, ALU.add,
                   r=[f'w2{b}', 'hfm', f'tmpb{b}'], w=['woall'])

        def chunk_end(c):
            t0 = c * CH
            kb.tt('dve', ct[:, 0, :], wlast[:, 0, :], sc[:, ERE, :], ALU.mult, r=['wlast', K], w=['ct'])
            kb.tt('dve', ct[:, 1, :], wlast[:, 1, :], sc[:, EIM, :], ALU.mult, r=['wlast', K], w=['ct'])
            kb.tt('dve', ct[:, 2, :], wlast[:, 0, :], sc[:, EIM, :], ALU.mult, r=['wlast', K], w=['ct'])
            kb.tt('dve', ct[:, 3, :], wlast[:, 1, :], sc[:, ERE, :], ALU.mult, r=['wlast', K], w=['ct'])
            kb.tt('dve', init[:, 0, :], ct[:, 0, :], ct[:, 1, :], ALU.subtract, r=['ct'], w=['init'])
            kb.tt('dve', init[:, 1, :], ct[:, 2, :], ct[:, 3, :], ALU.add, r=['ct'], w=['init'])
            for q4 in range(4):
                psl = slice(q4 * 4, q4 * 4 + 4)
                r0, r1, r2, r3 = [(q4 % 2) * 4 + k_ for k_ in range(4)]
                kb.tt('dve', rt[r0][:], woall[:, 0, psl, :], cpo[:, psl, :], ALU.mult, r=['woall', 'cpo'], w=[f'rt{r0}'])
                kb.tt('dve', rt[r1][:], woall[:, 1, psl, :], spo[:, psl, :], ALU.mult, r=['woall', 'spo'], w=[f'rt{r1}'])
                kb.tt('pool', zre[:, psl, :], rt[r0][:], rt[r1][:], ALU.subtract, r=[f'rt{r0}', f'rt{r1}'], w=['zre'])
                kb.tt('dve', rt[r2][:], woall[:, 0, psl, :], spo[:, psl, :], ALU.mult, r=['woall', 'spo'], w=[f'rt{r2}'])
                kb.tt('dve', rt[r3][:], woall[:, 1, psl, :], cpo[:, psl, :], ALU.mult, r=['woall', 'cpo'], w=[f'rt{r3}'])
                kb.tt('pool', zim[:, psl, :], rt[r2][:], rt[r3][:], ALU.add, r=[f'rt{r2}', f'rt{r3}'], w=['zim'])
            kb.act(uo[:], uT[:, :, t0:t0 + 128], AF.Copy, r=['uT', 'hfm'], w=['uo'], scale=hfm[:, 0:1])
            kb.stt(uo[:], uT[:, :, t0 + 128:t0 + 256], hfm[:, 1:2], uo[:], ALU.mult, ALU.add, r=['uT', 'hfm', 'uo'], w=['uo'])
            for mt in range(4):
                pb = 3 + mt % 2
                for q in range(4):
                    pt = mt * 4 + q
                    kb.mm(ps[pb][:, 0:128], CA[:, pt, :], zre[:, pt, :], q == 0, False, r=['CA', 'zre'], w=[f'ps{pb}'])
                    kb.mm(ps[pb][:, 0:128], CBm[:, pt, :], zim[:, pt, :], False, q == 3, r=['CB', 'zim'], w=[f'ps{pb}'])
                kb.stt(ytmp[:], uo[:, mt, :], dsk[:, mt:mt + 1], ps[pb][:, 0:128], ALU.mult, ALU.add, r=['uo', 'dsk', f'ps{pb}'], w=['ytmp'])
                kb.act(zT[:, mt, c * 128:(c + 1) * 128], ytmp[:], AF.Gelu_apprx_tanh, r=['ytmp'], w=['zT'])

        stA(0)
        stA(1)
        stA(2)
        stB(0)
        stB(1)
        for i in range(nit):
            if i + 3 < nit:
                stA(i + 3)
            if i + 2 < nit:
                stB(i + 2)
            stC1(i)
            next(pgen5, None)
            next(pgen5, None)
            if i > 0 and its[i][1] != 0:
                stC2(i - 1)
            if its[i][1] == 15:
                stC2(i)
                chunk_end(its[i][0])
        for _ in pgen5:
            pass
        kb.barrier()
        ph3.close()
        wg_bf = kb.sb(ph, 's5_wgbf', [128, 4, 512], BF16)
        bglu = kb.sb(ph, 's5_bglu', [128, 4], F32)
        sg = kb.sb(ph, 's5_sg', [128, 512], BF16)
        load_w(wg_bf[:], 'wgbf', kb.inp('w_glu', [512, 512]))
        kb.dma('sp', bglu[:], kb.inp('b_gluT', [128, 4]), w=['bglu'])
        for mt in range(4):
            for c in range(4):
                pb = c % 2
                for kc in range(4):
                    kb.mm(ps[pb][:, :], wg_bf[:, kc, mt * 128:(mt + 1) * 128], zT[:, kc, c * 512:(c + 1) * 512], kc == 0, kc == 3,
                          r=['wgbf', 'zT'], w=[f'ps{pb}'])
                kb.act(sg[:], ps[pb][:, :], AF.Sigmoid, r=[f'ps{pb}', 'bglu'], w=['sg'], bias=bglu[:, mt:mt + 1])
                kb.tt('dve', y_ssmT[:, mt, c * 512:(c + 1) * 512], zT[:, mt, c * 512:(c + 1) * 512], sg[:], ALU.mult,
                      r=['zT', 'sg'], w=['y_ssmT'])
        if dbg and stage == 'S5':
            kb.dump('zT', zT[:], [128, 4, SO], 'zT', BF16)
            kb.dump('y_ssmT', y_ssmT[:], [128, 4, SO], 'y_ssmT', BF16)
            kb.dump('sc', sc[:], [128, 24, 16], K)
        kb.barrier()
    phU.close()
    if not st('ATT'):
        attK.close()
        return finish_now()
    phO = ExitStack()
    hTo = kb.sb(phO, 'hTo', [128, 8, SO], BF16)
    o_nsaT = kb.sb(phO, 'o_nsaT', [128, 4, SO], BF16)
    with ExitStack() as ph:
        norm_transpose(kb.inp('xo', [SO, D]), NO, hTo, 'hTo', ph)
        kb.barrier()
    with ExitStack() as ph:
        qn = kb.sb(ph, 'qn', [128, 4, SO], BF16)
        cmb = kb.sb(ph, 'cmb', [128, 2, SO], BF16)
        selb = kb.sb(ph, 'selb', [128, NO, 64], F32)
        Etab = kb.sb(ph, 'Etab', [128, 32, 128], BF16)
        cbt = kb.sb(ph, 'cbt', [128, 2, 128], BF16)
        wbm = kb.sb(ph, 'wbm', [128, 6, 128], BF16)
        selm1T = kb.sb(ph, 'selm1T', [128, SO], BF16)
        gates = kb.sb(ph, 'gates', [128, NO, 24], F32)
        kb.dma('pool', cmb[:], kb.inp('cmb', [128, 2, SO]), w=['cmb'])
        kb.dma('sp', selb[:], kb.inp('selb', [128, NO, 64]), w=['selb'])
        kb.dma('pool', Etab[:], kb.inp('Etab', [128, 32, 128]), w=['Etab'])
        kb.dma('pool', cbt[:], kb.inp('cbt', [128, 2, 128]), w=['cbt'])
        kb.dma('pool', wbm[:], kb.inp('wbm', [128, 6, 128]), w=['wbm'])
        with ExitStack() as ph2:
            wq = kb.sb(ph2, 'wq', [128, 8, 512], BF16)
            wgn = kb.sb(ph2, 'wgn', [128, 8, 24], BF16)
            tmp = {'raw': kb.sb(ph2, 'fn_raw2', [128, 512], F32), 'sq': kb.sb(ph2, 'fn_sq2', [128, 512], BF16),
                   'rs': kb.sb(ph2, 'fn_rs2', [128, 512], F32)}
            load_w(wq[:], 'wq', w_in[:, OQ:OQ + 512])
            load_w(wgn[:], 'wgn', w_in[:, OGN:OGN + 24])
            for r in range(4):
                for c in range(4):
                    pb = c % 2
                    for g in range(2):
                        hd = 4 * g + r
                        for kc in range(8):
                            kb.mm(ps[pb][g * 64:(g + 1) * 64, :], wq[:, kc, hd * 64:(hd + 1) * 64], hTo[:, kc, c * 512:(c + 1) * 512],
                                  kc == 0, kc == 7, r=['wq', 'hTo'], w=[f'ps{pb}'])
                    fm_norm(ps[pb][:, :], f'ps{pb}', 512, qn[:, r, c * 512:(c + 1) * 512], 'qn', 0, tmp, 1.0, 64 * EPS, 2 + pb)
            for j in range(NO):
                for kc in range(8):
                    kb.mm(ps[6][:, 0:24], hTo[:, kc, j * 128:(j + 1) * 128], wgn[:, kc, :], kc == 0, kc == 7, r=['wgn', 'hTo'], w=['ps6'])
                kb.act(gates[:, j, :], ps[6][:, 0:24], AF.Sigmoid, r=['ps6'], w=['gates'])
            kb.barrier()
        Pb = [kb.sb(ph, f'Pb{i}', [128, 512], BF16) for i in range(3)]
        onsa = [kb.sb(ph, f'onsa{i}', [128, 512], F32) for i in range(2)]
        impacc = kb.sb(ph, 'impacc', [128, 128], F32)
        impb = kb.sb(ph, 'impb', [128, 128], F32)
        sel = kb.sb(ph, 'sel', [128, 128], F32)
        tmps = kb.sb(ph, 'tmps', [128, 64], F32)
        m8 = kb.sb(ph, 'm8', [128, 16], F32)
        rs = kb.sb(ph, 'rs', [128, 4], F32)
        coef = kb.sb(ph, 'coef', [128, 4], F32)
        tmpo = kb.sb(ph, 'tmpo', [128, 4, 64], F32)
        ctr = {'s': 0, 'p': 0, 'o': 0}

        def nxt(k, n):
            v = ctr[k] % n
            ctr[k] += 1
            return v

        def score_exp(mms):
            sb_ = nxt('s', 2)
            out3 = ps[sb_][:, :].rearrange("p (r q) -> p r q", r=4)
            for i, (lhsT, rhs, rk) in enumerate(mms):
                kb.mm(out3, lhsT, rhs, i == 0, i == len(mms) - 1, r=rk, w=[f'ps{sb_}'])
            pi = nxt('p', 3)
            kb.act(Pb[pi][:], ps[sb_][:, :], AF.Exp, r=[f'ps{sb_}'], w=[f'Pb{pi}'])
            pre_tick()
            return pi

        def cmp_att(g, j):
            gp = slice(g * 64, (g + 1) * 64)
            jsl = slice(j * 128, (j + 1) * 128)
            ob = j % 2
            def sc_(bt):
                return score_exp([(kcn[gp, bt * 128:(bt + 1) * 128], qn[gp, :, jsl], ['kcn', 'qn']),
                                  (identb[:, :], bc(cmb[:, bt, jsl].unsqueeze(1), [128, 4, 128]), ['identb', 'cmb'])])
            pis = {0: sc_(0)}
            for bt in range(2):
                if bt + 1 < 2:
                    pis[bt + 1] = sc_(bt + 1)
                pi = pis[bt]
                for r in range(4):
                    bank = 4 + r // 2
                    c0 = (r % 2) * 129
                    kb.mm(ps[bank][:, c0:c0 + 129], Pb[pi][:, r * 128:(r + 1) * 128], VOX[:, bt, g, :], bt == 0 and r % 2 == 0, bt == 1,
                          r=[f'Pb{pi}', 'VOX'], w=[f'ps{bank}'], sgc=True)
            for bank in (4, 5):
                v = ps[bank][:, 0:258].rearrange("p (r c) -> p r c", r=2)
                hd0 = 4 * g + 2 * (bank - 4)
                kb.ts('dve', rs[:, 0:2], v[:, :, 64], 1e-30, ALU.max, r=[f'ps{bank}'], w=['rs'])
                kb.op('dve', lambda e: e.reciprocal(out=rs[:, 0:2], in_=rs[:, 0:2]), r=['rs'], w=['rs'])
                kb.tt('dve', coef[:, 0:2], rs[:, 0:2], gates[:, j, hd0 * 3:hd0 * 3 + 6:3], ALU.mult, r=['rs', 'gates'], w=['coef'])
                for rr in range(2):
                    hd = hd0 + rr
                    ia = impacc[:, g * 64:(g + 1) * 64]
                    if hd % 4 == 0:
                        kb.ts('dve', ia, v[:, rr, 65:129], rs[:, rr:rr + 1], ALU.mult, r=[f'ps{bank}', 'rs'], w=['impacc'])
                    else:
                        kb.stt(ia, v[:, rr, 65:129], rs[:, rr:rr + 1], ia, ALU.mult, ALU.add, r=[f'ps{bank}', 'rs', 'impacc'], w=['impacc'])
                    kb.ts('dve', onsa[ob][:, hd * 64:(hd + 1) * 64], v[:, rr, 0:64], coef[:, rr:rr + 1], ALU.mult,
                          r=[f'ps{bank}', 'coef'], w=[f'onsa{ob}'])

        def select(j):
            kb.tt('dve', impb[:].rearrange("p (g b) -> p g b", g=2), impacc[:].rearrange("p (g b) -> p g b", g=2),
                  bc(selb[:, j, :].unsqueeze(1), [128, 2, 64]), ALU.add, r=['impacc', 'selb'], w=['impb'])
            for g in range(2):
                iv = impb[:, g * 64:(g + 1) * 64]
                kb.op('dve', lambda e: e.max(out=m8[:, 0:8], in_=iv), r=['impb'], w=['m8'])
                kb.op('dve', lambda e: e.match_replace(out=tmps[:], in_to_replace=m8[:, 0:8], in_values=iv, imm_value=-1e9),
                      r=['impb', 'm8'], w=['tmps'])
                kb.op('dve', lambda e: e.max(out=m8[:, 8:16], in_=tmps[:]), r=['tmps'], w=['m8'])
                kb.ts('dve', sel[:, g * 64:(g + 1) * 64], iv, m8[:, 15:16], ALU.is_ge, r=['impb', 'm8'], w=['sel'])
            kb.ts('dve', sel[:], sel[:], -1.0, ALU.add, r=['sel'], w=['sel'])
            kb.tr(ps[6][:, 0:128], sel[:], ident[:], r=['sel', 'ident'], w=['ps6'])
            kb.act(selm1T[:, j * 128:(j + 1) * 128], ps[6][:, 0:128], AF.Copy, r=['ps6'], w=['selm1T'])

        def evac_o(bank, g, j, br):
            ob = j % 2
            v = ps[bank][:, 0:260].rearrange("p (r c) -> p r c", r=4)
            kb.ts('dve', rs[:, 0:4], v[:, :, 64], 1e-30, ALU.max, r=[f'ps{bank}'], w=['rs'])
            kb.op('dve', lambda e: e.reciprocal(out=rs[:, 0:4], in_=rs[:, 0:4]), r=['rs'], w=['rs'])
            kb.tt('dve', coef[:, 0:4], rs[:, 0:4], gates[:, j, 12 * g + br:12 * g + 12:3], ALU.mult, r=['rs', 'gates'], w=['coef'])
            kb.tt('dve', tmpo[:], v[:, :, 0:64], bc(coef[:, 0:4].unsqueeze(2), [128, 4, 64]), ALU.mult, r=[f'ps{bank}', 'coef'], w=['tmpo'])
            od = onsa[ob][:, g * 256:(g + 1) * 256].rearrange("p (r d) -> p r d", r=4)
            kb.tt('pool', od, od, tmpo[:], ALU.add, r=[f'onsa{ob}', 'tmpo'], w=[f'onsa{ob}'])

        def sel_att(g, j):
            gp = slice(g * 64, (g + 1) * 64)
            jsl = slice(j * 128, (j + 1) * 128)
            nk = 2 * j + 2
            bank = 2 + nxt('o', 2)
            def sc_(kt):
                mms = [(ksT[gp, kt * 128:(kt + 1) * 128], qn[gp, :, jsl], ['ksT', 'qn']),
                       (Etab[gp, kt, :], bc(selm1T[gp, jsl].unsqueeze(1), [64, 4, 128]), ['Etab', 'selm1T'])]
                if kt >= 2 * j:
                    mms.append((identb[:, :], bc(cbt[:, kt - 2 * j, :].unsqueeze(1), [128, 4, 128]), ['identb', 'cbt']))
                return score_exp(mms)
            pis = {0: sc_(0)}
            for kt in range(nk):
                if kt + 1 < nk:
                    pis[kt + 1] = sc_(kt + 1)
                pi = pis.pop(kt)
                for r in range(4):
                    kb.mm(ps[bank][:, r * 65:(r + 1) * 65], Pb[pi][:, r * 128:(r + 1) * 128], VS[:, kt, g, :], kt == 0 and r == 0, kt == nk - 1,
                          r=[f'Pb{pi}', 'VS'], w=[f'ps{bank}'], sgc=True)
            evac_o(bank, g, j, 1)

        def win_att(g, j):
            gp = slice(g * 64, (g + 1) * 64)
            jsl = slice(j * 128, (j + 1) * 128)
            bank = 2 + nxt('o', 2)
            kts = [(i, 2 * j - 4 + i) for i in range(6) if 2 * j - 4 + i >= 0]

            def sc_(n):
                i, kt = kts[n]
                return score_exp([(kwT[gp, kt * 128:(kt + 1) * 128], qn[gp, :, jsl], ['kwT', 'qn']),
                                  (identb[:, :], bc(wbm[:, i, :].unsqueeze(1), [128, 4, 128]), ['identb', 'wbm'])])
            pis = {0: sc_(0)}
            for n in range(len(kts)):
                if n + 1 < len(kts):
                    pis[n + 1] = sc_(n + 1)
                pi = pis.pop(n)
                kt = kts[n][1]
                for r in range(4):
                    kb.mm(ps[bank][:, r * 65:(r + 1) * 65], Pb[pi][:, r * 128:(r + 1) * 128], VW[:, kt, g, :], n == 0 and r == 0, n == len(kts) - 1,
                          r=[f'Pb{pi}', 'VW'], w=[f'ps{bank}'], sgc=True)
            evac_o(bank, g, j, 2)

        def finalize(j):
            ob = j % 2
            for q in range(4):
                kb.tr(ps[6][:, q * 128:(q + 1) * 128], onsa[ob][:, q * 128:(q + 1) * 128], ident[:], r=[f'onsa{ob}', 'ident'], w=['ps6'])
            kb.act(o_nsaT[:, :, j * 128:(j + 1) * 128], ps[6][:, :].rearrange("p (q t) -> p q t", q=4), AF.Copy, r=['ps6'], w=['o_nsaT'])

        if pre_on and NPRE_ATT > 0:
            pgen = make_pre_gen(ph, 'a', NPRE_S5, NPRE_ATT, 6, 'pool', 1024)
        else:
            pgen = iter(())
        pstep = {'n': 0}

        def pre_tick():
            pstep['n'] += 1
            if (pstep['n'] * 6 * NPRE_ATT) // 800 != ((pstep['n'] - 1) * 6 * NPRE_ATT) // 800:
                next(pgen, None)

        NJ = NO if not (dbg and stage == 'ATT' and DBG_NJ) else DBG_NJ
        cmp_att(0, 0)
        cmp_att(1, 0)
        select(0)
        for j in range(NJ):
            if j + 1 < NJ:
                cmp_att(0, j + 1)
                cmp_att(1, j + 1)
                select(j + 1)
            sel_att(0, j)
            sel_att(1, j)
            win_att(0, j)
            win_att(1, j)
            finalize(j)
        for _ in pgen:
            pass
        if dbg and stage == 'ATT':
            kb.dump('qn', qn[:], [128, 4, SO], 'qn', BF16)
            kb.dump('gates', gates[:], [128, NO, 24], 'gates')
            kb.dump('selm1T', selm1T[:], [128, SO], 'selm1T', BF16)
            kb.dump('o_nsaT', o_nsaT[:], [128, 4, SO], 'o_nsaT', BF16)
        kb.barrier()
    if not st('MERGE'):
        phO.close()
        attK.close()
        return finish_now()
    x1s = kb.scratch('x1s', [SO, D], F32)
    xo_d = kb.inp('xo', [SO, D])
    with ExitStack() as ph:
        mergedT = kb.sb(ph, 'mergedT', [128, 8, SO], BF16)
        wo = kb.sb(ph, 'wo', [128, 8, 1024], BF16)
        load_w(wo[:], 'wo', kb.inp('w_out', [1024, 1024]))
        wga = [kb.sb(ph, f'wga{i}', [128, 8, 128], BF16) for i in range(2)]
        wgs = [kb.sb(ph, f'wgs{i}', [128, 8, 128], BF16) for i in range(2)]
        wua = [kb.sb(ph, f'wua{i}', [128, 4, 128], BF16) for i in range(2)]
        wus = [kb.sb(ph, f'wus{i}', [128, 4, 128], BF16) for i in range(2)]
        sgA = kb.sb(ph, 'sgA', [128, 512], F32)
        sgB = kb.sb(ph, 'sgB', [128, 512], F32)
        tmA = kb.sb(ph, 'tmA', [128, 512], F32)
        tmB = kb.sb(ph, 'tmB', [128, 512], F32)
        w_up_attn = kb.inp('w_up_attn', [512, 1024])
        w_up_ssm = kb.inp('w_up_ssm', [512, 1024])
        for m in range(8):
            b = m % 2
            msl = slice(m * 128, (m + 1) * 128)
            load_w(wga[b][:], f'wga{b}', w_in[:, OGA + m * 128:OGA + (m + 1) * 128])
            load_w(wgs[b][:], f'wgs{b}', w_in[:, OGS + m * 128:OGS + (m + 1) * 128])
            load_w(wua[b][:], f'wua{b}', w_up_attn[:, msl])
            load_w(wus[b][:], f'wus{b}', w_up_ssm[:, msl])
            for c in range(4):
                csl = slice(c * 512, (c + 1) * 512)
                for kc in range(4):
                    kb.mm(ps[0][:, :], wua[b][:, kc, :], o_nsaT[:, kc, csl], kc == 0, kc == 3, r=[f'wua{b}', 'o_nsaT'], w=['ps0'])
                for kc in range(8):
                    kb.mm(ps[1][:, :], wga[b][:, kc, :], hTo[:, kc, csl], kc == 0, kc == 7, r=[f'wga{b}', 'hTo'], w=['ps1'])
                for kc in range(4):
                    kb.mm(ps[2][:, :], wus[b][:, kc, :], y_ssmT[:, kc, csl], kc == 0, kc == 3, r=[f'wus{b}', 'y_ssmT'], w=['ps2'])
                for kc in range(8):
                    kb.mm(ps[3][:, :], wgs[b][:, kc, :], hTo[:, kc, csl], kc == 0, kc == 7, r=[f'wgs{b}', 'hTo'], w=['ps3'])
                kb.act(sgA[:], ps[1][:, :], AF.Sigmoid, r=['ps1'], w=['sgA'])
                kb.act(sgB[:], ps[3][:, :], AF.Sigmoid, r=['ps3'], w=['sgB'])
                kb.tt('dve', tmA[:], sgA[:], ps[0][:, :], ALU.mult, r=['sgA', 'ps0'], w=['tmA'])
                kb.tt('dve', tmB[:], sgB[:], ps[2][:, :], ALU.mult, r=['sgB', 'ps2'], w=['tmB'])
                kb.tt('pool', mergedT[:, m, csl], tmA[:], tmB[:], ALU.add, r=['tmA', 'tmB'], w=['mergedT'])
        gt1row = kb.sb(ph, 'gt1row', [128, 1024], F32)
        kb.dma('sp', gt1row[:], modrow_d[0:1, 0:1024].partition_broadcast(128), r=['modrow_d'], w=['gt1row'])
        xt = [kb.sb(ph, f'mg_x{i}', [128, 1024], F32) for i in range(2)]
        x1t = [kb.sb(ph, f'mg_x1{i}', [128, 1024], F32) for i in range(2)]
        tmx = kb.sb(ph, 'tmx', [128, 512], F32)
        for i in range(NO):
            b = i % 2
            isl = slice(i * 128, (i + 1) * 128)
            kb.dma('sp', xt[b][:], xo_d[isl, :], w=[f'mg_x{b}'])
            for h in range(2):
                pb = 4 + h
                hsl = slice(h * 512, (h + 1) * 512)
                for kc in range(8):
                    kb.mm(ps[pb][:, :], mergedT[:, kc, isl], wo[:, kc, hsl], kc == 0, kc == 7, r=['mergedT', 'wo'], w=[f'ps{pb}'])
                kb.tt('dve', tmx[:], ps[pb][:, :], gt1row[:, hsl], ALU.mult, r=[f'ps{pb}', 'gt1row'], w=['tmx'])
                kb.tt('pool', x1t[b][:, hsl], tmx[:], xt[b][:, hsl], ALU.add, r=['tmx', f'mg_x{b}'], w=[f'mg_x1{b}'])
            kb.dma('sp', x1s[isl, :], x1t[b][:], r=[f'mg_x1{b}'], w=[])
        if dbg and stage == 'MERGE':
            kb.dump('mergedT', mergedT[:], [128, 8, SO], 'mergedT', BF16)
            o_ = kb.outp('dbg_x1', [SO, D])
            kb.dma('sp', o_, x1s, r=['x1s'])
        kb.barrier()
    phO.close()
    attK.close()
    if not st('MOE'):
        return finish_now()
    NSLOT = NEXP * CAP
    xs_d = kb.scratch('xs_d', [NSLOT + 1, D], BF16)
    ys_d = kb.scratch('ys_d', [NSLOT + 1, D], BF16)
    out_d = kb.outp('out', [SO, D], F32)
    with ExitStack() as ph:
        h2T = kb.sb(ph, 'h2T', [128, 8, SO], BF16)
        selbf = kb.sb(ph, 'selbf', [128, NO, 256], BF16)
        w8 = kb.sb(ph, 'w8', [128, NO, 8], F32)
        idx = kb.sb(ph, 'idx', [128, NO, 8], I32)
        SU = kb.sb(ph, 'SU', [128, 128], BF16)
        onesb = kb.sb(ph, 'onesb', [128, 128], BF16)
        eCAP1 = kb.sb(ph, 'eCAP1', [128, 256], F32)
        rbias = kb.sb(ph, 'rbias', [128, 256], F32)
        zrow = kb.sb(ph, 'zrow', [1, 1024], BF16)
        sh2row = kb.sb(ph, 'sh2row', [128, 1024], F32)
        gt2row = kb.sb(ph, 'gt2row', [128, 1024], F32)
        a2row = kb.sb(ph, 'a2rowm', [128, 1024], F32)
        kb.dma('sp', sh2row[:], modrow_d[0:1, 1024:2048].partition_broadcast(128), r=['modrow_d'], w=['sh2row'])
        kb.dma('sp', gt2row[:], modrow_d[0:1, 3072:4096].partition_broadcast(128), r=['modrow_d'], w=['gt2row'])
        kb.dma('sp', a2row[:], a2row_d[0:1, :].partition_broadcast(128), r=['a2row_d'], w=['a2row'])
        kb.dma('pool', SU[:], kb.inp('SU', [128, 128]), w=['SU'])
        kb.op('pool', lambda e: e.memset(onesb[:], 1.0), w=['onesb'])
        kb.dma('sp', eCAP1[:], kb.inp('eCAP1', [128, 256]), w=['eCAP1'])
        kb.dma('sp', rbias[:], kb.inp('rbias', [1, 256])[0:1, :].partition_broadcast(128), w=['rbias'])
        kb.op('pool', lambda e: e.memset(zrow[:], 0.0), w=['zrow'])
        kb.dma('sp', ys_d[NSLOT:NSLOT + 1, :], zrow[:], r=['zrow'], w=['ys_d'])
        with ExitStack() as ph2:
            wr = kb.sb(ph2, 'wr', [128, 8, 256], F32)
            kb.dma('sp', wr[:], kb.inp('w_router', [1024, 256]).rearrange("(kc p) n -> p kc n", p=128), w=['wr'])

            def mk(name, shape, dt=F32):
                return [kb.sb(ph2, f'{name}{i}', shape, dt) for i in range(4)]
            x1t = mk('mo_x', [128, 1024])
            h2f = mk('mo_h', [128, 1024])
            h2b = mk('mo_hb', [128, 1024], BF16)
            h2Tf = mk('h2Tf', [128, 8, 128])
            junk = mk('mo_junk', [128, 1024], BF16)
            ss = mk('mo_ss', [128, 4])
            scr_ = mk('mo_sc', [128, 256])
            bia = mk('mo_bia', [128, 256])
            msk = mk('mo_msk', [128, 256])
            selm = mk('mo_sel', [128, 256])
            wm = mk('mo_wm', [128, 256])
            okm = mk('mo_okm', [128, 256])
            slotv = mk('mo_slotv', [128, 256])
            jk2 = mk('mo_jk2', [128, 256])
            m8g = mk('mo_m8g', [128, 8, 8])
            gs = mk('mo_gs', [128, 8])
            gm8 = mk('mo_gm8', [128, 8])
            gmask = mk('mo_gmask', [128, 8])
            gneg = mk('mo_gneg', [128, 8])
            t8 = mk('mo_t8', [128, 8])
            s8 = mk('mo_s8', [128, 8])
            s8b = mk('mo_s8b', [128, 8])
            idf = mk('mo_idf', [128, 8])
            wsum = mk('mo_wsum', [128, 2])

            def route_tile(i):
                b = i % 4
                B = str(b)
                isl = slice(i * 128, (i + 1) * 128)
                kb.dma('sp', x1t[b][:], x1s[isl, :], r=['x1s'], w=['mo_x' + B])
                kb.act(junk[b][:], x1t[b][:], AF.Square, r=['mo_x' + B], w=['mo_junk' + B, 'mo_ss0' + B], accum=ss[b][:, 0:1])
                yield
                kb.act(ss[b][:, 1:2], ss[b][:, 0:1], AF.Sqrt, r=['mo_ss0' + B], w=['mo_ss1' + B], scale=1.0 / D, bias=EPS)
                yield
                kb.op('dve', lambda e: e.reciprocal(out=ss[b][:, 2:3], in_=ss[b][:, 1:2]), r=['mo_ss1' + B], w=['mo_ss2' + B])
                yield
                kb.stt(h2f[b][:], x1t[b][:], ss[b][:, 2:3], a2row[:], ALU.mult, ALU.mult, r=['mo_x' + B, 'mo_ss2' + B, 'a2row'], w=['mo_h' + B])
                yield
                kb.tt('dve', h2f[b][:], h2f[b][:], sh2row[:], ALU.add, r=['mo_h' + B, 'sh2row'], w=['mo_h' + B])
                yield
                kb.act(h2b[b][:], h2f[b][:], AF.Copy, r=['mo_h' + B], w=['mo_hb' + B])
                for half in range(2):
                    pb = 2 * b
                    for q in range(4):
                        kc = half * 4 + q
                        kb.tr(ps[pb][:, q * 128:(q + 1) * 128], h2f[b][:, kc * 128:(kc + 1) * 128], ident[:], r=['mo_h' + B, 'ident'], w=[f'ps{pb}'])
                    yield
                    kb.act(h2Tf[b][:, half * 4:half * 4 + 4, :], ps[pb][:, :].rearrange("p (q t) -> p q t", q=4), AF.Copy, r=[f'ps{pb}'], w=['h2Tf' + B])
                yield
                kb.act(h2T[:, :, isl], h2Tf[b][:], AF.Copy, r=['h2Tf' + B], w=[f'h2T{i}'])
                pr = 2 * b + 1
                for kc in range(8):
                    kb.mm(ps[pr][:, 0:256], h2Tf[b][:, kc, :], wr[:, kc, :], kc == 0, kc == 7, r=['h2Tf' + B, 'wr'], w=[f'ps{pr}'])
                yield
                kb.act(scr_[b][:], ps[pr][:, 0:256], AF.Sigmoid, r=[f'ps{pr}'], w=['mo_sc' + B])
                yield
                kb.tt('dve', bia[b][:], scr_[b][:], rbias[:], ALU.add, r=['mo_sc' + B, 'rbias'], w=['mo_bia' + B])
                yield
                for gi in range(8):
                    kb.op('dve', lambda e: e.max(out=m8g[b][:, gi, :], in_=bia[b][:, gi * 32:(gi + 1) * 32]), r=['mo_bia' + B], w=['mo_m8g' + B + str(gi)])
                yield
                kb.tt('dve', gs[b][:], m8g[b][:, :, 0], m8g[b][:, :, 1], ALU.add, r=['mo_m8g' + B + str(g_) for g_ in range(8)], w=['mo_gs' + B])
                yield
                kb.op('dve', lambda e: e.max(out=gm8[b][:], in_=gs[b][:]), r=['mo_gs' + B], w=['mo_gm8' + B])
                yield
                kb.ts('dve', gmask[b][:], gs[b][:], gm8[b][:, 3:4], ALU.is_ge, r=['mo_gs' + B, 'mo_gm8' + B], w=['mo_gmask' + B])
                yield
                kb.ts('dve', gneg[b][:], gmask[b][:], -1.0, ALU.add, s2=10.0, op1=ALU.mult, r=['mo_gmask' + B], w=['mo_gneg' + B])
                b3 = bia[b][:].rearrange("p (g e) -> p g e", g=8)
                m3 = msk[b][:].rearrange("p (g e) -> p g e", g=8)
                kb.tt('dve', m3, b3, bc(gmask[b][:].unsqueeze(2), [128, 8, 32]), ALU.mult, r=['mo_bia' + B, 'mo_gmask' + B], w=['mo_msk' + B])
                yield
                kb.tt('dve', m3, m3, bc(gneg[b][:].unsqueeze(2), [128, 8, 32]), ALU.add, r=['mo_msk' + B, 'mo_gneg' + B], w=['mo_msk' + B])
                yield
                kb.op('dve', lambda e: e.max(out=t8[b][:], in_=msk[b][:]), r=['mo_msk' + B], w=['mo_t8' + B])
                yield
                kb.ts('dve', selm[b][:], msk[b][:], t8[b][:, 7:8], ALU.is_ge, r=['mo_msk' + B, 'mo_t8' + B], w=['mo_sel' + B])
                yield
                kb.tt('dve', wm[b][:], scr_[b][:], selm[b][:], ALU.mult, r=['mo_sc' + B, 'mo_sel' + B], w=['mo_wm' + B])
                kb.act(selbf[:, i, :], selm[b][:], AF.Copy, r=['mo_sel' + B], w=[f'selbf{i}'])
                yield
                kb.op('dve', lambda e: e.tensor_reduce(out=wsum[b][:, 0:1], in_=wm[b][:], axis=AX.X, op=ALU.add), r=['mo_wm' + B], w=['mo_wsum' + B])
                yield
                kb.op('dve', lambda e: e.reciprocal(out=wsum[b][:, 1:2], in_=wsum[b][:, 0:1]), r=['mo_wsum' + B], w=['mo_wsum' + B])
                yield
                kb.ts('dve', wm[b][:], wm[b][:], wsum[b][:, 1:2], ALU.mult, s2=2.5, op1=ALU.mult, r=['mo_wm' + B, 'mo_wsum' + B], w=['mo_wm' + B])
                pp = 2 * b + 1
                for i2 in range(i + 1):
                    lhs = SU[:, :] if i2 == i else onesb[:, :]
                    kb.mm(ps[pp][:, 256:512], lhs, selbf[:, i2, :], i2 == 0, i2 == i, r=['SU', 'onesb', f'selbf{i2}'], w=[f'ps{pp}'])
                yield
                kb.ts('dve', okm[b][:], ps[pp][:, 256:512], float(CAP), ALU.is_lt, r=[f'ps{pp}'], w=['mo_okm' + B])
                yield
                kb.tt('dve', okm[b][:], okm[b][:], selm[b][:], ALU.mult, r=['mo_okm' + B, 'mo_sel' + B], w=['mo_okm' + B])
                kb.tt('dve', slotv[b][:], ps[pp][:, 256:512], eCAP1[:], ALU.add, r=[f'ps{pp}', 'eCAP1'], w=['mo_slotv' + B])
                yield
                kb.tt('dve', slotv[b][:], slotv[b][:], okm[b][:], ALU.mult, r=['mo_slotv' + B, 'mo_okm' + B], w=['mo_slotv' + B])
                yield
                kb.op('dve', lambda e: e.max(out=s8[b][:], in_=slotv[b][:]), r=['mo_slotv' + B], w=['mo_s8' + B])
                yield
                for k in range(8):
                    kb.stt(jk2[b][:], slotv[b][:], s8[b][:, k:k + 1], wm[b][:], ALU.is_equal, ALU.mult, r=['mo_slotv' + B, 'mo_s8' + B, 'mo_wm' + B],
                           w=['mo_jk2' + B, f'w8_{i}_{k}'], accum=w8[:, i, k:k + 1])
                    if k % 2 == 1:
                        yield
                kb.ts('dve', s8b[b][:], s8[b][:], 0.0, ALU.is_equal, s2=float(NSLOT + 1), op1=ALU.mult, r=['mo_s8' + B], w=['mo_s8b' + B])
                yield
                kb.stt(idf[b][:], s8[b][:], -1.0, s8b[b][:], ALU.add, ALU.add, r=['mo_s8' + B, 'mo_s8b' + B], w=['mo_idf' + B])
                yield
                kb.cp('dve', idx[:, i, :], idf[b][:], r=['mo_idf' + B], w=[f'idx{i}'])
                yield
                for k in range(8):
                    kb.dma('pool', None, None, r=['mo_hb' + B, f'idx{i}'], w=[],
                           fn=lambda e: e.indirect_dma_start(out=xs_d[:, :], out_offset=bass.IndirectOffsetOnAxis(ap=idx[:, i, k:k + 1], axis=0),
                                                             in_=h2b[b][:], in_offset=None))
                    if k % 4 == 3:
                        yield
                if dbg and stage == 'MOE' and i == 0:
                    kb.dump('wm0', wm[b][:], [128, 256], 'mo_wm' + B)

            for i0_ in range(0, NO, 4):
                run_interleaved([route_tile(i0_ + k_) for k_ in range(4)], 4)
            kb.barrier()
        w_gate = kb.inp('w_gate', [NEXP, 1024, 256])
        w_up = kb.inp('w_up', [NEXP, 1024, 256])
        w_down = kb.inp('w_down', [NEXP, 256, 1024])
        if not st('MOE') or NPRE == 0:
            wpre = None
        NE = NEXP if not (dbg and DBG_NE) else DBG_NE
        with ExitStack() as ph2:
            NS = 3
            wgst = [kb.sb(ph2, f'wgst{i}', [128, 8, 256], F32) for i in range(NS)]
            wust = [kb.sb(ph2, f'wust{i}', [128, 8, 256], F32) for i in range(NS)]
            wdst = [kb.sb(ph2, f'wdst{i}', [128, 2, 1024], F32) for i in range(NS)]
            wgb = [kb.sb(ph2, f'wgb{i}', [128, 8, 256], BF16) for i in range(2)]
            wub = [kb.sb(ph2, f'wub{i}', [128, 8, 256], BF16) for i in range(2)]
            wdb = [kb.sb(ph2, f'wdb{i}', [128, 2, 1024], BF16) for i in range(2)]
            xse = [kb.sb(ph2, f'xse{i}', [128, 2, 1024], BF16) for i in range(NS)]
            hTe = [kb.sb(ph2, f'hTe{i}', [128, 8, CAP], BF16) for i in range(2)]
            sgt = kb.sb(ph2, 'sgt', [128, 2 * CAP], F32)
            actT = kb.sb(ph2, 'actT', [128, 2, CAP], BF16)
            yse = [kb.sb(ph2, f'yse{i}', [128, 2, 1024], BF16) for i in range(2)]
            p16a = ps[6][:, :].bitcast(BF16)
            p16b = ps[7][:, :].bitcast(BF16)

            def load_e(e):
                s_ = e % NS
                if not (wpre is not None and e < NPRE):
                    kb.dma('sp', wgst[s_][:], w_gate[e].rearrange("(p r) n -> p r n", r=8), w=[f'wgst{s_}'])
                    kb.dma('sp', wust[s_][:], w_up[e].rearrange("(p r) n -> p r n", r=8), w=[f'wust{s_}'])
                    kb.dma('sp', wdst[s_][:], w_down[e].rearrange("(kc p) n -> p kc n", p=128), w=[f'wdst{s_}'])
                kb.dma('sp', xse[s_][:, 0, :], xs_d[e * CAP:e * CAP + 128, :], r=['xs_d'], w=[f'xse{s_}a'])
                kb.dma('sp', xse[s_][0:64, 1, :], xs_d[e * CAP + 128:e * CAP + 192, :], r=['xs_d'], w=[f'xse{s_}b'])

            def cast_e(e):
                s_ = e % NS
                b = e % 2
                if wpre is not None and e < NPRE:
                    kb.dma('sp', wgb[b][:].rearrange("p r n -> p (r n)"), wpre[0][e], w=[f'wgb{b}'])
                    kb.dma('sp', wub[b][:].rearrange("p r n -> p (r n)"), wpre[1][e], w=[f'wub{b}'])
                    kb.dma('sp', wdb[b][:].rearrange("p r n -> p (r n)"), wpre[2][e], w=[f'wdb{b}'])
                    return
                kb.cp('pool', wgb[b][:, 0:4, :], wgst[s_][:, 0:4, :], r=[f'wgst{s_}'], w=[f'wgb{b}'])
                kb.cp('dve', wgb[b][:, 4:8, :], wgst[s_][:, 4:8, :], r=[f'wgst{s_}'], w=[f'wgb{b}'])
                kb.act(wub[b][:], wust[s_][:], AF.Copy, r=[f'wust{s_}'], w=[f'wub{b}'])
                kb.cp('dve', wdb[b][:, 0, :], wdst[s_][:, 0, :], r=[f'wdst{s_}'], w=[f'wdb{b}'])
                kb.act(wdb[b][:, 1, :], wdst[s_][:, 1, :], AF.Copy, r=[f'wdst{s_}'], w=[f'wdb{b}'])

            def transp_e(e):
                s_ = e % NS
                b = e % 2
                for kc in range(8):
                    kb.tr(p16a[:, kc * 128:(kc + 1) * 128], xse[s_][:, 0, kc:1024:8], identb[:], r=[f'xse{s_}a', 'identb'], w=['ps6'])
                for kc in range(8):
                    kb.tr(p16b[:, kc * 64:(kc + 1) * 64], xse[s_][0:64, 1, kc:1024:8], identb[0:64, 0:64], r=[f'xse{s_}b', 'identb'], w=['ps7'])
                kb.act(hTe[b][:, :, 0:128], p16a[:, :].rearrange("p (k s) -> p k s", k=8), AF.Copy, r=['ps6'], w=[f'hTe{b}'])
                kb.cp('dve', hTe[b][:, :, 128:192], p16b[:, 0:512].rearrange("p (k s) -> p k s", k=8), r=['ps7'], w=[f'hTe{b}'])

            load_e(0)
            if NE > 1:
                load_e(1)
            cast_e(0)
            transp_e(0)
            for e in range(NE):
                b = e % 2
                if e + 2 < NE:
                    load_e(e + 2)
                if e + 1 < NE:
                    cast_e(e + 1)
                for gu, (wt, wk, bank) in enumerate(((wgb, 'wgb', 0), (wub, 'wub', 1))):
                    for m in range(2):
                        for kc in range(8):
                            kb.mm(ps[bank][:, m * CAP:(m + 1) * CAP], wt[b][:, kc, m * 128:(m + 1) * 128], hTe[b][:, kc, :],
                                  m == 0 and kc == 0, m == 1 and kc == 7, r=[f'{wk}{b}', f'hTe{b}'], w=[f'ps{bank}'], sgc=True)
                kb.act(sgt[:], ps[0][:, 0:2 * CAP], AF.Silu, r=['ps0'], w=['sgt'])
                kb.tt('dve', actT[:].rearrange("p m s -> p (m s)"), sgt[:], ps[1][:, 0:2 * CAP], ALU.mult, r=['sgt', 'ps1'], w=['actT'])
                if e + 1 < NE:
                    transp_e(e + 1)
                for st_, (ns, banks) in enumerate(((128, (2, 3)), (64, (4, 5)))):
                    for h in range(2):
                        for m in range(2):
                            kb.mm(ps[banks[h]][0:ns, :], actT[:, m, st_ * 128:st_ * 128 + ns], wdb[b][:, m, h * 512:(h + 1) * 512], m == 0, m == 1,
                                  r=['actT', f'wdb{b}'], w=[f'ps{banks[h]}'])
                kb.act(yse[b][:, 0, 0:512], ps[2][:, :], AF.Copy, r=['ps2'], w=[f'yse{b}'])
                kb.cp('dve', yse[b][:, 0, 512:1024], ps[3][:, :], r=['ps3'], w=[f'yse{b}'])
                kb.act(yse[b][0:64, 1, 0:512], ps[4][0:64, :], AF.Copy, r=['ps4'], w=[f'yse{b}'])
                kb.cp('dve', yse[b][0:64, 1, 512:1024], ps[5][0:64, :], r=['ps5'], w=[f'yse{b}'])
                kb.dma('act', ys_d[e * CAP:e * CAP + 128, :], yse[b][:, 0, :], r=[f'yse{b}'], w=[])
                kb.dma('act', ys_d[e * CAP + 128:e * CAP + 192, :], yse[b][0:64, 1, :], r=[f'yse{b}'], w=[])
            kb.barrier()
        with ExitStack() as ph2:
            wsg = kb.sb(ph2, 'wsg', [128, 8, 256], BF16)
            wsu = kb.sb(ph2, 'wsu', [128, 8, 256], BF16)
            wsd = kb.sb(ph2, 'wsd', [128, 2, 1024], BF16)
            load_w(wsg[:], 'wsg', kb.inp('ws_gate', [1024, 256]))
            load_w(wsu[:], 'wsu', kb.inp('ws_up', [1024, 256]))
            load_w(wsd[:], 'wsd', kb.inp('ws_down', [256, 1024]))
            sgs = [kb.sb(ph2, f'sgs{i}', [128, 256], F32) for i in range(2)]
            acts = [kb.sb(ph2, f'acts{i}', [128, 2, 128], BF16) for i in range(2)]
            acc = [kb.sb(ph2, f'acc{i}', [128, 1024], F32) for i in range(2)]
            x1t = [kb.sb(ph2, f'cb_x{i}', [128, 1024], F32) for i in range(2)]
            gk = [kb.sb(ph2, f'gkx{i}', [128, 1024], BF16) for i in range(16)]

            def comb_tile(i):
                b = i % 2
                isl = slice(i * 128, (i + 1) * 128)
                kb.dma('sp', x1t[b][:], x1s[isl, :], r=['x1s'], w=[f'cb_x{b}'])
                for k in range(8):
                    gb = b * 8 + k
                    if k == 4:
                        yield
                    kb.dma('pool', None, None, r=['ys_d', f'idx{i}'], w=[f'gk{gb}'],
                           fn=lambda e: e.indirect_dma_start(out=gk[gb][:], out_offset=None, in_=ys_d[:, :],
                                                             in_offset=bass.IndirectOffsetOnAxis(ap=idx[:, i, k:k + 1], axis=0)))
                    if k == 3:
                        for gu, (wt, wk, bank) in enumerate(((wsg, 'wsg', 4 * b), (wsu, 'wsu', 4 * b + 1))):
                            for m in range(2):
                                for kc in range(8):
                                    kb.mm(ps[bank][:, m * 128:(m + 1) * 128], wt[:, kc, m * 128:(m + 1) * 128], h2T[:, kc, isl],
                                          m == 0 and kc == 0, m == 1 and kc == 7, r=[wk, f'h2T{i}'], w=[f'ps{bank}'], sgc=True)
                        yield
                        kb.act(sgs[b][:], ps[4 * b][:, 0:256], AF.Silu, r=[f'ps{4 * b}'], w=[f'sgs{b}'])
                        yield
                        kb.tt('dve', acts[b][:].rearrange("p m s -> p (m s)"), sgs[b][:], ps[4 * b + 1][:, 0:256], ALU.mult, r=[f'sgs{b}', f'ps{4 * b + 1}'], w=[f'acts{b}'])
                        yield
                        for h in range(2):
                            for m in range(2):
                                kb.mm(ps[4 * b + 2 + h][:, :], acts[b][:, m, :], wsd[:, m, h * 512:(h + 1) * 512], m == 0, m == 1, r=[f'acts{b}', 'wsd'], w=[f'ps{4 * b + 2 + h}'])
                        yield
                        kb.act(acc[b][:, 0:512], ps[4 * b + 2][:, :], AF.Copy, r=[f'ps{4 * b + 2}'], w=[f'acc{b}'])
                        kb.act(acc[b][:, 512:1024], ps[4 * b + 3][:, :], AF.Copy, r=[f'ps{4 * b + 3}'], w=[f'acc{b}'])
                        yield
                for k in range(8):
                    gb = b * 8 + k
                    kb.stt(acc[b][:], gk[gb][:], w8[:, i, k:k + 1], acc[b][:], ALU.mult, ALU.add, r=[f'gk{gb}', f'w8_{i}_{k}', f'acc{b}'], w=[f'acc{b}'])
                    yield
                kb.tt('dve', acc[b][:], acc[b][:], gt2row[:], ALU.mult, r=[f'acc{b}', 'gt2row'], w=[f'acc{b}'])
                yield
                kb.tt('dve', acc[b][:], acc[b][:], x1t[b][:], ALU.add, r=[f'acc{b}', f'cb_x{b}'], w=[f'acc{b}'])
                yield
                kb.dma('sp', out_d[isl, :], acc[b][:], r=[f'acc{b}'], w=[])
            for i0_ in range(0, NO, 2):
                run_interleaved([comb_tile(i0_), comb_tile(i0_ + 1)], 2)
            kb.barrier()
    return finish_now()


def host_consts(hf):
    c = {}
    m = np.zeros((128, 2), np.float32)
    m[:, hf] = 1.0
    c['hfm'] = m
    f = np.float32
    own_t = (np.arange(SO) // 128 * 2 + hf) * 128 + np.arange(SO) % 128
    n = np.arange(256)
    vis = (n[:, None] <= 254) & (16 * n[:, None] + 31 <= own_t[None, :])
    c['cmb'] = np.ascontiguousarray(np.where(vis, 0.0, -BIG).astype(f).reshape(2, 128, SO).transpose(1, 0, 2))
    i = np.arange(128)
    selb = np.zeros((128, NO, 64), f)
    jj = np.arange(64)
    for j in range(NO):
        cur = 2 * (2 * j + hf) + (i >= 64)
        forced = (jj[None, :] == 0) | (jj[None, :] == cur[:, None]) | (jj[None, :] == cur[:, None] - 1)
        valid = jj[None, :] <= cur[:, None]
        selb[:, j, :] = np.where(forced, 100.0, np.where(valid, 0.0, -100.0))
    c['selb'] = selb
    p = np.arange(128)
    E = np.zeros((128, 32, 128), f)
    for kt in range(32):
        E[:, kt, :] = np.where((p[:, None] % 64) == 2 * kt + (i[None, :] // 64), BIG, 0.0)
    c['Etab'] = E
    key = np.arange(128)[:, None]
    q = np.arange(128)[None, :]
    tri = np.where(key > q, -BIG, 0.0).astype(f)
    cb = np.zeros((128, 2, 128), f)
    if hf == 0:
        cb[:, 0, :] = tri
        cb[:, 1, :] = -BIG
    else:
        cb[:, 0, :] = 0.0
        cb[:, 1, :] = tri
    c['cbt'] = cb
    wb = np.zeros((128, 6, 128), f)
    for ii in range(6):
        diff = 128 * (4 + hf - ii) + q - key
        wb[:, ii, :] = np.where((diff >= 0) & (diff < 512), 0.0, -BIG)
    c['wbm'] = wb
    return c


def prep_shared(inp):
    L = 0
    f = np.float32
    sh = {}
    sh['w_ada'] = np.ascontiguousarray(inp['w_ada'][L], f)
    b_ada = np.asarray(inp['b_ada'][L], f)
    sh['b_adaT'] = np.ascontiguousarray(b_ada.reshape(48, 128).T)
    sh['b_ada_row'] = np.ascontiguousarray(b_ada.reshape(1, 6144))
    sh['g1T'] = np.ascontiguousarray(np.asarray(inp['g_norm1'][L], f).reshape(8, 128).T)
    sh['g2row'] = np.ascontiguousarray(np.asarray(inp['g_norm2'][L], f).reshape(1, 1024))
    sh['w_in'] = np.ascontiguousarray(inp['w_in'][L], f)
    def pl(a):
        return np.ascontiguousarray(a.reshape(16, 2, 64).transpose(1, 2, 0).reshape(128, 16))
    are = pl(np.asarray(inp['a_re'][L], f))
    aim = pl(np.asarray(inp['a_im'][L], f))
    ldt = pl(np.repeat(np.asarray(inp['log_dt'][L], f)[:, None], 64, axis=1))
    sh['s5_prm'] = np.ascontiguousarray(np.stack([are, aim, ldt], axis=1))
    Bl = np.zeros((128, 2, 16, 128), f)
    Cl = np.zeros((128, 2, 16, 128), f)
    b_re = np.asarray(inp['b_re'][L], f)
    b_im = np.asarray(inp['b_im'][L], f)
    c_re = np.asarray(inp['c_re'][L], f)
    c_im = np.asarray(inp['c_im'][L], f)
    for g in range(32):
        pt, g2, gl = g // 2, g % 2, g % 8
        Bl[gl * 16:(gl + 1) * 16, 0, pt, g2 * 64:(g2 + 1) * 64] = b_re[g].T
        Bl[gl * 16:(gl + 1) * 16, 1, pt, g2 * 64:(g2 + 1) * 64] = b_im[g].T
        Cl[g2 * 64:(g2 + 1) * 64, 0, pt, gl * 16:(gl + 1) * 16] = c_re[g].T
        Cl[g2 * 64:(g2 + 1) * 64, 1, pt, gl * 16:(gl + 1) * 16] = c_im[g].T
    sh['s5_B'] = Bl
    sh['s5_C'] = Cl
    sh['dskipT'] = np.ascontiguousarray(np.asarray(inp['d_skip'][L], f).reshape(4, 128).T)
    sh['w_glu'] = np.ascontiguousarray(inp['w_glu'][L], f)
    sh['b_gluT'] = np.ascontiguousarray(np.asarray(inp['b_glu'][L], f).reshape(4, 128).T)

    def dup(a):
        return np.concatenate([a, a])
    sh['gainT'] = np.ascontiguousarray(np.stack([dup(np.asarray(inp[k][L], f)) for k in ('q_gain', 'kc_gain', 'ks_gain', 'kw_gain')], axis=1))
    cstart = 16 * np.arange(256)
    sstart = 64 * np.arange(64)
    ov = ((cstart[:, None] < sstart[None, :] + 64) & (cstart[:, None] + 32 > sstart[None, :])).astype(f)
    ov[255] = 0
    sh['ovl'] = np.ascontiguousarray(ov.reshape(2, 128, 64).transpose(1, 0, 2))
    for k_ in ('w_up_attn', 'w_up_ssm', 'w_out', 'w_router', 'w_gate', 'w_up', 'w_down', 'ws_gate', 'ws_up', 'ws_down'):
        sh[k_] = np.ascontiguousarray(inp[k_][L], f)
    sh['rbias'] = np.ascontiguousarray(np.asarray(inp['router_bias'][L], f).reshape(1, 256))
    i_ = np.arange(128)
    sh['SU'] = np.ascontiguousarray((i_[:, None] < i_[None, :]).astype(f))
    sh['eCAP1'] = np.ascontiguousarray(np.broadcast_to((np.arange(256) * CAP + 1).astype(f)[None, :], (128, 256)))
    for nm in ('k', 'v'):
        pe = np.asarray(inp['pe_' + nm][L], f)
        sh['peS_' + nm] = np.ascontiguousarray(pe.reshape(16, 2, 64).transpose(1, 2, 0).reshape(128, 16))
        sh[f'w_cmp_{nm}1'] = np.ascontiguousarray(inp[f'w_cmp_{nm}1'][L], f)
        sh[f'w_cmp_{nm}2'] = np.ascontiguousarray(inp[f'w_cmp_{nm}2'][L], f)
    return sh


def core_inputs(inp, sh, b, hf, names):
    f = np.float32
    d = dict(sh)
    x = np.asarray(inp['x'][b], f)
    d['xf'] = x
    d['xo'] = np.ascontiguousarray(x.reshape(32, 128, 1024)[hf::2].reshape(SO, D))
    d['cT'] = np.ascontiguousarray(np.asarray(inp['c'][b], f).reshape(8, 128).T)
    d.update(host_consts(hf))
    return {k: d[k] for k in names}


_CACHE = {}


def kernel(**inputs):
    if 'prog' not in _CACHE:
        _CACHE['prog'] = build('all')
    nc, kb = _CACHE['prog']
    sh = prep_shared(inputs)
    names = list(kb.inputs.keys())
    in_maps = []
    for core in range(8):
        b, hf = core // 2, core % 2
        in_maps.append(core_inputs(inputs, sh, b, hf, names))
    res = run_bass_kernel_spmd(nc, in_maps, core_ids=list(range(8)))
    out = np.zeros((4, 32, 128, 1024), np.float32)
    for core in range(8):
        b, hf = core // 2, core % 2
        out[b, hf::2] = res.results[core]['out'].reshape(16, 128, 1024)
    return out.reshape(4, S, D)
```

```python
import numpy as np
from contextlib import ExitStack
import concourse.bass as bass
import concourse.mybir as mybir
from concourse.bass_utils import run_bass_kernel_spmd

F32 = mybir.dt.float32
BF16 = mybir.dt.bfloat16
I32 = mybir.dt.int32
ALU = mybir.AluOpType
AF = mybir.ActivationFunctionType
AX = mybir.AxisListType

S = 4096
D = 1024
NT = 32
NO = 16
SO = 2048
CAP = 192
NEXP = 256
BIG = 30000.0
EPS = 1e-6
CH = 256
NDS = 6
PI = 3.14159265358979
DBG_NJ = 0
NPRE_S5 = 0
NPRE_ATT = 64
NPRE_RT = 24
NPRE = NPRE_S5 + NPRE_ATT + NPRE_RT
DBG_NE = 0


class KB:
    def __init__(self, nc, es):
        self.nc = nc
        self.es = es
        self.E = {'pe': nc.tensor, 'act': nc.scalar, 'dve': nc.vector, 'pool': nc.gpsimd, 'sp': nc.sync}
        self.sem = {}
        self.cnt = {}
        for e in ['pe', 'act', 'dve', 'pool']:
            self.sem[e] = es.enter_context(nc.semaphore('sem_' + e))
            self.cnt[e] = 0
        self.dq = {}
        self.nds = {'sp': 16, 'pool': 24, 'act': 8}
        for q in ['sp', 'pool', 'act']:
            names = []
            for i in range(self.nds[q]):
                n = f'd_{q}{i}'
                self.sem[n] = es.enter_context(nc.semaphore(n))
                self.cnt[n] = 0
                names.append(n)
            self.dq[q] = names
        self.dqi = {q: 0 for q in self.dq}
        self.waited = {e: {} for e in self.E}
        self.lastw = {}
        self.readers = {}
        self.ninst = 0
        self.inputs = {}
        self.outputs = {}
        self.psb = [es.enter_context(nc.psum_tensor(f'psb{i}', [128, 512], F32)) for i in range(8)]

    def inp(self, name, shape, dt=F32):
        if name not in self.inputs:
            self.inputs[name] = self.nc.dram_tensor(name, list(shape), dt, kind="ExternalInput").ap()
        return self.inputs[name]

    def outp(self, name, shape, dt=F32):
        self.outputs[name] = self.nc.dram_tensor(name, list(shape), dt, kind="ExternalOutput").ap()
        return self.outputs[name]

    def scratch(self, name, shape, dt=F32):
        return self.nc.dram_tensor(name, list(shape), dt).ap()

    def sb(self, stack, name, shape, dt):
        self.uid = getattr(self, 'uid', 0) + 1
        return stack.enter_context(self.nc.sbuf_tensor(f'sb{self.uid}_' + name, list(shape), dt))

    def _deps(self, eng, r, w):
        deps = {}

        def need(name, val):
            if val > deps.get(name, 0):
                deps[name] = val
        for k in r:
            if k in self.lastw:
                need(*self.lastw[k])
        for k in w:
            if k in self.lastw:
                need(*self.lastw[k])
            for n, v in self.readers.get(k, {}).items():
                need(n, v)
        e = self.E[eng]
        for name, val in deps.items():
            if eng == 'pe' and name == 'pe':
                continue
            if self.waited[eng].get(name, 0) >= val:
                continue
            e.wait_ge(self.sem[name], val)
            self.waited[eng][name] = val
            self.ninst += 1

    def _record(self, sv, r, w):
        for k in w:
            self.lastw[k] = sv
            self.readers[k] = {}
        for k in r:
            d = self.readers.setdefault(k, {})
            if sv[1] > d.get(sv[0], 0):
                d[sv[0]] = sv[1]

    def op(self, eng, fn, r=(), w=()):
        self._deps(eng, r, w)
        inst = fn(self.E[eng])
        self.cnt[eng] += 1
        inst.then_inc(self.sem[eng], 1)
        self.ninst += 1
        self._record((eng, self.cnt[eng]), r, w)

    def dma(self, q, out, in_, r=(), w=(), fn=None):
        self._deps(q, r, w)
        names = self.dq[q]
        n = names[self.dqi[q] % self.nds[q]]
        self.dqi[q] += 1
        if self.cnt[n] > self.waited[q].get(n, 0):
            self.E[q].wait_ge(self.sem[n], self.cnt[n])
            self.waited[q][n] = self.cnt[n]
        if fn is None:
            inst = self.E[q].dma_start(out=out, in_=in_)
        else:
            inst = fn(self.E[q])
        self.cnt[n] += 16
        inst.then_inc(self.sem[n], 16)
        self.ninst += 1
        self._record((n, self.cnt[n]), r, w)

    def barrier(self):
        for eng in ['pe', 'act', 'dve', 'pool', 'sp']:
            e = self.E[eng]
            for name, val in self.cnt.items():
                if val > self.waited[eng].get(name, 0) and not (name == eng):
                    e.wait_ge(self.sem[name], val)
                    self.waited[eng][name] = val

    def finish(self):
        e = self.E['sp']
        for name, val in self.cnt.items():
            if val > self.waited['sp'].get(name, 0):
                e.wait_ge(self.sem[name], val)
                self.waited['sp'][name] = val

    def mm(self, out, lhsT, rhs, start, stop, r=(), w=(), sgc=False):
        self.op('pe', lambda e: e.matmul(out, lhsT=lhsT, rhs=rhs, start=start, stop=stop, skip_group_check=sgc), r=r, w=w)

    def tr(self, out, in_, ident, r=(), w=()):
        self.op('pe', lambda e: e.transpose(out=out, in_=in_, identity=ident), r=r, w=w)

    def act(self, out, in_, func, r=(), w=(), bias=None, scale=None, accum=None):
        kw = {}
        if bias is not None:
            kw['bias'] = bias
        if scale is not None:
            kw['scale'] = scale
        if accum is not None:
            kw['accum_out'] = accum
        self.op('act', lambda e: e.activation(out=out, in_=in_, func=func, **kw), r=r, w=w)

    def tt(self, eng, out, a, b, op, r=(), w=()):
        self.op(eng, lambda e: e.tensor_tensor(out=out, in0=a, in1=b, op=op), r=r, w=w)

    def ts(self, eng, out, a, s1, op0, s2=None, op1=None, r=(), w=(), accum=None):
        kw = {}
        if op1 is not None:
            kw['op1'] = op1
        if accum is not None:
            kw['accum_out'] = accum
        self.op(eng, lambda e: e.tensor_scalar(out=out, in0=a, scalar1=s1, scalar2=s2, op0=op0, **kw), r=r, w=w)

    def stt(self, out, a, s, b, op0, op1, r=(), w=(), accum=None):
        kw = {}
        if accum is not None:
            kw['accum_out'] = accum
        self.op('dve', lambda e: e.scalar_tensor_tensor(out=out, in0=a, scalar=s, in1=b, op0=op0, op1=op1, **kw), r=r, w=w)

    def cp(self, eng, out, in_, r=(), w=()):
        self.op(eng, lambda e: e.tensor_copy(out=out, in_=in_), r=r, w=w)

    def dump(self, name, ap, shape, key, dt=F32):
        o = self.outp('dbg_' + name, shape, dt)
        self.dma('sp', o, ap, r=[key])


def bc(ap, shape):
    return ap.to_broadcast(list(shape))


def build(stage='all', dbg=False):
    nc = bass.Bass("TRN2", target_bir_lowering=False)
    es = ExitStack()
    kb = KB(nc, es)
    P = es
    ps = kb.psb

    def st(x):
        order = ['p0', 'A', 'KV', 'S5', 'ATT', 'MERGE', 'MOE', 'all']
        return order.index(stage) >= order.index(x)

    ident = kb.sb(P, 'ident', [128, 128], F32)
    identb = kb.sb(P, 'identb', [128, 128], BF16)
    kb.op('pool', lambda e: e.memset(ident[:], 0.0), w=['ident'])
    kb.op('pool', lambda e: e.affine_select(out=ident[:], in_=ident[:], pattern=[[-1, 128]], compare_op=ALU.not_equal,
                                            fill=1.0, base=0, channel_multiplier=1), r=['ident'], w=['ident'])
    kb.cp('pool', identb[:], ident[:], r=['ident'], w=['identb'])
    hfm = kb.sb(P, 'hfm', [128, 2], F32)
    kb.dma('sp', hfm[:], kb.inp('hfm', [128, 2]), w=['hfm'])

    modT = kb.sb(P, 'modT', [128, 16], F32)
    a1T = kb.sb(P, 'a1T', [128, 8], F32)
    modrow_d = kb.scratch('modrow_d', [1, 4 * 1024], F32)
    a2row_d = kb.scratch('a2row_d', [1, 1024], F32)
    with ExitStack() as ph:
        modrow = kb.sb(ph, 'modrow', [128, 4, 1024], F32)
        a2row = kb.sb(ph, 'a2row', [128, 1024], F32)
        cT = kb.sb(ph, 'cT', [128, 8], F32)
        scb = kb.sb(ph, 'scb', [128, 8], BF16)
        scbb = kb.sb(ph, 'scbb', [128, 8, 128], BF16)
        badaT = kb.sb(ph, 'badaT', [128, 48], F32)
        badarow = kb.sb(ph, 'badarow', [128, 4096], F32)
        g1T = kb.sb(ph, 'g1T', [128, 8], F32)
        g2row = kb.sb(ph, 'g2row', [128, 1024], F32)
        wbf = [kb.sb(ph, f'wbf{i}', [128, 8, 512], BF16) for i in range(2)]
        kb.dma('sp', cT[:], kb.inp('cT', [128, 8]), w=['cT'])
        kb.dma('sp', badaT[:], kb.inp('b_adaT', [128, 48]), w=['badaT'])
        kb.dma('sp', badarow[:], kb.inp('b_ada_row', [1, 6144])[0:1, 2048:6144].partition_broadcast(128), w=['badarow'])
        kb.dma('sp', g1T[:], kb.inp('g1T', [128, 8]), w=['g1T'])
        kb.dma('sp', g2row[:], kb.inp('g2row', [1, 1024])[0:1, :].partition_broadcast(128), w=['g2row'])
        kb.act(scb[:], cT[:], AF.Silu, r=['cT'], w=['scb'])
        kb.cp('dve', scbb[:], bc(scb[:].unsqueeze(2), [128, 8, 128]), r=['scb'], w=['scbb'])
        w_ada = kb.inp('w_ada', [1024, 6144])
        for n in range(12):
            b = n % 2
            kb.dma('pool', wbf[b][:], w_ada[:, n * 512:(n + 1) * 512].rearrange("(kc p) n -> p kc n", p=128), w=[f'wbf{b}'])
            if n < 4:
                for ct in range(4):
                    idx = n * 4 + ct
                    for kc in range(8):
                        kb.mm(ps[7][:, idx:idx + 1], wbf[b][:, kc, ct * 128:(ct + 1) * 128], scb[:, kc:kc + 1],
                              kc == 0, kc == 7, r=[f'wbf{b}', 'scb'], w=['ps7'])
                if n == 3:
                    kb.tt('dve', modT[:], ps[7][:, 0:16], badaT[:, 0:16], ALU.add, r=['ps7', 'badaT'], w=['modT'])
            else:
                pb = n % 2
                for kc in range(8):
                    kb.mm(ps[pb][:, :], scbb[:, kc, :], wbf[b][:, kc, :], kc == 0, kc == 7, r=[f'wbf{b}', 'scbb'], w=[f'ps{pb}'])
                sec = n // 2 - 2
                half = n % 2
                kb.tt('dve', modrow[:, sec, half * 512:(half + 1) * 512], ps[pb][:, :], badarow[:, (n - 4) * 512:(n - 3) * 512],
                      ALU.add, r=[f'ps{pb}', 'badarow'], w=['modrow'])
        kb.stt(a1T[:], modT[:, 8:16], 1.0, g1T[:], ALU.add, ALU.mult, r=['modT', 'g1T'], w=['a1T'])
        kb.stt(a2row[:], modrow[:, 2, :], 1.0, g2row[:], ALU.add, ALU.mult, r=['modrow', 'g2row'], w=['a2row'])
        kb.dma('sp', modrow_d[0:1, :], modrow[0:1, :, :].rearrange("p a b -> p (a b)"), r=['modrow'], w=['modrow_d'])
        kb.dma('sp', a2row_d[0:1, :], a2row[0:1, :], r=['a2row'], w=['a2row_d'])
        if dbg and stage == 'p0':
            kb.dump('modT', modT[:], [128, 16], 'modT')
            kb.dump('modrow', modrow[:], [128, 4, 1024], 'modrow')
            kb.dump('a1T', a1T[:], [128, 8], 'a1T')
        kb.barrier()
    if not st('A'):
        kb.finish()
        es.close()
        return nc, kb

    def run_interleaved(gens, width):
        active = []
        gens = list(gens)
        gi = 0
        while gi < len(gens) or active:
            while len(active) < width and gi < len(gens):
                active.append(gens[gi])
                gi += 1
            for g in list(active):
                try:
                    next(g)
                except StopIteration:
                    active.remove(g)

    def norm_transpose(xd, ntiles, hT, hkey, ph):
        xt = [kb.sb(ph, f'nt_x{i}', [128, 1024], F32) for i in range(2)]
        junk = [kb.sb(ph, f'nt_junk{i}', [128, 1024], BF16) for i in range(2)]
        ss = [kb.sb(ph, f'nt_ss{i}', [128, 4], F32) for i in range(2)]

        def tile_gen(t):
            b = t % 2
            B = str(b)
            kb.dma('sp', xt[b][:], xd[t * 128:(t + 1) * 128, :], w=['nt_x' + B])
            kb.stt(junk[b][:], xt[b][:], 1.0, xt[b][:], ALU.mult, ALU.mult, r=['nt_x' + B], w=['nt_junk' + B, 'nt_ss0' + B], accum=ss[b][:, 0:1])
            yield
            kb.act(ss[b][:, 1:2], ss[b][:, 0:1], AF.Sqrt, r=['nt_ss0' + B], w=['nt_ss1' + B], scale=1.0 / D, bias=EPS)
            yield
            kb.op('dve', lambda e: e.reciprocal(out=ss[b][:, 2:3], in_=ss[b][:, 1:2]), r=['nt_ss1' + B], w=['nt_ss2' + B])
            yield
            kb.ts('dve', xt[b][:], xt[b][:], ss[b][:, 2:3], ALU.mult, r=['nt_x' + B, 'nt_ss2' + B], w=['nt_x' + B])
            yield
            for half in range(2):
                pb = 4 + 2 * b + half
                for q in range(4):
                    kc = half * 4 + q
                    kb.tr(ps[pb][:, q * 128:(q + 1) * 128], xt[b][:, kc * 128:(kc + 1) * 128], ident[:],
                          r=['nt_x' + B, 'ident'], w=[f'ps{pb}'])
                yield
                for q in range(4):
                    kc = half * 4 + q
                    kb.act(hT[:, kc, t * 128:(t + 1) * 128], ps[pb][:, q * 128:(q + 1) * 128], AF.Identity,
                           r=[f'ps{pb}', 'a1T', 'modT'], w=[f'{hkey}_{t}_{kc}'], scale=a1T[:, kc:kc + 1], bias=modT[:, kc:kc + 1])
                yield
        for t0_ in range(0, ntiles, 2):
            run_interleaved([tile_gen(t0_), tile_gen(t0_ + 1)], 2)

    def load_w(dst, dkey, src_ap):
        kb.dma('pool', dst, src_ap.rearrange("(kc p) n -> p kc n", p=128), w=[dkey])

    w_in = kb.inp('w_in', [1024, 3864])
    OQ, OKC, OVC, OKS, OVS, OKW, OVW, OGN, OU, OGA, OGS = 0, 512, 640, 768, 896, 1024, 1152, 1280, 1304, 1816, 2840

    pre_on = st('MOE') and NPRE > 0
    if pre_on:
        w_gate_d = kb.inp('w_gate', [NEXP, 1024, 256])
        w_up_d = kb.inp('w_up', [NEXP, 1024, 256])
        w_down_d = kb.inp('w_down', [NEXP, 256, 1024])
        wpre = [kb.scratch(f'wpre{t}', [NPRE, 128, 2048], BF16) for t in range(3)]
    else:
        wpre = None

    def make_pre_gen(stack, tag, e0, ne, depth, cast_eng, pcols):
        pst = [kb.sb(stack, f'pst{tag}{t}', [128, pcols], F32) for t in range(depth)]
        pbf = [kb.sb(stack, f'pbf{tag}{t}', [128, pcols], BF16) for t in range(depth)]
        ppw = 2048 // pcols
        ppe = 3 * ppw

        def pre_src(n):
            e = e0 + n // ppe
            t = n % ppe
            w, hh = t // ppw, t % ppw
            c0 = hh * pcols
            if w == 0:
                return w_gate_d[e].rearrange("(p r) n -> p (r n)", r=8)[:, c0:c0 + pcols]
            if w == 1:
                return w_up_d[e].rearrange("(p r) n -> p (r n)", r=8)[:, c0:c0 + pcols]
            kc, cc = c0 // 1024, c0 % 1024
            return w_down_d[e][kc * 128:(kc + 1) * 128, cc:cc + pcols]

        def pre_load(n):
            s_ = n % depth
            kb.dma('sp', pst[s_][:], pre_src(n), w=[f'pst{tag}{s_}'])

        def gen():
            npc = ppe * ne
            for n in range(min(depth, npc)):
                pre_load(n)
            yield
            for n in range(npc):
                s_ = n % depth
                e = e0 + n // ppe
                t = n % ppe
                w, hh = t // ppw, t % ppw
                if cast_eng == 'act':
                    kb.act(pbf[s_][:], pst[s_][:], AF.Copy, r=[f'pst{tag}{s_}'], w=[f'pbf{tag}{s_}'])
                else:
                    kb.cp(cast_eng, pbf[s_][:], pst[s_][:], r=[f'pst{tag}{s_}'], w=[f'pbf{tag}{s_}'])
                kb.dma('sp', wpre[w][e][:, hh * pcols:(hh + 1) * pcols], pbf[s_][:], r=[f'pbf{tag}{s_}'], w=[])
                if n + depth < npc:
                    pre_load(n + depth)
                yield
        return gen()

    def finish_now():
        kb.barrier()
        kb.finish()
        es.close()
        return nc, kb

    y_ssmT = kb.sb(P, 'y_ssmT', [128, 4, SO], BF16)
    onesblk = kb.sb(P, 'onesblk', [128, 128], BF16)
    kb.op('pool', lambda e: e.memset(onesblk[:], 0.0), w=['onesblk'])
    kb.op('pool', lambda e: e.memset(onesblk[0:64, 0:64], 1.0), r=['onesblk'], w=['onesblk'])
    kb.op('pool', lambda e: e.memset(onesblk[64:128, 64:128], 1.0), r=['onesblk'], w=['onesblk'])
    gainT = kb.sb(P, 'gainT', [128, 4], F32)
    kb.dma('sp', gainT[:], kb.inp('gainT', [128, 4]), w=['gainT'])

    attK = ExitStack()
    ksT = kb.sb(attK, 'ksT', [128, S], BF16)
    kwT = kb.sb(attK, 'kwT', [128, S], BF16)
    VS = kb.sb(attK, 'VS', [128, NT, 2, 65], BF16)
    VW = kb.sb(attK, 'VW', [128, NT, 2, 65], BF16)
    kcn = kb.sb(attK, 'kcn', [128, 256], BF16)
    VOX = kb.sb(attK, 'VOX', [128, 2, 2, 129], BF16)
    phU = ExitStack()
    uT = kb.sb(phU, 'uT', [128, 4, S], BF16)

    def fm_norm(psrc, pskey, n, dst, dkey, gcol, tmp, sc_scale, sc_bias, pbn):
        kb.act(tmp['raw'][:, 0:n], psrc, AF.Copy, r=[pskey], w=['fn_raw'])
        kb.act(tmp['sq'][:, 0:n], psrc, AF.Square, r=[pskey], w=['fn_sq'])
        kb.mm(ps[pbn][:, 0:n], onesblk[:, :], tmp['sq'][:, 0:n], True, True, r=['onesblk', 'fn_sq'], w=[f'ps{pbn}'])
        kb.act(tmp['rs'][:, 0:n], ps[pbn][:, 0:n], AF.Sqrt, r=[f'ps{pbn}'], w=['fn_rs'], scale=sc_scale, bias=sc_bias)
        kb.op('dve', lambda e: e.reciprocal(out=tmp['rs'][:, 0:n], in_=tmp['rs'][:, 0:n]), r=['fn_rs'], w=['fn_rs'])
        kb.stt(dst, tmp['raw'][:, 0:n], gainT[:, gcol:gcol + 1], tmp['rs'][:, 0:n], ALU.mult, ALU.mult,
               r=['fn_raw', 'fn_rs', 'gainT'], w=[dkey])

    with ExitStack() as phA:
        hT = kb.sb(phA, 'hT', [128, 8, S], BF16)
        with ExitStack() as ph:
            norm_transpose(kb.inp('xf', [S, D]), NT, hT, 'hT', ph)
            kb.barrier()
        if dbg and stage == 'A':
            kb.dump('hT', hT[:], [128, 8, S], 'hT', BF16)
        if not st('KV'):
            return finish_now()
        with ExitStack() as ph:
            wb_ = kb.sb(ph, 'kv_wb', [128, 8, 512], BF16)
            tmp = {'raw': kb.sb(ph, 'fn_raw', [128, 512], F32), 'sq': kb.sb(ph, 'fn_sq', [128, 512], BF16),
                   'rs': kb.sb(ph, 'fn_rs', [128, 512], F32)}
            load_w(wb_[:], 'kv_wb', w_in[:, OU:OU + 512])
            for mt in range(4):
                for c in range(8):
                    pb = c % 2
                    for kc in range(8):
                        kb.mm(ps[pb][:, :], wb_[:, kc, mt * 128:(mt + 1) * 128], hT[:, kc, c * 512:(c + 1) * 512], kc == 0, kc == 7,
                              r=['kv_wb', 'hT'], w=[f'ps{pb}'])
                    kb.act(uT[:, mt, c * 512:(c + 1) * 512], ps[pb][:, :], AF.Copy, r=[f'ps{pb}'], w=['uT'])
            load_w(wb_[:], 'kv_wb', w_in[:, OKS:OKS + 512])
            for (off, dst, dkey, gcol) in ((0, ksT, 'ksT', 2), (256, kwT, 'kwT', 3)):
                for c in range(8):
                    pb = c % 2
                    for kc in range(8):
                        kb.mm(ps[pb][:, :], wb_[:, kc, off:off + 128], hT[:, kc, c * 512:(c + 1) * 512], kc == 0, kc == 7,
                              r=['kv_wb', 'hT'], w=[f'ps{pb}'])
                    fm_norm(ps[pb][:, :], f'ps{pb}', 512, dst[:, c * 512:(c + 1) * 512], dkey, gcol, tmp, 1.0 / 64, EPS, 2 + pb)
            kb.op('pool', lambda e: e.memset(VS[:, :, :, 64:65], 1.0), w=['VS'])
            kb.op('pool', lambda e: e.memset(VW[:, :, :, 64:65], 1.0), w=['VW'])
            for t in range(NT):
                pb = t % 2
                for (off, dst, dkey, col0) in ((128, VS, 'VS', 0), (384, VW, 'VW', 128)):
                    for kc in range(8):
                        kb.mm(ps[pb][:, col0:col0 + 128], hT[:, kc, t * 128:(t + 1) * 128], wb_[:, kc, off:off + 128], kc == 0, kc == 7,
                              r=['kv_wb', 'hT'], w=[f'ps{pb}'], sgc=(col0 > 0))
                kb.act(VS[:, t, :, 0:64], ps[pb][:, 0:128].rearrange("p (g d) -> p g d", g=2), AF.Copy, r=[f'ps{pb}'], w=['VS'])
                kb.act(VW[:, t, :, 0:64], ps[pb][:, 128:256].rearrange("p (g d) -> p g d", g=2), AF.Copy, r=[f'ps{pb}'], w=['VW'])
            kcS = kb.sb(ph, 'kcS', [128, S], BF16)
            w2b = kb.sb(ph, 'w2b', [128, 2, 64], BF16)
            peS = kb.sb(ph, 'peS', [128, 16], F32)
            peb = kb.sb(ph, 'peb', [128, 16], BF16)
            hb = kb.sb(ph, 'hb', [128, 2], F32)
            hid = kb.sb(ph, 'hid', [128, 2, 256], BF16)
            wkc = kb.sb(ph, 'wkc', [128, 8, 256], BF16)
            load_w(wkc[:], 'wkc', w_in[:, OKC:OKC + 256])
            kb.op('pool', lambda e: e.memset(kcn[:], 0.0), w=['kcn'])
            kb.op('pool', lambda e: e.memset(VOX[:], 0.0), w=['VOX'])
            ovl = kb.sb(ph, 'ovl', [128, 2, 64], F32)
            kb.dma('sp', ovl[:], kb.inp('ovl', [128, 2, 64]), w=['ovl'])
            for g in range(2):
                kb.cp('pool', VOX[:, :, g, 65:129], ovl[:], r=['ovl', 'VOX'], w=['VOX'])
            kb.op('pool', lambda e: e.memset(VOX[:, 0, :, 64:65], 1.0), r=['VOX'], w=['VOX'])
            kb.op('pool', lambda e: e.memset(VOX[0:127, 1, :, 64:65], 1.0), r=['VOX'], w=['VOX'])
            wb16 = wb_[:].rearrange("p a (b c) -> p (a b) c", b=2)
            for kv in range(2):
                nm = ['k', 'v'][kv]
                kb.dma('pool', wb16, kb.inp(f'w_cmp_{nm}1', [2048, 256]).rearrange("(lp p) n -> p lp n", p=128), w=['kv_wb'])
                kb.dma('pool', w2b[:], kb.inp(f'w_cmp_{nm}2', [256, 64]).rearrange("(m p) n -> p m n", p=128), w=['w2b'])
                kb.dma('sp', peS[:], kb.inp(f'peS_{nm}', [128, 16]), w=['peS'])
                kb.cp('pool', peb[:], peS[:], r=['peS'], w=['peb'])
                for m in range(2):
                    for lp in range(16):
                        kb.mm(ps[6][:, m:m + 1], wb16[:, lp, m * 128:(m + 1) * 128], peb[:, lp:lp + 1], lp == 0, lp == 15,
                              r=['kv_wb', 'peb'], w=['ps6'])
                kb.cp('dve', hb[:], ps[6][:, 0:2], r=['ps6'], w=['hb'])
                for g in range(2):
                    wcol = kv * 128 + g * 64
                    kb.op('pool', lambda e: e.memset(kcS[64:128, S - 1:S], 0.0), w=['kcS'])
                    for c in range(8):
                        pb = c % 2
                        n2 = 512 if c < 7 else 511
                        for kc in range(8):
                            kb.mm(ps[pb][0:64, :], wkc[:, kc, wcol:wcol + 64], hT[:, kc, c * 512:(c + 1) * 512], kc == 0, kc == 7,
                                  r=['wkc', 'hT'], w=[f'ps{pb}'])
                        for kc in range(8):
                            kb.mm(ps[pb][64:128, 0:n2], wkc[:, kc, wcol:wcol + 64], hT[:, kc, c * 512 + 1:c * 512 + 1 + n2], kc == 0, kc == 7,
                                  r=['wkc', 'hT'], w=[f'ps{pb}'])
                        kb.act(kcS[0:64, c * 512:(c + 1) * 512], ps[pb][0:64, :], AF.Copy, r=[f'ps{pb}'], w=['kcS'])
                        kb.act(kcS[64:128, c * 512:c * 512 + n2], ps[pb][64:128, 0:n2], AF.Copy, r=[f'ps{pb}'], w=['kcS'])
                    for m in range(2):
                        pb = 2 + m
                        for lp in range(16):
                            kb.mm(ps[pb][:, 0:255], wb16[:, lp, m * 128:(m + 1) * 128], kcS[:, 2 * lp:2 * lp + 16 * 254 + 1:16], lp == 0, lp == 15,
                                  r=['kv_wb', 'kcS'], w=[f'ps{pb}'])
                        kb.act(hid[:, m, 0:255], ps[pb][:, 0:255], AF.Gelu_apprx_tanh, r=[f'ps{pb}', 'hb'], w=['hid'], bias=hb[:, m:m + 1])
                    if kv == 0:
                        for m in range(2):
                            kb.mm(ps[4][g * 64:(g + 1) * 64, 0:255], w2b[:, m, :], hid[:, m, 0:255], m == 0, m == 1, r=['w2b', 'hid'], w=['ps4'])
                    else:
                        for bt in range(2):
                            nb = 128 if bt == 0 else 127
                            for m in range(2):
                                kb.mm(ps[4][0:nb, bt * 64:(bt + 1) * 64], hid[:, m, bt * 128:bt * 128 + nb], w2b[:, m, :], m == 0, m == 1,
                                      r=['w2b', 'hid'], w=['ps4'], sgc=(bt == 1))
                        kb.act(VOX[:, 0, g, 0:64], ps[4][:, 0:64], AF.Copy, r=['ps4'], w=['VOX'])
                        kb.act(VOX[0:127, 1, g, 0:64], ps[4][0:127, 64:128], AF.Copy, r=['ps4'], w=['VOX'])
                if kv == 0:
                    fm_norm(ps[4][:, 0:255], 'ps4', 255, kcn[:, 0:255], 'kcn', 1, tmp, 1.0 / 64, EPS, 5)
            if dbg and stage == 'KV':
                kb.dump('uT', uT[:], [128, 4, S], 'uT', BF16)
                kb.dump('ksT', ksT[:], [128, S], 'ksT', BF16)
                kb.dump('kwT', kwT[:], [128, S], 'kwT', BF16)
                kb.dump('VS', VS[:], [128, NT, 2, 65], 'VS', BF16)
                kb.dump('kcn', kcn[:], [128, 256], 'kcn', BF16)
                kb.dump('VOX', VOX[:], [128, 2, 2, 129], 'VOX', BF16)
            kb.barrier()
    if not st('S5'):
        return finish_now()
    with ExitStack() as ph:
        prm = kb.sb(ph, 's5_prm', [128, 3, 16], F32)
        kb.dma('sp', prm[:], kb.inp('s5_prm', [128, 3, 16]), w=['prm'])
        are, aim, ldt = prm[:, 0, :], prm[:, 1, :], prm[:, 2, :]
        sc = kb.sb(ph, 's5_sc', [128, 24, 16], F32)
        DT, RHO, TH, C0, S0, T1, T2, T3, FRE, FIM, ERE, EIM, NR, NI, DEN, MFIM = range(16)
        K = 's5sc'
        kb.act(sc[:, DT, :], ldt, AF.Exp, r=['prm'], w=[K])
        kb.tt('dve', sc[:, T1, :], are, sc[:, DT, :], ALU.mult, r=['prm', K], w=[K])
        kb.act(sc[:, RHO, :], sc[:, T1, :], AF.Exp, r=[K], w=[K])
        kb.tt('dve', sc[:, TH, :], aim, sc[:, DT, :], ALU.mult, r=['prm', K], w=[K])
        hpi = kb.sb(ph, 's5_hpi', [128, 1], F32)
        kb.op('dve', lambda e: e.memset(hpi[:], PI / 2), w=['hpi'])
        kb.act(sc[:, S0, :], sc[:, TH, :], AF.Sin, r=[K], w=[K], scale=1.0 / 16)
        kb.act(sc[:, C0, :], sc[:, TH, :], AF.Sin, r=[K, 'hpi'], w=[K], scale=1.0 / 16, bias=hpi[:, 0:1])

        def csq(cre, cim):
            kb.tt('dve', sc[:, T1, :], sc[:, cre, :], sc[:, cre, :], ALU.mult, r=[K], w=[K])
            kb.tt('dve', sc[:, T2, :], sc[:, cim, :], sc[:, cim, :], ALU.mult, r=[K], w=[K])
            kb.tt('dve', sc[:, T3, :], sc[:, cre, :], sc[:, cim, :], ALU.mult, r=[K], w=[K])
            kb.tt('dve', sc[:, cre, :], sc[:, T1, :], sc[:, T2, :], ALU.subtract, r=[K], w=[K])
            kb.ts('dve', sc[:, cim, :], sc[:, T3, :], 2.0, ALU.mult, r=[K], w=[K])
        for _ in range(4):
            csq(C0, S0)
        kb.tt('dve', sc[:, NR, :], sc[:, RHO, :], sc[:, C0, :], ALU.mult, r=[K], w=[K])
        kb.ts('dve', sc[:, NR, :], sc[:, NR, :], -1.0, ALU.add, r=[K], w=[K])
        kb.tt('dve', sc[:, NI, :], sc[:, RHO, :], sc[:, S0, :], ALU.mult, r=[K], w=[K])
        kb.tt('dve', sc[:, T1, :], are, are, ALU.mult, r=['prm'], w=[K])
        kb.tt('dve', sc[:, T2, :], aim, aim, ALU.mult, r=['prm'], w=[K])
        kb.tt('dve', sc[:, DEN, :], sc[:, T1, :], sc[:, T2, :], ALU.add, r=[K], w=[K])
        kb.op('dve', lambda e: e.reciprocal(out=sc[:, DEN, :], in_=sc[:, DEN, :]), r=[K], w=[K])
        kb.tt('dve', sc[:, T1, :], sc[:, NR, :], are, ALU.mult, r=[K, 'prm'], w=[K])
        kb.tt('dve', sc[:, T2, :], sc[:, NI, :], aim, ALU.mult, r=[K, 'prm'], w=[K])
        kb.tt('dve', sc[:, T1, :], sc[:, T1, :], sc[:, T2, :], ALU.add, r=[K], w=[K])
        kb.tt('dve', sc[:, FRE, :], sc[:, T1, :], sc[:, DEN, :], ALU.mult, r=[K], w=[K])
        kb.tt('dve', sc[:, T1, :], sc[:, NI, :], are, ALU.mult, r=[K, 'prm'], w=[K])
        kb.tt('dve', sc[:, T2, :], sc[:, NR, :], aim, ALU.mult, r=[K, 'prm'], w=[K])
        kb.tt('dve', sc[:, T1, :], sc[:, T1, :], sc[:, T2, :], ALU.subtract, r=[K], w=[K])
        kb.tt('dve', sc[:, FIM, :], sc[:, T1, :], sc[:, DEN, :], ALU.mult, r=[K], w=[K])
        kb.ts('dve', sc[:, MFIM, :], sc[:, FIM, :], -1.0, ALU.mult, r=[K], w=[K])
        cs3 = kb.sb(ph, 's5_cs3', [128, 16, 3 * CH], BF16)
        cpo = kb.sb(ph, 's5_cpo', [128, 16, 128], BF16)
        spo = kb.sb(ph, 's5_spo', [128, 16, 128], BF16)
        Bbf = kb.sb(ph, 's5_Bbf', [128, 2, 16, 128], BF16)
        CA = kb.sb(ph, 's5_CA', [128, 16, 128], BF16)
        CBm = kb.sb(ph, 's5_CB', [128, 16, 128], BF16)
        with ExitStack() as ph2:
            cpF = kb.sb(ph2, 's5_cpF', [128, 16, CH], F32)
            spF = kb.sb(ph2, 's5_spF', [128, 16, CH], F32)
            tA = kb.sb(ph2, 's5_tA', [128, 16, CH // 2], F32)
            tB = kb.sb(ph2, 's5_tB', [128, 16, CH // 2], F32)
            kb.op('pool', lambda e: e.memset(cpF[:, :, 0:1], 1.0), w=['cpF'])
            kb.op('pool', lambda e: e.memset(spF[:, :, 0:1], 0.0), w=['spF'])
            kb.cp('dve', sc[:, ERE, :], sc[:, C0, :], r=[K], w=[K])
            kb.cp('dve', sc[:, EIM, :], sc[:, S0, :], r=[K], w=[K])
            k = 1
            while k < CH:
                eb_re = bc(sc[:, ERE, :].unsqueeze(2), [128, 16, k])
                eb_im = bc(sc[:, EIM, :].unsqueeze(2), [128, 16, k])
                kb.tt('dve', tA[:, :, 0:k], cpF[:, :, 0:k], eb_re, ALU.mult, r=['cpF', K], w=['tA'])
                kb.tt('dve', tB[:, :, 0:k], spF[:, :, 0:k], eb_im, ALU.mult, r=['spF', K], w=['tB'])
                kb.tt('dve', cpF[:, :, k:2 * k], tA[:, :, 0:k], tB[:, :, 0:k], ALU.subtract, r=['tA', 'tB'], w=['cpF'])
                kb.tt('dve', tA[:, :, 0:k], spF[:, :, 0:k], eb_re, ALU.mult, r=['spF', K], w=['tA'])
                kb.tt('dve', tB[:, :, 0:k], cpF[:, :, 0:k], eb_im, ALU.mult, r=['cpF', K], w=['tB'])
                kb.tt('dve', spF[:, :, k:2 * k], tA[:, :, 0:k], tB[:, :, 0:k], ALU.add, r=['tA', 'tB'], w=['spF'])
                csq(ERE, EIM)
                k *= 2
            kb.cp('dve', cs3[:, :, 0:CH], cpF[:], r=['cpF'], w=['cs3a'])
            kb.act(cs3[:, :, CH:2 * CH], spF[:], AF.Copy, r=['spF'], w=['cs3b'])
            kb.cp('dve', cs3[:, :, 2 * CH:3 * CH], cpF[:], r=['cpF'], w=['cs3c'])
            for (dst, src, kd, ks_) in ((cpo, cpF, 'cpo', 'cpF'), (spo, spF, 'spo', 'spF')):
                kb.ts('dve', tA[:], src[:, :, 0:128], hfm[:, 0:1], ALU.mult, r=[ks_, 'hfm'], w=['tA'])
                kb.stt(dst[:], src[:, :, 128:256], hfm[:, 1:2], tA[:], ALU.mult, ALU.add, r=[ks_, 'hfm', 'tA'], w=[kd])
            kb.barrier()
        with ExitStack() as ph2:
            Bst = kb.sb(ph2, 's5_Bst', [128, 2, 16, 128], F32)
            kb.dma('sp', Bst[:], kb.inp('s5_B', [128, 2, 16, 128]), w=['Bst'])
            kb.cp('pool', Bbf[:], Bst[:], r=['Bst'], w=['Bbf'])
            Cst = kb.sb(ph2, 's5_Cst', [128, 2, 16, 128], F32)
            kb.dma('sp', Cst[:], kb.inp('s5_C', [128, 2, 16, 128]), w=['Cst'])
            tC = kb.sb(ph2, 's5_tC', [128, 16, 128], F32)
            tD = kb.sb(ph2, 's5_tD', [128, 16, 128], F32)
            fre_b = bc(sc[:, FRE, :].unsqueeze(2), [128, 16, 128])
            fim_b = bc(sc[:, FIM, :].unsqueeze(2), [128, 16, 128])
            mfim_b = bc(sc[:, MFIM, :].unsqueeze(2), [128, 16, 128])
            kb.tt('dve', tC[:], Cst[:, 0, :, :], fre_b, ALU.mult, r=['Cst', K], w=['tC'])
            kb.tt('dve', tD[:], Cst[:, 1, :, :], fim_b, ALU.mult, r=['Cst', K], w=['tD'])
            kb.tt('dve', CA[:], tC[:], tD[:], ALU.subtract, r=['tC', 'tD'], w=['CA'])
            kb.tt('dve', tC[:], Cst[:, 0, :, :], mfim_b, ALU.mult, r=['Cst', K], w=['tC'])
            kb.tt('dve', tD[:], Cst[:, 1, :, :], fre_b, ALU.mult, r=['Cst', K], w=['tD'])
            kb.tt('dve', CBm[:], tC[:], tD[:], ALU.subtract, r=['tC', 'tD'], w=['CB'])
            kb.barrier()
        dsk = kb.sb(ph, 's5_dsk', [128, 4], F32)
        kb.dma('sp', dsk[:], kb.inp('dskipT', [128, 4]), w=['dsk'])
        zT = kb.sb(ph, 's5_zT', [128, 4, SO], BF16)
        ph3 = ExitStack()
        init = kb.sb(ph3, 's5_init', [128, 2, 16], F32)
        kb.op('pool', lambda e: e.memset(init[:], 0.0), w=['init'])
        NA = 3
        tA4 = [kb.sb(ph3, f's5_tA4{i}', [128, 2, 2 * CH], BF16) for i in range(NA)]
        bub = [kb.sb(ph3, f's5_bub{i}', [128, 2 * CH], BF16) for i in range(NA)]
        dre = [kb.sb(ph3, f's5_dre{i}', [128, CH], F32) for i in range(3)]
        dim_ = [kb.sb(ph3, f's5_dim{i}', [128, CH], F32) for i in range(3)]
        w2 = [kb.sb(ph3, f's5_w2{i}', [128, 2, CH], F32) for i in range(2)]
        tmpb = [kb.sb(ph3, f's5_tmpb{i}', [128, 2, 128], F32) for i in range(2)]
        wlast = kb.sb(ph3, 's5_wlast', [128, 2, 16], F32)
        woall = kb.sb(ph3, 's5_woall', [128, 2, 16, 128], BF16)
        rt = [kb.sb(ph3, f's5_rt{i}', [128, 4, 128], BF16) for i in range(8)]
        ct = kb.sb(ph3, 's5_ct', [128, 4, 16], F32)
        zre = kb.sb(ph3, 's5_zre', [128, 16, 128], BF16)
        zim = kb.sb(ph3, 's5_zim', [128, 16, 128], BF16)
        uo = kb.sb(ph3, 's5_uo', [128, 4, 128], F32)
        ytmp = kb.sb(ph3, 's5_ytmp', [128, 128], F32)
        if pre_on and NPRE_S5 > 0:
            pgen5 = make_pre_gen(ph3, 's', 0, NPRE_S5, 3, 'act', 512)
        else:
            pgen5 = iter(())
        its = [(c, pt) for c in range(S // CH) for pt in range(16)]
        nit = len(its)

        def stA(i):
            c, pt = its[i]
            t0 = c * CH
            pb = i % 3
            a = i % NA
            kt = pt // 4
            kb.mm(ps[pb][:, 0:CH], Bbf[:, 0, pt, :], uT[:, kt, t0:t0 + CH], True, True, r=['Bbf', 'uT'], w=[f'ps{pb}'])
            kb.mm(ps[pb][:, CH:2 * CH], Bbf[:, 1, pt, :], uT[:, kt, t0:t0 + CH], False, True, r=['Bbf', 'uT'], w=[f'ps{pb}'], sgc=True)
            kb.act(bub[a][:], ps[pb][:, :], AF.Copy, r=[f'ps{pb}'], w=[f'bub{a}'])
            kb.tt('dve', tA4[a][:, 0, :], bub[a][:], cs3[:, pt, 0:2 * CH], ALU.mult, r=[f'bub{a}', 'cs3a', 'cs3b'], w=[f'tA4{a}'])
            kb.tt('dve', tA4[a][:, 1, :], bub[a][:], cs3[:, pt, CH:3 * CH], ALU.mult, r=[f'bub{a}', 'cs3b', 'cs3c'], w=[f'tA4{a}'])

        def stB(i):
            a = i % NA
            d = i % 3
            kb.tt('pool', dre[d][:], tA4[a][:, 0, 0:CH], tA4[a][:, 0, CH:2 * CH], ALU.add, r=[f'tA4{a}'], w=[f'dre{d}'])
            kb.tt('pool', dim_[d][:], tA4[a][:, 1, CH:2 * CH], tA4[a][:, 1, 0:CH], ALU.subtract, r=[f'tA4{a}'], w=[f'dim{d}'])

        def stC1(i):
            c, pt = its[i]
            d = i % 3
            b = i % 2
            rho_b = bc(sc[:, RHO, pt:pt + 1], [128, CH])
            kb.op('dve', lambda e: e.tensor_tensor_scan(out=w2[b][:, 0, :], data0=rho_b, data1=dre[d][:], initial=init[:, 0, pt:pt + 1],
                                                        op0=ALU.mult, op1=ALU.add), r=[f'dre{d}', K, 'init'], w=[f'w2{b}'])
            kb.op('dve', lambda e: e.tensor_tensor_scan(out=w2[b][:, 1, :], data0=rho_b, data1=dim_[d][:], initial=init[:, 1, pt:pt + 1],
                                                        op0=ALU.mult, op1=ALU.add), r=[f'dim{d}', K, 'init'], w=[f'w2{b}'])
            kb.act(tmpb[b][:], w2[b][:, :, 0:128], AF.Copy, r=[f'w2{b}', 'hfm'], w=[f'tmpb{b}'], scale=hfm[:, 0:1])
            kb.act(wlast[:, :, pt], w2[b][:, :, CH - 1], AF.Copy, r=[f'w2{b}'], w=['wlast'])

        def stC2(i):
            c, pt = its[i]
            b = i % 2
            kb.stt(woall[:, :, pt, :], w2[b][:, :, 128:256], hfm[:, 1:2], tmpb[b][:], ALU.mult, ALU.add,
                   r=[f'w2{b}', 'hfm', f'tmpb{b}'], w=['woall'])

        def chunk_end(c):
            t0 = c * CH
            kb.tt('dve', ct[:, 0, :], wlast[:, 0, :], sc[:, ERE, :], ALU.mult, r=['wlast', K], w=['ct'])
            kb.tt('dve', ct[:, 1, :], wlast[:, 1, :], sc[:, EIM, :], ALU.mult, r=['wlast', K], w=['ct'])
            kb.tt('dve', ct[:, 2, :], wlast[:, 0, :], sc[:, EIM, :], ALU.mult, r=['wlast', K], w=['ct'])
            kb.tt('dve', ct[:, 3, :], wlast[:, 1, :], sc[:, ERE, :], ALU.mult, r=['wlast', K], w=['ct'])
            kb.tt('dve', init[:, 0, :], ct[:, 0, :], ct[:, 1, :], ALU.subtract, r=['ct'], w=['init'])
            kb.tt('dve', init[:, 1, :], ct[:, 2, :], ct[:, 3, :], ALU.add, r=['ct'], w=['init'])
            for q4 in range(4):
                psl = slice(q4 * 4, q4 * 4 + 4)
                r0, r1, r2, r3 = [(q4 % 2) * 4 + k_ for k_ in range(4)]
                kb.tt('dve', rt[r0][:], woall[:, 0, psl, :], cpo[:, psl, :], ALU.mult, r=['woall', 'cpo'], w=[f'rt{r0}'])
                kb.tt('dve', rt[r1][:], woall[:, 1, psl, :], spo[:, psl, :], ALU.mult, r=['woall', 'spo'], w=[f'rt{r1}'])
                kb.tt('pool', zre[:, psl, :], rt[r0][:], rt[r1][:], ALU.subtract, r=[f'rt{r0}', f'rt{r1}'], w=['zre'])
                kb.tt('dve', rt[r2][:], woall[:, 0, psl, :], spo[:, psl, :], ALU.mult, r=['woall', 'spo'], w=[f'rt{r2}'])
                kb.tt('dve', rt[r3][:], woall[:, 1, psl, :], cpo[:, psl, :], ALU.mult, r=['woall', 'cpo'], w=[f'rt{r3}'])
                kb.tt('pool', zim[:, psl, :], rt[r2][:], rt[r3][:], ALU.add, r=[f'rt{r2}', f'rt{r3}'], w=['zim'])
            kb.act(uo[:], uT[:, :, t0:t0 + 128], AF.Copy, r=['uT', 'hfm'], w=['uo'], scale=hfm[:, 0:1])
            kb.stt(uo[:], uT[:, :, t0 + 128:t0 + 256], hfm[:, 1:2], uo[:], ALU.mult, ALU.add, r=['uT', 'hfm', 'uo'], w=['uo'])
            for mt in range(4):
                pb = 3 + mt % 2
                for q in range(4):
                    pt = mt * 4 + q
                    kb.mm(ps[pb][:, 0:128], CA[:, pt, :], zre[:, pt, :], q == 0, False, r=['CA', 'zre'], w=[f'ps{pb}'])
                    kb.mm(ps[pb][:, 0:128], CBm[:, pt, :], zim[:, pt, :], False, q == 3, r=['CB', 'zim'], w=[f'ps{pb}'])
                kb.stt(ytmp[:], uo[:, mt, :], dsk[:, mt:mt + 1], ps[pb][:, 0:128], ALU.mult, ALU.add, r=['uo', 'dsk', f'ps{pb}'], w=['ytmp'])
                kb.act(zT[:, mt, c * 128:(c + 1) * 128], ytmp[:], AF.Gelu_apprx_tanh, r=['ytmp'], w=['zT'])

        stA(0)
        stA(1)
        stA(2)
        stB(0)
        stB(1)
        for i in range(nit):
            if i + 3 < nit:
                stA(i + 3)
            if i + 2 < nit:
                stB(i + 2)
            stC1(i)
            next(pgen5, None)
            next(pgen5, None)
            if i > 0 and its[i][1] != 0:
                stC2(i - 1)
            if its[i][1] == 15:
                stC2(i)
                chunk_end(its[i][0])
        for _ in pgen5:
            pass
        kb.barrier()
        ph3.close()
        wg_bf = kb.sb(ph, 's5_wgbf', [128, 4, 512], BF16)
        bglu = kb.sb(ph, 's5_bglu', [128, 4], F32)
        sg = kb.sb(ph, 's5_sg', [128, 512], BF16)
        load_w(wg_bf[:], 'wgbf', kb.inp('w_glu', [512, 512]))
        kb.dma('sp', bglu[:], kb.inp('b_gluT', [128, 4]), w=['bglu'])
        for mt in range(4):
            for c in range(4):
                pb = c % 2
                for kc in range(4):
                    kb.mm(ps[pb][:, :], wg_bf[:, kc, mt * 128:(mt + 1) * 128], zT[:, kc, c * 512:(c + 1) * 512], kc == 0, kc == 3,
                          r=['wgbf', 'zT'], w=[f'ps{pb}'])
                kb.act(sg[:], ps[pb][:, :], AF.Sigmoid, r=[f'ps{pb}', 'bglu'], w=['sg'], bias=bglu[:, mt:mt + 1])
                kb.tt('dve', y_ssmT[:, mt, c * 512:(c + 1) * 512], zT[:, mt, c * 512:(c + 1) * 512], sg[:], ALU.mult,
                      r=['zT', 'sg'], w=['y_ssmT'])
        if dbg and stage == 'S5':
            kb.dump('zT', zT[:], [128, 4, SO], 'zT', BF16)
            kb.dump('y_ssmT', y_ssmT[:], [128, 4, SO], 'y_ssmT', BF16)
            kb.dump('sc', sc[:], [128, 24, 16], K)
        kb.barrier()
    phU.close()
    if not st('ATT'):
        attK.close()
        return finish_now()
    phO = ExitStack()
    hTo = kb.sb(phO, 'hTo', [128, 8, SO], BF16)
    o_nsaT = kb.sb(phO, 'o_nsaT', [128, 4, SO], BF16)
    with ExitStack() as ph:
        norm_transpose(kb.inp('xo', [SO, D]), NO, hTo, 'hTo', ph)
        kb.barrier()
    with ExitStack() as ph:
        qn = kb.sb(ph, 'qn', [128, 4, SO], BF16)
        cmb = kb.sb(ph, 'cmb', [128, 2, SO], BF16)
        selb = kb.sb(ph, 'selb', [128, NO, 64], F32)
        Etab = kb.sb(ph, 'Etab', [128, 32, 128], BF16)
        cbt = kb.sb(ph, 'cbt', [128, 2, 128], BF16)
        wbm = kb.sb(ph, 'wbm', [128, 6, 128], BF16)
        selm1T = kb.sb(ph, 'selm1T', [128, SO], BF16)
        gates = kb.sb(ph, 'gates', [128, NO, 24], F32)
        kb.dma('pool', cmb[:], kb.inp('cmb', [128, 2, SO]), w=['cmb'])
        kb.dma('sp', selb[:], kb.inp('selb', [128, NO, 64]), w=['selb'])
        kb.dma('pool', Etab[:], kb.inp('Etab', [128, 32, 128]), w=['Etab'])
        kb.dma('pool', cbt[:], kb.inp('cbt', [128, 2, 128]), w=['cbt'])
        kb.dma('pool', wbm[:], kb.inp('wbm', [128, 6, 128]), w=['wbm'])
        with ExitStack() as ph2:
            wq = kb.sb(ph2, 'wq', [128, 8, 512], BF16)
            wgn = kb.sb(ph2, 'wgn', [128, 8, 24], BF16)
            tmp = {'raw': kb.sb(ph2, 'fn_raw2', [128, 512], F32), 'sq': kb.sb(ph2, 'fn_sq2', [128, 512], BF16),
                   'rs': kb.sb(ph2, 'fn_rs2', [128, 512], F32)}
            load_w(wq[:], 'wq', w_in[:, OQ:OQ + 512])
            load_w(wgn[:], 'wgn', w_in[:, OGN:OGN + 24])
            for r in range(4):
                for c in range(4):
                    pb = c % 2
                    for g in range(2):
                        hd = 4 * g + r
                        for kc in range(8):
                            kb.mm(ps[pb][g * 64:(g + 1) * 64, :], wq[:, kc, hd * 64:(hd + 1) * 64], hTo[:, kc, c * 512:(c + 1) * 512],
                                  kc == 0, kc == 7, r=['wq', 'hTo'], w=[f'ps{pb}'])
                    fm_norm(ps[pb][:, :], f'ps{pb}', 512, qn[:, r, c * 512:(c + 1) * 512], 'qn', 0, tmp, 1.0, 64 * EPS, 2 + pb)
            for j in range(NO):
                for kc in range(8):
                    kb.mm(ps[6][:, 0:24], hTo[:, kc, j * 128:(j + 1) * 128], wgn[:, kc, :], kc == 0, kc == 7, r=['wgn', 'hTo'], w=['ps6'])
                kb.act(gates[:, j, :], ps[6][:, 0:24], AF.Sigmoid, r=['ps6'], w=['gates'])
            kb.barrier()
        Pb = [kb.sb(ph, f'Pb{i}', [128, 512], BF16) for i in range(3)]
        onsa = [kb.sb(ph, f'onsa{i}', [128, 512], F32) for i in range(2)]
        impacc = kb.sb(ph, 'impacc', [128, 128], F32)
        impb = kb.sb(ph, 'impb', [128, 128], F32)
        sel = kb.sb(ph, 'sel', [128, 128], F32)
        tmps = kb.sb(ph, 'tmps', [128, 64], F32)
        m8 = kb.sb(ph, 'm8', [128, 16], F32)
        rs = kb.sb(ph, 'rs', [128, 4], F32)
        coef = kb.sb(ph, 'coef', [128, 4], F32)
        tmpo = kb.sb(ph, 'tmpo', [128, 4, 64], F32)
        ctr = {'s': 0, 'p': 0, 'o': 0}

        def nxt(k, n):
            v = ctr[k] % n
            ctr[k] += 1
            return v

        def score_exp(mms):
            sb_ = nxt('s', 2)
            out3 = ps[sb_][:, :].rearrange("p (r q) -> p r q", r=4)
            for i, (lhsT, rhs, rk) in enumerate(mms):
                kb.mm(out3, lhsT, rhs, i == 0, i == len(mms) - 1, r=rk, w=[f'ps{sb_}'])
            pi = nxt('p', 3)
            kb.act(Pb[pi][:], ps[sb_][:, :], AF.Exp, r=[f'ps{sb_}'], w=[f'Pb{pi}'])
            pre_tick()
            return pi

        def cmp_att(g, j):
            gp = slice(g * 64, (g + 1) * 64)
            jsl = slice(j * 128, (j + 1) * 128)
            ob = j % 2
            def sc_(bt):
                return score_exp([(kcn[gp, bt * 128:(bt + 1) * 128], qn[gp, :, jsl], ['kcn', 'qn']),
                                  (identb[:, :], bc(cmb[:, bt, jsl].unsqueeze(1), [128, 4, 128]), ['identb', 'cmb'])])
            pis = {0: sc_(0)}
            for bt in range(2):
                if bt + 1 < 2:
                    pis[bt + 1] = sc_(bt + 1)
                pi = pis[bt]
                for r in range(4):
                    bank = 4 + r // 2
                    c0 = (r % 2) * 129
                    kb.mm(ps[bank][:, c0:c0 + 129], Pb[pi][:, r * 128:(r + 1) * 128], VOX[:, bt, g, :], bt == 0 and r % 2 == 0, bt == 1,
                          r=[f'Pb{pi}', 'VOX'], w=[f'ps{bank}'], sgc=True)
            for bank in (4, 5):
                v = ps[bank][:, 0:258].rearrange("p (r c) -> p r c", r=2)
                hd0 = 4 * g + 2 * (bank - 4)
                kb.ts('dve', rs[:, 0:2], v[:, :, 64], 1e-30, ALU.max, r=[f'ps{bank}'], w=['rs'])
                kb.op('dve', lambda e: e.reciprocal(out=rs[:, 0:2], in_=rs[:, 0:2]), r=['rs'], w=['rs'])
                kb.tt('dve', coef[:, 0:2], rs[:, 0:2], gates[:, j, hd0 * 3:hd0 * 3 + 6:3], ALU.mult, r=['rs', 'gates'], w=['coef'])
                for rr in range(2):
                    hd = hd0 + rr
                    ia = impacc[:, g * 64:(g + 1) * 64]
                    if hd % 4 == 0:
                        kb.ts('dve', ia, v[:, rr, 65:129], rs[:, rr:rr + 1], ALU.mult, r=[f'ps{bank}', 'rs'], w=['impacc'])
                    else:
                        kb.stt(ia, v[:, rr, 65:129], rs[:, rr:rr + 1], ia, ALU.mult, ALU.add, r=[f'ps{bank}', 'rs', 'impacc'], w=['impacc'])
                    kb.ts('dve', onsa[ob][:, hd * 64:(hd + 1) * 64], v[:, rr, 0:64], coef[:, rr:rr + 1], ALU.mult,
                          r=[f'ps{bank}', 'coef'], w=[f'onsa{ob}'])

        def select(j):
            kb.tt('dve', impb[:].rearrange("p (g b) -> p g b", g=2), impacc[:].rearrange("p (g b) -> p g b", g=2),
                  bc(selb[:, j, :].unsqueeze(1), [128, 2, 64]), ALU.add, r=['impacc', 'selb'], w=['impb'])
            for g in range(2):
                iv = impb[:, g * 64:(g + 1) * 64]
                kb.op('dve', lambda e: e.max(out=m8[:, 0:8], in_=iv), r=['impb'], w=['m8'])
                kb.op('dve', lambda e: e.match_replace(out=tmps[:], in_to_replace=m8[:, 0:8], in_values=iv, imm_value=-1e9),
                      r=['impb', 'm8'], w=['tmps'])
                kb.op('dve', lambda e: e.max(out=m8[:, 8:16], in_=tmps[:]), r=['tmps'], w=['m8'])
                kb.ts('dve', sel[:, g * 64:(g + 1) * 64], iv, m8[:, 15:16], ALU.is_ge, r=['impb', 'm8'], w=['sel'])
            kb.ts('dve', sel[:], sel[:], -1.0, ALU.add, r=['sel'], w=['sel'])
            kb.tr(ps[6][:, 0:128], sel[:], ident[:], r=['sel', 'ident'], w=['ps6'])
            kb.act(selm1T[:, j * 128:(j + 1) * 128], ps[6][:, 0:128], AF.Copy, r=['ps6'], w=['selm1T'])

        def evac_o(bank, g, j, br):
            ob = j % 2
            v = ps[bank][:, 0:260].rearrange("p (r c) -> p r c", r=4)
            kb.ts('dve', rs[:, 0:4], v[:, :, 64], 1e-30, ALU.max, r=[f'ps{bank}'], w=['rs'])
            kb.op('dve', lambda e: e.reciprocal(out=rs[:, 0:4], in_=rs[:, 0:4]), r=['rs'], w=['rs'])
            kb.tt('dve', coef[:, 0:4], rs[:, 0:4], gates[:, j, 12 * g + br:12 * g + 12:3], ALU.mult, r=['rs', 'gates'], w=['coef'])
            kb.tt('dve', tmpo[:], v[:, :, 0:64], bc(coef[:, 0:4].unsqueeze(2), [128, 4, 64]), ALU.mult, r=[f'ps{bank}', 'coef'], w=['tmpo'])
            od = onsa[ob][:, g * 256:(g + 1) * 256].rearrange("p (r d) -> p r d", r=4)
            kb.tt('pool', od, od, tmpo[:], ALU.add, r=[f'onsa{ob}', 'tmpo'], w=[f'onsa{ob}'])

        def sel_att(g, j):
            gp = slice(g * 64, (g + 1) * 64)
            jsl = slice(j * 128, (j + 1) * 128)
            nk = 2 * j + 2
            bank = 2 + nxt('o', 2)
            def sc_(kt):
                mms = [(ksT[gp, kt * 128:(kt + 1) * 128], qn[gp, :, jsl], ['ksT', 'qn']),
                       (Etab[gp, kt, :], bc(selm1T[gp, jsl].unsqueeze(1), [64, 4, 128]), ['Etab', 'selm1T'])]
                if kt >= 2 * j:
                    mms.append((identb[:, :], bc(cbt[:, kt - 2 * j, :].unsqueeze(1), [128, 4, 128]), ['identb', 'cbt']))
                return score_exp(mms)
            pis = {0: sc_(0)}
            for kt in range(nk):
                if kt + 1 < nk:
                    pis[kt + 1] = sc_(kt + 1)
                pi = pis.pop(kt)
                for r in range(4):
                    kb.mm(ps[bank][:, r * 65:(r + 1) * 65], Pb[pi][:, r * 128:(r + 1) * 128], VS[:, kt, g, :], kt == 0 and r == 0, kt == nk - 1,
                          r=[f'Pb{pi}', 'VS'], w=[f'ps{bank}'], sgc=True)
            evac_o(bank, g, j, 1)

        def win_att(g, j):
            gp = slice(g * 64, (g + 1) * 64)
            jsl = slice(j * 128, (j + 1) * 128)
            bank = 2 + nxt('o', 2)
            kts = [(i, 2 * j - 4 + i) for i in range(6) if 2 * j - 4 + i >= 0]

            def sc_(n):
                i, kt = kts[n]
                return score_exp([(kwT[gp, kt * 128:(kt + 1) * 128], qn[gp, :, jsl], ['kwT', 'qn']),
                                  (identb[:, :], bc(wbm[:, i, :].unsqueeze(1), [128, 4, 128]), ['identb', 'wbm'])])
            pis = {0: sc_(0)}
            for n in range(len(kts)):
                if n + 1 < len(kts):
                    pis[n + 1] = sc_(n + 1)
                pi = pis.pop(n)
                kt = kts[n][1]
                for r in range(4):
                    kb.mm(ps[bank][:, r * 65:(r + 1) * 65], Pb[pi][:, r * 128:(r + 1) * 128], VW[:, kt, g, :], n == 0 and r == 0, n == len(kts) - 1,
                          r=[f'Pb{pi}', 'VW'], w=[f'ps{bank}'], sgc=True)
            evac_o(bank, g, j, 2)

        def finalize(j):
            ob = j % 2
            for q in range(4):
                kb.tr(ps[6][:, q * 128:(q + 1) * 128], onsa[ob][:, q * 128:(q + 1) * 128], ident[:], r=[f'onsa{ob}', 'ident'], w=['ps6'])
            kb.act(o_nsaT[:, :, j * 128:(j + 1) * 128], ps[6][:, :].rearrange("p (q t) -> p q t", q=4), AF.Copy, r=['ps6'], w=['o_nsaT'])

        if pre_on and NPRE_ATT > 0:
            pgen = make_pre_gen(ph, 'a', NPRE_S5, NPRE_ATT, 6, 'dve', 1024)
        else:
            pgen = iter(())
        pstep = {'n': 0}

        def pre_tick():
            pstep['n'] += 1
            if pstep['n'] % 2 == 0:
                next(pgen, None)

        NJ = NO if not (dbg and stage == 'ATT' and DBG_NJ) else DBG_NJ
        cmp_att(0, 0)
        cmp_att(1, 0)
        select(0)
        for j in range(NJ):
            if j + 1 < NJ:
                cmp_att(0, j + 1)
                cmp_att(1, j + 1)
                select(j + 1)
            sel_att(0, j)
            sel_att(1, j)
            win_att(0, j)
            win_att(1, j)
            finalize(j)
        for _ in pgen:
            pass
        if dbg and stage == 'ATT':
            kb.dump('qn', qn[:], [128, 4, SO], 'qn', BF16)
            kb.dump('gates', gates[:], [128, NO, 24], 'gates')
            kb.dump('selm1T', selm1T[:], [128, SO], 'selm1T', BF16)
            kb.dump('o_nsaT', o_nsaT[:], [128, 4, SO], 'o_nsaT', BF16)
        kb.barrier()
    if not st('MERGE'):
        phO.close()
        attK.close()
        return finish_now()
    x1s = kb.scratch('x1s', [SO, D], F32)
    xo_d = kb.inp('xo', [SO, D])
    with ExitStack() as ph:
        mergedT = kb.sb(ph, 'mergedT', [128, 8, SO], BF16)
        wo = kb.sb(ph, 'wo', [128, 8, 1024], BF16)
        load_w(wo[:], 'wo', kb.inp('w_out', [1024, 1024]))
        wga = [kb.sb(ph, f'wga{i}', [128, 8, 128], BF16) for i in range(2)]
        wgs = [kb.sb(ph, f'wgs{i}', [128, 8, 128], BF16) for i in range(2)]
        wua = [kb.sb(ph, f'wua{i}', [128, 4, 128], BF16) for i in range(2)]
        wus = [kb.sb(ph, f'wus{i}', [128, 4, 128], BF16) for i in range(2)]
        sgA = kb.sb(ph, 'sgA', [128, 512], F32)
        sgB = kb.sb(ph, 'sgB', [128, 512], F32)
        tmA = kb.sb(ph, 'tmA', [128, 512], F32)
        tmB = kb.sb(ph, 'tmB', [128, 512], F32)
        w_up_attn = kb.inp('w_up_attn', [512, 1024])
        w_up_ssm = kb.inp('w_up_ssm', [512, 1024])
        for m in range(8):
            b = m % 2
            msl = slice(m * 128, (m + 1) * 128)
            load_w(wga[b][:], f'wga{b}', w_in[:, OGA + m * 128:OGA + (m + 1) * 128])
            load_w(wgs[b][:], f'wgs{b}', w_in[:, OGS + m * 128:OGS + (m + 1) * 128])
            load_w(wua[b][:], f'wua{b}', w_up_attn[:, msl])
            load_w(wus[b][:], f'wus{b}', w_up_ssm[:, msl])
            for c in range(4):
                csl = slice(c * 512, (c + 1) * 512)
                for kc in range(4):
                    kb.mm(ps[0][:, :], wua[b][:, kc, :], o_nsaT[:, kc, csl], kc == 0, kc == 3, r=[f'wua{b}', 'o_nsaT'], w=['ps0'])
                for kc in range(8):
                    kb.mm(ps[1][:, :], wga[b][:, kc, :], hTo[:, kc, csl], kc == 0, kc == 7, r=[f'wga{b}', 'hTo'], w=['ps1'])
                for kc in range(4):
                    kb.mm(ps[2][:, :], wus[b][:, kc, :], y_ssmT[:, kc, csl], kc == 0, kc == 3, r=[f'wus{b}', 'y_ssmT'], w=['ps2'])
                for kc in range(8):
                    kb.mm(ps[3][:, :], wgs[b][:, kc, :], hTo[:, kc, csl], kc == 0, kc == 7, r=[f'wgs{b}', 'hTo'], w=['ps3'])
                kb.act(sgA[:], ps[1][:, :], AF.Sigmoid, r=['ps1'], w=['sgA'])
                kb.act(sgB[:], ps[3][:, :], AF.Sigmoid, r=['ps3'], w=['sgB'])
                kb.tt('dve', tmA[:], sgA[:], ps[0][:, :], ALU.mult, r=['sgA', 'ps0'], w=['tmA'])
                kb.tt('dve', tmB[:], sgB[:], ps[2][:, :], ALU.mult, r=['sgB', 'ps2'], w=['tmB'])
                kb.tt('pool', mergedT[:, m, csl], tmA[:], tmB[:], ALU.add, r=['tmA', 'tmB'], w=['mergedT'])
        gt1row = kb.sb(ph, 'gt1row', [128, 1024], F32)
        kb.dma('sp', gt1row[:], modrow_d[0:1, 0:1024].partition_broadcast(128), r=['modrow_d'], w=['gt1row'])
        xt = [kb.sb(ph, f'mg_x{i}', [128, 1024], F32) for i in range(2)]
        x1t = [kb.sb(ph, f'mg_x1{i}', [128, 1024], F32) for i in range(2)]
        tmx = kb.sb(ph, 'tmx', [128, 512], F32)
        for i in range(NO):
            b = i % 2
            isl = slice(i * 128, (i + 1) * 128)
            kb.dma('sp', xt[b][:], xo_d[isl, :], w=[f'mg_x{b}'])
            for h in range(2):
                pb = 4 + h
                hsl = slice(h * 512, (h + 1) * 512)
                for kc in range(8):
                    kb.mm(ps[pb][:, :], mergedT[:, kc, isl], wo[:, kc, hsl], kc == 0, kc == 7, r=['mergedT', 'wo'], w=[f'ps{pb}'])
                kb.tt('dve', tmx[:], ps[pb][:, :], gt1row[:, hsl], ALU.mult, r=[f'ps{pb}', 'gt1row'], w=['tmx'])
                kb.tt('pool', x1t[b][:, hsl], tmx[:], xt[b][:, hsl], ALU.add, r=['tmx', f'mg_x{b}'], w=[f'mg_x1{b}'])
            kb.dma('sp', x1s[isl, :], x1t[b][:], r=[f'mg_x1{b}'], w=[])
        if dbg and stage == 'MERGE':
            kb.dump('mergedT', mergedT[:], [128, 8, SO], 'mergedT', BF16)
            o_ = kb.outp('dbg_x1', [SO, D])
            kb.dma('sp', o_, x1s, r=['x1s'])
        kb.barrier()
    phO.close()
    attK.close()
    if not st('MOE'):
        return finish_now()
    NSLOT = NEXP * CAP
    xs_d = kb.scratch('xs_d', [NSLOT + 1, D], BF16)
    ys_d = kb.scratch('ys_d', [NSLOT + 1, D], BF16)
    out_d = kb.outp('out', [SO, D], F32)
    with ExitStack() as ph:
        h2T = kb.sb(ph, 'h2T', [128, 8, SO], BF16)
        selbf = kb.sb(ph, 'selbf', [128, NO, 256], BF16)
        w8 = kb.sb(ph, 'w8', [128, NO, 8], F32)
        idx = kb.sb(ph, 'idx', [128, NO, 8], I32)
        SU = kb.sb(ph, 'SU', [128, 128], BF16)
        onesb = kb.sb(ph, 'onesb', [128, 128], BF16)
        eCAP1 = kb.sb(ph, 'eCAP1', [128, 256], F32)
        rbias = kb.sb(ph, 'rbias', [128, 256], F32)
        zrow = kb.sb(ph, 'zrow', [1, 1024], BF16)
        sh2row = kb.sb(ph, 'sh2row', [128, 1024], F32)
        gt2row = kb.sb(ph, 'gt2row', [128, 1024], F32)
        a2row = kb.sb(ph, 'a2rowm', [128, 1024], F32)
        kb.dma('sp', sh2row[:], modrow_d[0:1, 1024:2048].partition_broadcast(128), r=['modrow_d'], w=['sh2row'])
        kb.dma('sp', gt2row[:], modrow_d[0:1, 3072:4096].partition_broadcast(128), r=['modrow_d'], w=['gt2row'])
        kb.dma('sp', a2row[:], a2row_d[0:1, :].partition_broadcast(128), r=['a2row_d'], w=['a2row'])
        kb.dma('pool', SU[:], kb.inp('SU', [128, 128]), w=['SU'])
        kb.op('pool', lambda e: e.memset(onesb[:], 1.0), w=['onesb'])
        kb.dma('sp', eCAP1[:], kb.inp('eCAP1', [128, 256]), w=['eCAP1'])
        kb.dma('sp', rbias[:], kb.inp('rbias', [1, 256])[0:1, :].partition_broadcast(128), w=['rbias'])
        kb.op('pool', lambda e: e.memset(zrow[:], 0.0), w=['zrow'])
        kb.dma('sp', ys_d[NSLOT:NSLOT + 1, :], zrow[:], r=['zrow'], w=['ys_d'])
        with ExitStack() as ph2:
            wr = kb.sb(ph2, 'wr', [128, 8, 256], F32)
            kb.dma('sp', wr[:], kb.inp('w_router', [1024, 256]).rearrange("(kc p) n -> p kc n", p=128), w=['wr'])

            def mk(name, shape, dt=F32):
                return [kb.sb(ph2, f'{name}{i}', shape, dt) for i in range(4)]
            x1t = mk('mo_x', [128, 1024])
            h2f = mk('mo_h', [128, 1024])
            h2b = mk('mo_hb', [128, 1024], BF16)
            h2Tf = mk('h2Tf', [128, 8, 128])
            junk = mk('mo_junk', [128, 1024], BF16)
            ss = mk('mo_ss', [128, 4])
            scr_ = mk('mo_sc', [128, 256])
            bia = mk('mo_bia', [128, 256])
            msk = mk('mo_msk', [128, 256])
            selm = mk('mo_sel', [128, 256])
            wm = mk('mo_wm', [128, 256])
            okm = mk('mo_okm', [128, 256])
            slotv = mk('mo_slotv', [128, 256])
            jk2 = mk('mo_jk2', [128, 256])
            m8g = mk('mo_m8g', [128, 8, 8])
            gs = mk('mo_gs', [128, 8])
            gm8 = mk('mo_gm8', [128, 8])
            gmask = mk('mo_gmask', [128, 8])
            gneg = mk('mo_gneg', [128, 8])
            t8 = mk('mo_t8', [128, 8])
            s8 = mk('mo_s8', [128, 8])
            s8b = mk('mo_s8b', [128, 8])
            idf = mk('mo_idf', [128, 8])
            wsum = mk('mo_wsum', [128, 2])

            def route_tile(i):
                b = i % 4
                B = str(b)
                isl = slice(i * 128, (i + 1) * 128)
                kb.dma('sp', x1t[b][:], x1s[isl, :], r=['x1s'], w=['mo_x' + B])
                kb.act(junk[b][:], x1t[b][:], AF.Square, r=['mo_x' + B], w=['mo_junk' + B, 'mo_ss0' + B], accum=ss[b][:, 0:1])
                yield
                kb.act(ss[b][:, 1:2], ss[b][:, 0:1], AF.Sqrt, r=['mo_ss0' + B], w=['mo_ss1' + B], scale=1.0 / D, bias=EPS)
                yield
                kb.op('dve', lambda e: e.reciprocal(out=ss[b][:, 2:3], in_=ss[b][:, 1:2]), r=['mo_ss1' + B], w=['mo_ss2' + B])
                yield
                kb.stt(h2f[b][:], x1t[b][:], ss[b][:, 2:3], a2row[:], ALU.mult, ALU.mult, r=['mo_x' + B, 'mo_ss2' + B, 'a2row'], w=['mo_h' + B])
                yield
                kb.tt('dve', h2f[b][:], h2f[b][:], sh2row[:], ALU.add, r=['mo_h' + B, 'sh2row'], w=['mo_h' + B])
                yield
                kb.act(h2b[b][:], h2f[b][:], AF.Copy, r=['mo_h' + B], w=['mo_hb' + B])
                for half in range(2):
                    pb = 2 * b
                    for q in range(4):
                        kc = half * 4 + q
                        kb.tr(ps[pb][:, q * 128:(q + 1) * 128], h2f[b][:, kc * 128:(kc + 1) * 128], ident[:], r=['mo_h' + B, 'ident'], w=[f'ps{pb}'])
                    yield
                    kb.act(h2Tf[b][:, half * 4:half * 4 + 4, :], ps[pb][:, :].rearrange("p (q t) -> p q t", q=4), AF.Copy, r=[f'ps{pb}'], w=['h2Tf' + B])
                yield
                kb.act(h2T[:, :, isl], h2Tf[b][:], AF.Copy, r=['h2Tf' + B], w=[f'h2T{i}'])
                pr = 2 * b + 1
                for kc in range(8):
                    kb.mm(ps[pr][:, 0:256], h2Tf[b][:, kc, :], wr[:, kc, :], kc == 0, kc == 7, r=['h2Tf' + B, 'wr'], w=[f'ps{pr}'])
                yield
                kb.act(scr_[b][:], ps[pr][:, 0:256], AF.Sigmoid, r=[f'ps{pr}'], w=['mo_sc' + B])
                yield
                kb.tt('dve', bia[b][:], scr_[b][:], rbias[:], ALU.add, r=['mo_sc' + B, 'rbias'], w=['mo_bia' + B])
                yield
                for gi in range(8):
                    kb.op('dve', lambda e: e.max(out=m8g[b][:, gi, :], in_=bia[b][:, gi * 32:(gi + 1) * 32]), r=['mo_bia' + B], w=['mo_m8g' + B + str(gi)])
                yield
                kb.tt('dve', gs[b][:], m8g[b][:, :, 0], m8g[b][:, :, 1], ALU.add, r=['mo_m8g' + B + str(g_) for g_ in range(8)], w=['mo_gs' + B])
                yield
                kb.op('dve', lambda e: e.max(out=gm8[b][:], in_=gs[b][:]), r=['mo_gs' + B], w=['mo_gm8' + B])
                yield
                kb.ts('dve', gmask[b][:], gs[b][:], gm8[b][:, 3:4], ALU.is_ge, r=['mo_gs' + B, 'mo_gm8' + B], w=['mo_gmask' + B])
                yield
                kb.ts('dve', gneg[b][:], gmask[b][:], -1.0, ALU.add, s2=10.0, op1=ALU.mult, r=['mo_gmask' + B], w=['mo_gneg' + B])
                b3 = bia[b][:].rearrange("p (g e) -> p g e", g=8)
                m3 = msk[b][:].rearrange("p (g e) -> p g e", g=8)
                kb.tt('dve', m3, b3, bc(gmask[b][:].unsqueeze(2), [128, 8, 32]), ALU.mult, r=['mo_bia' + B, 'mo_gmask' + B], w=['mo_msk' + B])
                yield
                kb.tt('dve', m3, m3, bc(gneg[b][:].unsqueeze(2), [128, 8, 32]), ALU.add, r=['mo_msk' + B, 'mo_gneg' + B], w=['mo_msk' + B])
                yield
                kb.op('dve', lambda e: e.max(out=t8[b][:], in_=msk[b][:]), r=['mo_msk' + B], w=['mo_t8' + B])
                yield
                kb.ts('dve', selm[b][:], msk[b][:], t8[b][:, 7:8], ALU.is_ge, r=['mo_msk' + B, 'mo_t8' + B], w=['mo_sel' + B])
                yield
                kb.tt('dve', wm[b][:], scr_[b][:], selm[b][:], ALU.mult, r=['mo_sc' + B, 'mo_sel' + B], w=['mo_wm' + B])
                kb.act(selbf[:, i, :], selm[b][:], AF.Copy, r=['mo_sel' + B], w=[f'selbf{i}'])
                yield
                kb.op('dve', lambda e: e.tensor_reduce(out=wsum[b][:, 0:1], in_=wm[b][:], axis=AX.X, op=ALU.add), r=['mo_wm' + B], w=['mo_wsum' + B])
                yield
                kb.op('dve', lambda e: e.reciprocal(out=wsum[b][:, 1:2], in_=wsum[b][:, 0:1]), r=['mo_wsum' + B], w=['mo_wsum' + B])
                yield
                kb.ts('dve', wm[b][:], wm[b][:], wsum[b][:, 1:2], ALU.mult, s2=2.5, op1=ALU.mult, r=['mo_wm' + B, 'mo_wsum' + B], w=['mo_wm' + B])
                pp = 2 * b + 1
                for i2 in range(i + 1):
                    lhs = SU[:, :] if i2 == i else onesb[:, :]
                    kb.mm(ps[pp][:, 256:512], lhs, selbf[:, i2, :], i2 == 0, i2 == i, r=['SU', 'onesb', f'selbf{i2}'], w=[f'ps{pp}'])
                yield
                kb.ts('dve', okm[b][:], ps[pp][:, 256:512], float(CAP), ALU.is_lt, r=[f'ps{pp}'], w=['mo_okm' + B])
                yield
                kb.tt('dve', okm[b][:], okm[b][:], selm[b][:], ALU.mult, r=['mo_okm' + B, 'mo_sel' + B], w=['mo_okm' + B])
                kb.tt('dve', slotv[b][:], ps[pp][:, 256:512], eCAP1[:], ALU.add, r=[f'ps{pp}', 'eCAP1'], w=['mo_slotv' + B])
                yield
                kb.tt('dve', slotv[b][:], slotv[b][:], okm[b][:], ALU.mult, r=['mo_slotv' + B, 'mo_okm' + B], w=['mo_slotv' + B])
                yield
                kb.op('dve', lambda e: e.max(out=s8[b][:], in_=slotv[b][:]), r=['mo_slotv' + B], w=['mo_s8' + B])
                yield
                for k in range(8):
                    kb.stt(jk2[b][:], slotv[b][:], s8[b][:, k:k + 1], wm[b][:], ALU.is_equal, ALU.mult, r=['mo_slotv' + B, 'mo_s8' + B, 'mo_wm' + B],
                           w=['mo_jk2' + B, f'w8_{i}_{k}'], accum=w8[:, i, k:k + 1])
                    if k % 2 == 1:
                        yield
                kb.ts('dve', s8b[b][:], s8[b][:], 0.0, ALU.is_equal, s2=float(NSLOT + 1), op1=ALU.mult, r=['mo_s8' + B], w=['mo_s8b' + B])
                yield
                kb.stt(idf[b][:], s8[b][:], -1.0, s8b[b][:], ALU.add, ALU.add, r=['mo_s8' + B, 'mo_s8b' + B], w=['mo_idf' + B])
                yield
                kb.cp('dve', idx[:, i, :], idf[b][:], r=['mo_idf' + B], w=[f'idx{i}'])
                yield
                for k in range(8):
                    kb.dma('pool', None, None, r=['mo_hb' + B, f'idx{i}'], w=[],
                           fn=lambda e: e.indirect_dma_start(out=xs_d[:, :], out_offset=bass.IndirectOffsetOnAxis(ap=idx[:, i, k:k + 1], axis=0),
                                                             in_=h2b[b][:], in_offset=None))
                    if k % 4 == 3:
                        yield
                if dbg and stage == 'MOE' and i == 0:
                    kb.dump('wm0', wm[b][:], [128, 256], 'mo_wm' + B)

            if pre_on and NPRE_RT > 0:
                pgen_r = make_pre_gen(ph2, 'r', NPRE_S5 + NPRE_ATT, NPRE_RT, 4, 'act', 1024)
            else:
                pgen_r = iter(())

            def take(g, n):
                for _ in range(n):
                    next(g, None)
                    yield
            for i0_ in range(0, NO, 4):
                run_interleaved([route_tile(i0_ + k_) for k_ in range(4)] + [take(pgen_r, 37)], 5)
            for _ in pgen_r:
                pass
            kb.barrier()
        w_gate = kb.inp('w_gate', [NEXP, 1024, 256])
        w_up = kb.inp('w_up', [NEXP, 1024, 256])
        w_down = kb.inp('w_down', [NEXP, 256, 1024])
        if not st('MOE') or NPRE == 0:
            wpre = None
        NE = NEXP if not (dbg and DBG_NE) else DBG_NE
        with ExitStack() as ph2:
            NS = 3
            wgst = [kb.sb(ph2, f'wgst{i}', [128, 8, 256], F32) for i in range(NS)]
            wust = [kb.sb(ph2, f'wust{i}', [128, 8, 256], F32) for i in range(NS)]
            wdst = [kb.sb(ph2, f'wdst{i}', [128, 2, 1024], F32) for i in range(NS)]
            wgb = [kb.sb(ph2, f'wgb{i}', [128, 8, 256], BF16) for i in range(2)]
            wub = [kb.sb(ph2, f'wub{i}', [128, 8, 256], BF16) for i in range(2)]
            wdb = [kb.sb(ph2, f'wdb{i}', [128, 2, 1024], BF16) for i in range(2)]
            xse = [kb.sb(ph2, f'xse{i}', [128, 2, 1024], BF16) for i in range(NS)]
            hTe = [kb.sb(ph2, f'hTe{i}', [128, 8, CAP], BF16) for i in range(2)]
            sgt = kb.sb(ph2, 'sgt', [128, 2 * CAP], F32)
            actT = kb.sb(ph2, 'actT', [128, 2, CAP], BF16)
            yse = [kb.sb(ph2, f'yse{i}', [128, 2, 1024], BF16) for i in range(2)]
            p16a = ps[6][:, :].bitcast(BF16)
            p16b = ps[7][:, :].bitcast(BF16)

            def load_e(e):
                s_ = e % NS
                if not (wpre is not None and e < NPRE):
                    kb.dma('sp', wgst[s_][:], w_gate[e].rearrange("(p r) n -> p r n", r=8), w=[f'wgst{s_}'])
                    kb.dma('sp', wust[s_][:], w_up[e].rearrange("(p r) n -> p r n", r=8), w=[f'wust{s_}'])
                    kb.dma('sp', wdst[s_][:], w_down[e].rearrange("(kc p) n -> p kc n", p=128), w=[f'wdst{s_}'])
                kb.dma('sp', xse[s_][:, 0, :], xs_d[e * CAP:e * CAP + 128, :], r=['xs_d'], w=[f'xse{s_}a'])
                kb.dma('sp', xse[s_][0:64, 1, :], xs_d[e * CAP + 128:e * CAP + 192, :], r=['xs_d'], w=[f'xse{s_}b'])

            def cast_e(e):
                s_ = e % NS
                b = e % 2
                if wpre is not None and e < NPRE:
                    kb.dma('sp', wgb[b][:].rearrange("p r n -> p (r n)"), wpre[0][e], w=[f'wgb{b}'])
                    kb.dma('sp', wub[b][:].rearrange("p r n -> p (r n)"), wpre[1][e], w=[f'wub{b}'])
                    kb.dma('sp', wdb[b][:].rearrange("p r n -> p (r n)"), wpre[2][e], w=[f'wdb{b}'])
                    return
                kb.cp('pool', wgb[b][:, 0:4, :], wgst[s_][:, 0:4, :], r=[f'wgst{s_}'], w=[f'wgb{b}'])
                kb.cp('dve', wgb[b][:, 4:8, :], wgst[s_][:, 4:8, :], r=[f'wgst{s_}'], w=[f'wgb{b}'])
                kb.act(wub[b][:], wust[s_][:], AF.Copy, r=[f'wust{s_}'], w=[f'wub{b}'])
                kb.cp('dve', wdb[b][:, 0, :], wdst[s_][:, 0, :], r=[f'wdst{s_}'], w=[f'wdb{b}'])
                kb.act(wdb[b][:, 1, :], wdst[s_][:, 1, :], AF.Copy, r=[f'wdst{s_}'], w=[f'wdb{b}'])

            def transp_e(e):
                s_ = e % NS
                b = e % 2
                for kc in range(8):
                    kb.tr(p16a[:, kc * 128:(kc + 1) * 128], xse[s_][:, 0, kc:1024:8], identb[:], r=[f'xse{s_}a', 'identb'], w=['ps6'])
                for kc in range(8):
                    kb.tr(p16b[:, kc * 64:(kc + 1) * 64], xse[s_][0:64, 1, kc:1024:8], identb[0:64, 0:64], r=[f'xse{s_}b', 'identb'], w=['ps7'])
                kb.act(hTe[b][:, :, 0:128], p16a[:, :].rearrange("p (k s) -> p k s", k=8), AF.Copy, r=['ps6'], w=[f'hTe{b}'])
                kb.cp('dve', hTe[b][:, :, 128:192], p16b[:, 0:512].rearrange("p (k s) -> p k s", k=8), r=['ps7'], w=[f'hTe{b}'])

            load_e(0)
            if NE > 1:
                load_e(1)
            cast_e(0)
            transp_e(0)
            for e in range(NE):
                b = e % 2
                if e + 2 < NE:
                    load_e(e + 2)
                if e + 1 < NE:
                    cast_e(e + 1)
                for gu, (wt, wk, bank) in enumerate(((wgb, 'wgb', 0), (wub, 'wub', 1))):
                    for m in range(2):
                        for kc in range(8):
                            kb.mm(ps[bank][:, m * CAP:(m + 1) * CAP], wt[b][:, kc, m * 128:(m + 1) * 128], hTe[b][:, kc, :],
                                  m == 0 and kc == 0, kc == 7, r=[f'{wk}{b}', f'hTe{b}'], w=[f'ps{bank}'], sgc=True)
                kb.act(sgt[:], ps[0][:, 0:2 * CAP], AF.Silu, r=['ps0'], w=['sgt'])
                kb.tt('dve', actT[:].rearrange("p m s -> p (m s)"), sgt[:], ps[1][:, 0:2 * CAP], ALU.mult, r=['sgt', 'ps1'], w=['actT'])
                if e + 1 < NE:
                    transp_e(e + 1)
                for st_, (ns, banks) in enumerate(((128, (2, 3)), (64, (4, 5)))):
                    for h in range(2):
                        for m in range(2):
                            kb.mm(ps[banks[h]][0:ns, :], actT[:, m, st_ * 128:st_ * 128 + ns], wdb[b][:, m, h * 512:(h + 1) * 512], m == 0, m == 1,
                                  r=['actT', f'wdb{b}'], w=[f'ps{banks[h]}'])
                kb.act(yse[b][:, 0, 0:512], ps[2][:, :], AF.Copy, r=['ps2'], w=[f'yse{b}'])
                kb.cp('dve', yse[b][:, 0, 512:1024], ps[3][:, :], r=['ps3'], w=[f'yse{b}'])
                kb.act(yse[b][0:64, 1, 0:512], ps[4][0:64, :], AF.Copy, r=['ps4'], w=[f'yse{b}'])
                kb.cp('dve', yse[b][0:64, 1, 512:1024], ps[5][0:64, :], r=['ps5'], w=[f'yse{b}'])
                kb.dma('act', ys_d[e * CAP:e * CAP + 128, :], yse[b][:, 0, :], r=[f'yse{b}'], w=[])
                kb.dma('act', ys_d[e * CAP + 128:e * CAP + 192, :], yse[b][0:64, 1, :], r=[f'yse{b}'], w=[])
            kb.barrier()
        with ExitStack() as ph2:
            wsg = kb.sb(ph2, 'wsg', [128, 8, 256], BF16)
            wsu = kb.sb(ph2, 'wsu', [128, 8, 256], BF16)
            wsd = kb.sb(ph2, 'wsd', [128, 2, 1024], BF16)
            load_w(wsg[:], 'wsg', kb.inp('ws_gate', [1024, 256]))
            load_w(wsu[:], 'wsu', kb.inp('ws_up', [1024, 256]))
            load_w(wsd[:], 'wsd', kb.inp('ws_down', [256, 1024]))
            sgs = [kb.sb(ph2, f'sgs{i}', [128, 256], F32) for i in range(2)]
            acts = [kb.sb(ph2, f'acts{i}', [128, 2, 128], BF16) for i in range(2)]
            acc = [kb.sb(ph2, f'acc{i}', [128, 1024], F32) for i in range(2)]
            x1t = [kb.sb(ph2, f'cb_x{i}', [128, 1024], F32) for i in range(2)]
            gk = [kb.sb(ph2, f'gkx{i}', [128, 1024], BF16) for i in range(16)]

            def comb_tile(i):
                b = i % 2
                isl = slice(i * 128, (i + 1) * 128)
                kb.dma('sp', x1t[b][:], x1s[isl, :], r=['x1s'], w=[f'cb_x{b}'])
                for k in range(8):
                    gb = b * 8 + k
                    if k == 4:
                        yield
                    kb.dma('pool', None, None, r=['ys_d', f'idx{i}'], w=[f'gk{gb}'],
                           fn=lambda e: e.indirect_dma_start(out=gk[gb][:], out_offset=None, in_=ys_d[:, :],
                                                             in_offset=bass.IndirectOffsetOnAxis(ap=idx[:, i, k:k + 1], axis=0)))
                    if k == 3:
                        for gu, (wt, wk, bank) in enumerate(((wsg, 'wsg', 4 * b), (wsu, 'wsu', 4 * b + 1))):
                            for m in range(2):
                                for kc in range(8):
                                    kb.mm(ps[bank][:, m * 128:(m + 1) * 128], wt[:, kc, m * 128:(m + 1) * 128], h2T[:, kc, isl],
                                          m == 0 and kc == 0, kc == 7, r=[wk, f'h2T{i}'], w=[f'ps{bank}'], sgc=True)
                        yield
                        kb.act(sgs[b][:], ps[4 * b][:, 0:256], AF.Silu, r=[f'ps{4 * b}'], w=[f'sgs{b}'])
                        yield
                        kb.tt('dve', acts[b][:].rearrange("p m s -> p (m s)"), sgs[b][:], ps[4 * b + 1][:, 0:256], ALU.mult, r=[f'sgs{b}', f'ps{4 * b + 1}'], w=[f'acts{b}'])
                        yield
                        for h in range(2):
                            for m in range(2):
                                kb.mm(ps[4 * b + 2 + h][:, :], acts[b][:, m, :], wsd[:, m, h * 512:(h + 1) * 512], m == 0, m == 1, r=[f'acts{b}', 'wsd'], w=[f'ps{4 * b + 2 + h}'])
                        yield
                        kb.act(acc[b][:, 0:512], ps[4 * b + 2][:, :], AF.Copy, r=[f'ps{4 * b + 2}'], w=[f'acc{b}'])
                        kb.act(acc[b][:, 512:1024], ps[4 * b + 3][:, :], AF.Copy, r=[f'ps{4 * b + 3}'], w=[f'acc{b}'])
                        yield
                for k in range(8):
                    gb = b * 8 + k
                    kb.stt(acc[b][:], gk[gb][:], w8[:, i, k:k + 1], acc[b][:], ALU.mult, ALU.add, r=[f'gk{gb}', f'w8_{i}_{k}', f'acc{b}'], w=[f'acc{b}'])
                    yield
                kb.tt('dve', acc[b][:], acc[b][:], gt2row[:], ALU.mult, r=[f'acc{b}', 'gt2row'], w=[f'acc{b}'])
                yield
                kb.tt('dve', acc[b][:], acc[b][:], x1t[b][:], ALU.add, r=[f'acc{b}', f'cb_x{b}'], w=[f'acc{b}'])
                yield
                kb.dma('sp', out_d[isl, :], acc[b][:], r=[f'acc{b}'], w=[])
            for i0_ in range(0, NO, 2):
                run_interleaved([comb_tile(i0_), comb_tile(i0_ + 1)], 2)
            kb.barrier()
    return finish_now()


def host_consts(hf):
    c = {}
    m = np.zeros((128, 2), np.float32)
    m[:, hf] = 1.0
    c['hfm'] = m
    f = np.float32
    own_t = (np.arange(SO) // 128 * 2 + hf) * 128 + np.arange(SO) % 128
    n = np.arange(256)
    vis = (n[:, None] <= 254) & (16 * n[:, None] + 31 <= own_t[None, :])
    c['cmb'] = np.ascontiguousarray(np.where(vis, 0.0, -BIG).astype(f).reshape(2, 128, SO).transpose(1, 0, 2))
    i = np.arange(128)
    selb = np.zeros((128, NO, 64), f)
    jj = np.arange(64)
    for j in range(NO):
        cur = 2 * (2 * j + hf) + (i >= 64)
        forced = (jj[None, :] == 0) | (jj[None, :] == cur[:, None]) | (jj[None, :] == cur[:, None] - 1)
        valid = jj[None, :] <= cur[:, None]
        selb[:, j, :] = np.where(forced, 100.0, np.where(valid, 0.0, -100.0))
    c['selb'] = selb
    p = np.arange(128)
    E = np.zeros((128, 32, 128), f)
    for kt in range(32):
        E[:, kt, :] = np.where((p[:, None] % 64) == 2 * kt + (i[None, :] // 64), BIG, 0.0)
    c['Etab'] = E
    key = np.arange(128)[:, None]
    q = np.arange(128)[None, :]
    tri = np.where(key > q, -BIG, 0.0).astype(f)
    cb = np.zeros((128, 2, 128), f)
    if hf == 0:
        cb[:, 0, :] = tri
        cb[:, 1, :] = -BIG
    else:
        cb[:, 0, :] = 0.0
        cb[:, 1, :] = tri
    c['cbt'] = cb
    wb = np.zeros((128, 6, 128), f)
    for ii in range(6):
        diff = 128 * (4 + hf - ii) + q - key
        wb[:, ii, :] = np.where((diff >= 0) & (diff < 512), 0.0, -BIG)
    c['wbm'] = wb
    return c


def prep_shared(inp):
    L = 0
    f = np.float32
    sh = {}
    sh['w_ada'] = np.ascontiguousarray(inp['w_ada'][L], f)
    b_ada = np.asarray(inp['b_ada'][L], f)
    sh['b_adaT'] = np.ascontiguousarray(b_ada.reshape(48, 128).T)
    sh['b_ada_row'] = np.ascontiguousarray(b_ada.reshape(1, 6144))
    sh['g1T'] = np.ascontiguousarray(np.asarray(inp['g_norm1'][L], f).reshape(8, 128).T)
    sh['g2row'] = np.ascontiguousarray(np.asarray(inp['g_norm2'][L], f).reshape(1, 1024))
    sh['w_in'] = np.ascontiguousarray(inp['w_in'][L], f)
    def pl(a):
        return np.ascontiguousarray(a.reshape(16, 2, 64).transpose(1, 2, 0).reshape(128, 16))
    are = pl(np.asarray(inp['a_re'][L], f))
    aim = pl(np.asarray(inp['a_im'][L], f))
    ldt = pl(np.repeat(np.asarray(inp['log_dt'][L], f)[:, None], 64, axis=1))
    sh['s5_prm'] = np.ascontiguousarray(np.stack([are, aim, ldt], axis=1))
    Bl = np.zeros((128, 2, 16, 128), f)
    Cl = np.zeros((128, 2, 16, 128), f)
    b_re = np.asarray(inp['b_re'][L], f)
    b_im = np.asarray(inp['b_im'][L], f)
    c_re = np.asarray(inp['c_re'][L], f)
    c_im = np.asarray(inp['c_im'][L], f)
    for g in range(32):
        pt, g2, gl = g // 2, g % 2, g % 8
        Bl[gl * 16:(gl + 1) * 16, 0, pt, g2 * 64:(g2 + 1) * 64] = b_re[g].T
        Bl[gl * 16:(gl + 1) * 16, 1, pt, g2 * 64:(g2 + 1) * 64] = b_im[g].T
        Cl[g2 * 64:(g2 + 1) * 64, 0, pt, gl * 16:(gl + 1) * 16] = c_re[g].T
        Cl[g2 * 64:(g2 + 1) * 64, 1, pt, gl * 16:(gl + 1) * 16] = c_im[g].T
    sh['s5_B'] = Bl
    sh['s5_C'] = Cl
    sh['dskipT'] = np.ascontiguousarray(np.asarray(inp['d_skip'][L], f).reshape(4, 128).T)
    sh['w_glu'] = np.ascontiguousarray(inp['w_glu'][L], f)
    sh['b_gluT'] = np.ascontiguousarray(np.asarray(inp['b_glu'][L], f).reshape(4, 128).T)

    def dup(a):
        return np.concatenate([a, a])
    sh['gainT'] = np.ascontiguousarray(np.stack([dup(np.asarray(inp[k][L], f)) for k in ('q_gain', 'kc_gain', 'ks_gain', 'kw_gain')], axis=1))
    cstart = 16 * np.arange(256)
    sstart = 64 * np.arange(64)
    ov = ((cstart[:, None] < sstart[None, :] + 64) & (cstart[:, None] + 32 > sstart[None, :])).astype(f)
    ov[255] = 0
    sh['ovl'] = np.ascontiguousarray(ov.reshape(2, 128, 64).transpose(1, 0, 2))
    for k_ in ('w_up_attn', 'w_up_ssm', 'w_out', 'w_router', 'w_gate', 'w_up', 'w_down', 'ws_gate', 'ws_up', 'ws_down'):
        sh[k_] = np.ascontiguousarray(inp[k_][L], f)
    sh['rbias'] = np.ascontiguousarray(np.asarray(inp['router_bias'][L], f).reshape(1, 256))
    i_ = np.arange(128)
    sh['SU'] = np.ascontiguousarray((i_[:, None] < i_[None, :]).astype(f))
    sh['eCAP1'] = np.ascontiguousarray(np.broadcast_to((np.arange(256) * CAP + 1).astype(f)[None, :], (128, 256)))
    for nm in ('k', 'v'):
        pe = np.asarray(inp['pe_' + nm][L], f)
        sh['peS_' + nm] = np.ascontiguousarray(pe.reshape(16, 2, 64).transpose(1, 2, 0).reshape(128, 16))
        sh[f'w_cmp_{nm}1'] = np.ascontiguousarray(inp[f'w_cmp_{nm}1'][L], f)
        sh[f'w_cmp_{nm}2'] = np.ascontiguousarray(inp[f'w_cmp_{nm}2'][L], f)
    return sh


def core_inputs(inp, sh, b, hf, names):
    f = np.float32
    d = dict(sh)
    x = np.asarray(inp['x'][b], f)
    d['xf'] = x
    d['xo'] = np.ascontiguousarray(x.reshape(32, 128, 1024)[hf::2].reshape(SO, D))
    d['cT'] = np.ascontiguousarray(np.asarray(inp['c'][b], f).reshape(8, 128).T)
    d.update(host_consts(hf))
    return {k: d[k] for k in names}


_CACHE = {}


def kernel(**inputs):
    if 'prog' not in _CACHE:
        _CACHE['prog'] = build('all')
    nc, kb = _CACHE['prog']
    sh = prep_shared(inputs)
    names = list(kb.inputs.keys())
    in_maps = []
    for core in range(8):
        b, hf = core // 2, core % 2
        in_maps.append(core_inputs(inputs, sh, b, hf, names))
    res = run_bass_kernel_spmd(nc, in_maps, core_ids=list(range(8)))
    out = np.zeros((4, 32, 128, 1024), np.float32)
    for core in range(8):
        b, hf = core // 2, core % 2
        out[b, hf::2] = res.results[core]['out'].reshape(16, 128, 1024)
    return out.reshape(4, S, D)
```
